# Optimizing a Trainium2 kernel written in Bass

```python
import math
import jax
import jax.numpy as jnp
from jax import lax
import numpy as np

D_MODEL = 1024
BATCH = 16
SEQ = 4096
DEPTH = 2

HEAD_DIM = 64
Q_BLOCK = 128
RMS_EPS = 1e-6
NEG_INF = -1e30
FORCE_SCORE = 1e9

N_BUCKETS = 32
BUCKET_EXACT = 16
BUCKET_MAX_DIST = 2048

A_HEADS = 4
A_CMP_LEN = 32
A_CMP_STRIDE = 16
A_PHI_HIDDEN = 128
A_SEL_BLOCK = 64
A_SEL_TOPK = 16
A_WINDOW = 512

B_GROUPS = ((128, 1), (512, 4), (2048, 16))
B_HEADS_PER_GROUP = 2
B_HEADS = B_HEADS_PER_GROUP * len(B_GROUPS)

C_HEADS = 4

D_HEADS = 4
D_Q_LORA = 256
D_KV_LORA = 128
D_NOPE = 64
D_ROPE = 32
D_VDIM = 64
ROPE_THETA = 10000.0

D_FF = 2816
FFN_CONV_WIDTH = 3

N_BRANCHES = 4
IN_SPLITS = (
    A_HEADS * HEAD_DIM,
    6 * HEAD_DIM,
    3 * A_HEADS,
    3 * B_HEADS * HEAD_DIM,
    3 * C_HEADS * HEAD_DIM,
    C_HEADS,
    D_Q_LORA,
    D_KV_LORA,
    D_ROPE,
)
D_IN = sum(IN_SPLITS)
A_OUT = A_HEADS * HEAD_DIM
B_OUT = B_HEADS_PER_GROUP * HEAD_DIM
C_OUT = C_HEADS * HEAD_DIM
D_OUT = D_HEADS * D_VDIM

kernel_name = 'hybrid_nsa_dilated_fox_mla_block'


def rms_norm(x, gain):
    xf = x.astype(jnp.float32)
    y = xf * lax.rsqrt(jnp.mean(xf * xf, axis=-1, keepdims=True) + RMS_EPS)
    return (y * gain.astype(jnp.float32)).astype(x.dtype)


def split_cols(a, sizes):
    return jnp.split(a, np.cumsum(sizes)[:-1].tolist(), axis=-1)


def rel_bucket(dist):
    dist = jnp.maximum(dist, 0)
    d = jnp.maximum(dist, 1).astype(jnp.float32)
    large = BUCKET_EXACT + (jnp.log(d / BUCKET_EXACT) / math.log(BUCKET_MAX_DIST / BUCKET_EXACT)
                            * (N_BUCKETS - BUCKET_EXACT)).astype(jnp.int32)
    large = jnp.minimum(large, N_BUCKETS - 1)
    return jnp.where(dist < BUCKET_EXACT, dist, large)


def masked_softmax(logits, mask):
    logits = jnp.where(mask, logits, NEG_INF)
    m = jnp.max(logits, axis=-1, keepdims=True)
    p = jnp.where(mask, jnp.exp(logits - m), 0.0)
    den = jnp.sum(p, axis=-1, keepdims=True)
    den_safe = jnp.maximum(den, 1e-30)
    return p / den_safe, m[..., 0] + jnp.log(den_safe[..., 0])


def sweep_query_blocks(fn, *q_arrays):
    b, s = q_arrays[0].shape[:2]
    nb = s // Q_BLOCK
    blocked = tuple(jnp.moveaxis(a.reshape((b, nb, Q_BLOCK) + a.shape[2:]), 1, 0) for a in q_arrays)
    out = lax.map(lambda args: fn(args[0], *args[1:]), (jnp.arange(nb, dtype=jnp.int32),) + blocked)
    return jnp.moveaxis(out, 0, 1).reshape((b, s) + out.shape[3:])


def rope(x, cos, sin):
    x1, x2 = jnp.split(x, 2, axis=-1)
    return jnp.concatenate([x1 * cos - x2 * sin, x2 * cos + x1 * sin], axis=-1)


def nsa_mixer(q, kv_a, gate_logits, cmp_pos, phi_k_w1, phi_k_w2, phi_v_w1, phi_v_w2, rel_table):
    b, s = q.shape[:2]
    k_cmp, v_cmp, k_slc, v_slc, k_win, v_win = jnp.split(kv_a, 6, axis=-1)
    scale = HEAD_DIM ** -0.5
    n_cmp = (s - A_CMP_LEN) // A_CMP_STRIDE + 1

    def compress(kv, w1, w2):
        ch = kv.reshape(b, s // A_CMP_STRIDE, A_CMP_STRIDE, HEAD_DIM)
        blocks = jnp.concatenate([ch[:, j:j + n_cmp] for j in range(A_CMP_LEN // A_CMP_STRIDE)], axis=2)
        blocks = (blocks + cmp_pos).reshape(b, n_cmp, A_CMP_LEN * HEAD_DIM)
        return jax.nn.gelu(blocks @ w1) @ w2

    kc = compress(k_cmp, phi_k_w1, phi_k_w2)
    vc = compress(v_cmp, phi_v_w1, phi_v_w2)
    cmp_start = jnp.arange(n_cmp) * A_CMP_STRIDE
    cmp_end = cmp_start + A_CMP_LEN - 1
    n_sel = s // A_SEL_BLOCK
    sel_start = jnp.arange(n_sel) * A_SEL_BLOCK
    overlap = ((cmp_start[:, None] <= sel_start[None, :] + A_SEL_BLOCK - 1)
               & (cmp_end[:, None] >= sel_start[None, :])).astype(jnp.float32)
    n_top = min(A_SEL_TOPK, n_sel)
    ks_blocks = k_slc.reshape(b, n_sel, A_SEL_BLOCK, HEAD_DIM)
    vs_blocks = v_slc.reshape(b, n_sel, A_SEL_BLOCK, HEAD_DIM)
    kw_pad = jnp.pad(k_win, ((0, 0), (A_WINDOW, 0), (0, 0)))
    vw_pad = jnp.pad(v_win, ((0, 0), (A_WINDOW, 0), (0, 0)))
    tab = rel_table[:, :A_HEADS]
    batch_ix = jnp.arange(b)[:, None, None]

    def block(bi, q_blk, g_blk):
        t = bi * Q_BLOCK + jnp.arange(Q_BLOCK)
        lc = jnp.einsum('bqhd,bcd->bhqc', q_blk, kc, preferred_element_type=jnp.float32) * scale
        p_c, _ = masked_softmax(lc, cmp_end[None, :] <= t[:, None])
        o_c = jnp.einsum('bhqc,bcd->bqhd', p_c.astype(vc.dtype), vc)
        imp = jnp.einsum('bhqc,cj->bqj', p_c, overlap)
        cur = (t // A_SEL_BLOCK)[:, None]
        j = jnp.arange(n_sel)[None, :]
        imp = jnp.where((j == 0) | (j == cur) | (j == cur - 1), FORCE_SCORE,
                        jnp.where(j > cur, -FORCE_SCORE, imp))
        _, sel = lax.top_k(imp, n_top)
        ks = ks_blocks[batch_ix, sel].reshape(b, Q_BLOCK, n_top * A_SEL_BLOCK, HEAD_DIM)
        vs = vs_blocks[batch_ix, sel].reshape(b, Q_BLOCK, n_top * A_SEL_BLOCK, HEAD_DIM)
        pos = (sel[..., None] * A_SEL_BLOCK + jnp.arange(A_SEL_BLOCK)).reshape(b, Q_BLOCK, n_top * A_SEL_BLOCK)
        dist = t[None, :, None] - pos
        ls = (jnp.einsum('bqhd,bqkd->bhqk', q_blk, ks, preferred_element_type=jnp.float32) * scale
              + jnp.moveaxis(tab[rel_bucket(dist)], -1, 1))
        p_s, _ = masked_softmax(ls, (dist >= 0)[:, None])
        o_s = jnp.einsum('bhqk,bqkd->bqhd', p_s.astype(vs.dtype), vs)
        kw = lax.dynamic_slice_in_dim(kw_pad, bi * Q_BLOCK, A_WINDOW + Q_BLOCK, axis=1)
        vw = lax.dynamic_slice_in_dim(vw_pad, bi * Q_BLOCK, A_WINDOW + Q_BLOCK, axis=1)
        kpos = bi * Q_BLOCK - A_WINDOW + jnp.arange(A_WINDOW + Q_BLOCK)
        dist_w = t[:, None] - kpos[None, :]
        mask_w = (kpos[None, :] >= 0) & (dist_w >= 0) & (dist_w < A_WINDOW)
        lw = (jnp.einsum('bqhd,bkd->bhqk', q_blk, kw, preferred_element_type=jnp.float32) * scale
              + jnp.moveaxis(tab[rel_bucket(dist_w)], -1, 0)[None])
        p_w, _ = masked_softmax(lw, mask_w)
        o_w = jnp.einsum('bhqk,bkd->bqhd', p_w.astype(vw.dtype), vw)
        g = jax.nn.sigmoid(g_blk.astype(jnp.float32)).astype(q_blk.dtype)
        return g[..., 0:1] * o_c + g[..., 1:2] * o_s + g[..., 2:3] * o_w

    return sweep_query_blocks(block, q, gate_logits)


def dilated_mixer(q, k, v, rel_table):
    scale = HEAD_DIM ** -0.5
    tab = rel_table[:, A_HEADS:]
    hsl = [slice(g * B_HEADS_PER_GROUP, (g + 1) * B_HEADS_PER_GROUP) for g in range(len(B_GROUPS))]
    k_groups = [k[:, :, hs] for hs in hsl]
    v_groups = [v[:, :, hs] for hs in hsl]

    def block(bi, q_blk):
        t = bi * Q_BLOCK + jnp.arange(Q_BLOCK)
        outs, lses = [], []
        for g, (window, dilation) in enumerate(B_GROUPS):
            offs = dilation * jnp.arange(window // dilation + 1)
            idx = t[:, None] - offs[None, :]
            safe = jnp.maximum(idx, 0)
            kg = k_groups[g][:, safe]
            vg = v_groups[g][:, safe]
            bias = jnp.moveaxis(tab[rel_bucket(offs), hsl[g]], -1, 0)
            logits = (jnp.einsum('bqhd,bqkhd->bhqk', q_blk[:, :, hsl[g]], kg, preferred_element_type=jnp.float32)
                      * scale + bias[None, :, None, :])
            p, lse = masked_softmax(logits, (idx >= 0)[None, None])
            outs.append(jnp.einsum('bhqk,bqkhd->bqhd', p.astype(vg.dtype), vg))
            lses.append(jnp.moveaxis(lse, -1, 1))
        wts = jax.nn.softmax(jnp.stack(lses), axis=0)
        return jnp.einsum('gbqh,gbqhd->bqhd', wts.astype(q_blk.dtype), jnp.stack(outs))

    return sweep_query_blocks(block, q)


def forgetting_mixer(q, k, v, f_logits, f_bias):
    s = q.shape[1]
    scale = HEAD_DIM ** -0.5
    log_f = jax.nn.log_sigmoid(f_logits.astype(jnp.float32) + f_bias.astype(jnp.float32))
    cum = jnp.cumsum(log_f, axis=1)
    cum_keys = jnp.moveaxis(cum, 1, 2)
    kpos = jnp.arange(s)

    def block(bi, q_blk, cum_blk):
        t = bi * Q_BLOCK + jnp.arange(Q_BLOCK)
        logits = (jnp.einsum('bqhd,bkhd->bhqk', q_blk, k, preferred_element_type=jnp.float32) * scale
                  + jnp.moveaxis(cum_blk, 1, 2)[..., None] - cum_keys[:, :, None, :])
        p, _ = masked_softmax(logits, kpos[None, :] <= t[:, None])
        return jnp.einsum('bhqk,bkhd->bqhd', p.astype(v.dtype), v)

    return sweep_query_blocks(block, q, cum)


def mla_mixer(q_lat, kv_lat, k_rope, q_norm, kv_norm, w_uq, w_ukv):
    b, s = q_lat.shape[:2]
    scale = (D_NOPE + D_ROPE) ** -0.5
    q = (rms_norm(q_lat, q_norm) @ w_uq).reshape(b, s, D_HEADS, D_NOPE + D_ROPE)
    kv = (rms_norm(kv_lat, kv_norm) @ w_ukv).reshape(b, s, D_HEADS, D_NOPE + D_VDIM)
    q_nope, q_rot = q[..., :D_NOPE], q[..., D_NOPE:]
    k_nope, v = kv[..., :D_NOPE], kv[..., D_NOPE:]
    inv_freq = ROPE_THETA ** (-jnp.arange(0, D_ROPE, 2, dtype=jnp.float32) / D_ROPE)
    ang = jnp.arange(s, dtype=jnp.float32)[:, None] * inv_freq[None, :]
    cos, sin = jnp.cos(ang).astype(q.dtype), jnp.sin(ang).astype(q.dtype)
    q_rot = rope(q_rot, cos[:, None], sin[:, None])
    k_rot = rope(k_rope, cos, sin)
    kpos = jnp.arange(s)

    def block(bi, qn_blk, qr_blk):
        t = bi * Q_BLOCK + jnp.arange(Q_BLOCK)
        logits = (jnp.einsum('bqhd,bkhd->bhqk', qn_blk, k_nope, preferred_element_type=jnp.float32)
                  + jnp.einsum('bqhr,bkr->bhqk', qr_blk, k_rot, preferred_element_type=jnp.float32)) * scale
        p, _ = masked_softmax(logits, kpos[None, :] <= t[:, None])
        return jnp.einsum('bhqk,bkhd->bqhd', p.astype(v.dtype), v)

    return sweep_query_blocks(block, q_nope, q_rot)


def setup_inputs(seed: int = 0) -> dict:
    key = jax.random.key(seed)
    ks = iter(list(jax.random.split(key, 32)))
    f32 = jnp.float32

    def nrm(shape, fan_in, scale=1.0):
        return scale * jax.random.normal(next(ks), shape, f32) * fan_in ** -0.5

    def gain(shape):
        return 1.0 + 0.05 * jax.random.normal(next(ks), shape, f32)

    return {
        'x': jax.random.normal(next(ks), (BATCH, SEQ, D_MODEL), f32),
        'rel_bias_table': 0.5 * jax.random.normal(next(ks), (N_BUCKETS, A_HEADS + B_HEADS), f32),
        'norm_attn_pre': gain((DEPTH, D_MODEL)),
        'norm_attn_post': gain((DEPTH, D_MODEL)),
        'norm_ffn_pre': gain((DEPTH, D_MODEL)),
        'norm_ffn_post': gain((DEPTH, D_MODEL)),
        'w_in': nrm((DEPTH, D_MODEL, D_IN), D_MODEL),
        'nsa_cmp_pos': 0.1 * jax.random.normal(next(ks), (DEPTH, A_CMP_LEN, HEAD_DIM), f32),
        'nsa_phi_k_w1': nrm((DEPTH, A_CMP_LEN * HEAD_DIM, A_PHI_HIDDEN), A_CMP_LEN * HEAD_DIM),
        'nsa_phi_k_w2': nrm((DEPTH, A_PHI_HIDDEN, HEAD_DIM), A_PHI_HIDDEN),
        'nsa_phi_v_w1': nrm((DEPTH, A_CMP_LEN * HEAD_DIM, A_PHI_HIDDEN), A_CMP_LEN * HEAD_DIM),
        'nsa_phi_v_w2': nrm((DEPTH, A_PHI_HIDDEN, HEAD_DIM), A_PHI_HIDDEN),
        'fox_forget_bias': 2.0 + 0.5 * jax.random.normal(next(ks), (DEPTH, C_HEADS), f32),
        'mla_q_norm': gain((DEPTH, D_Q_LORA)),
        'mla_kv_norm': gain((DEPTH, D_KV_LORA)),
        'mla_w_uq': nrm((DEPTH, D_Q_LORA, D_HEADS * (D_NOPE + D_ROPE)), D_Q_LORA),
        'mla_w_ukv': nrm((DEPTH, D_KV_LORA, D_HEADS * (D_NOPE + D_VDIM)), D_KV_LORA),
        'w_branch_a': nrm((DEPTH, A_OUT, D_MODEL), A_OUT),
        'w_branch_b': nrm((DEPTH, B_OUT, D_MODEL), B_OUT),
        'w_branch_c': nrm((DEPTH, C_OUT, D_MODEL), C_OUT),
        'w_branch_d': nrm((DEPTH, D_OUT, D_MODEL), D_OUT),
        'w_merge_gate': nrm((DEPTH, N_BRANCHES, D_MODEL, D_MODEL), D_MODEL),
        'w_o': nrm((DEPTH, D_MODEL, D_MODEL), D_MODEL),
        'ffn_w_up': nrm((DEPTH, D_MODEL, 2 * D_FF), D_MODEL),
        'ffn_conv_w': nrm((DEPTH, FFN_CONV_WIDTH, 2 * D_FF), FFN_CONV_WIDTH),
        'ffn_conv_b': 0.02 * jax.random.normal(next(ks), (DEPTH, 2 * D_FF), f32),
        'ffn_w_down': nrm((DEPTH, D_FF, D_MODEL), D_FF),
    }


def reference(x, rel_bias_table, norm_attn_pre, norm_attn_post, norm_ffn_pre, norm_ffn_post,
              w_in, nsa_cmp_pos, nsa_phi_k_w1, nsa_phi_k_w2, nsa_phi_v_w1, nsa_phi_v_w2,
              fox_forget_bias, mla_q_norm, mla_kv_norm, mla_w_uq, mla_w_ukv,
              w_branch_a, w_branch_b, w_branch_c, w_branch_d, w_merge_gate, w_o,
              ffn_w_up, ffn_conv_w, ffn_conv_b, ffn_w_down):
    batch, seq = x.shape[:2]
    for l in range(DEPTH):
        h = rms_norm(x, norm_attn_pre[l])
        (a_q, a_kv, a_g, b_qkv, c_qkv, c_f, d_qlat, d_kvlat, d_krope) = split_cols(h @ w_in[l], IN_SPLITS)
        o_a = nsa_mixer(a_q.reshape(batch, seq, A_HEADS, HEAD_DIM), a_kv,
                        a_g.reshape(batch, seq, A_HEADS, 3), nsa_cmp_pos[l],
                        nsa_phi_k_w1[l], nsa_phi_k_w2[l], nsa_phi_v_w1[l], nsa_phi_v_w2[l],
                        rel_bias_table)
        b_qkv = b_qkv.reshape(batch, seq, 3, B_HEADS, HEAD_DIM)
        o_b = dilated_mixer(b_qkv[:, :, 0], b_qkv[:, :, 1], b_qkv[:, :, 2], rel_bias_table)
        c_qkv = c_qkv.reshape(batch, seq, 3, C_HEADS, HEAD_DIM)
        o_c = forgetting_mixer(c_qkv[:, :, 0], c_qkv[:, :, 1], c_qkv[:, :, 2], c_f, fox_forget_bias[l])
        o_d = mla_mixer(d_qlat, d_kvlat, d_krope, mla_q_norm[l], mla_kv_norm[l], mla_w_uq[l], mla_w_ukv[l])
        branches = (
            (o_a.reshape(batch, seq, A_OUT), w_branch_a[l]),
            (o_b.reshape(batch, seq, B_OUT), w_branch_b[l]),
            (o_c.reshape(batch, seq, C_OUT), w_branch_c[l]),
            (o_d.reshape(batch, seq, D_OUT), w_branch_d[l]),
        )
        merged = None
        for i, (o_i, w_i) in enumerate(branches):
            term = jax.nn.sigmoid(h @ w_merge_gate[l, i]) * (o_i @ w_i)
            merged = term if i == 0 else merged + term
        x = x + rms_norm(merged @ w_o[l], norm_attn_post[l])
        h = rms_norm(x, norm_ffn_pre[l])
        u = h @ ffn_w_up[l]
        u_pad = jnp.pad(u, ((0, 0), (FFN_CONV_WIDTH - 1, 0), (0, 0)))
        conv = ffn_conv_b[l]
        for j in range(FFN_CONV_WIDTH):
            conv = conv + ffn_conv_w[l, j] * u_pad[:, j:j + seq]
        gate, val = jnp.split(conv, 2, axis=-1)
        y = (jax.nn.silu(gate) * val) @ ffn_w_down[l]
        x = x + rms_norm(y, norm_ffn_post[l])
    return x
```

```python
import math
import numpy as np
import concourse.bass as bass
import concourse.mybir as mybir
from concourse.bass_utils import run_bass_kernel_spmd

F32 = mybir.dt.float32
BF16 = mybir.dt.bfloat16
AF = mybir.ActivationFunctionType
ALU = mybir.AluOpType

S = 4096
D = 1024
NB = 2
DEPTH = 2
DFF = 2816
D_IN = 2992
EPS = 1e-6
NEG = -30000.0
TINY = 1e-30
NTC = 8
ENGS = ("pe", "act", "dve", "pool", "sp")


class Sem:
    def __init__(self, handle):
        self.h = handle
        self.count = 0
        self.last_op = None


class Buf:
    def __init__(self, name, dma_sems=None, disjoint=False):
        self.name = name
        self.writers = {}
        self.readers = {}
        self.dma_sems = dma_sems or []
        self.rr = 0
        self.disjoint = disjoint


class Op:
    __slots__ = ("eng", "fn", "deps", "needed", "val", "sem", "is_dma")

    def __init__(self, eng, fn):
        self.eng = eng
        self.fn = fn
        self.deps = []
        self.needed = False
        self.val = None
        self.sem = None
        self.is_dma = False


class Prog:
    def __init__(self, nc):
        self.nc = nc
        self.streams = {e: [] for e in ENGS}
        self.free_sems = []
        self.all_sems = []
        self.eng_sems = {e: Sem(nc.alloc_semaphore(f"eng_{e}")) for e in ENGS if e != "sp"}

    def sem_alloc(self):
        if self.free_sems:
            return self.free_sems.pop()
        s = Sem(self.nc.alloc_semaphore(f"ks{len(self.all_sems)}"))
        self.all_sems.append(s)
        return s

    def buf(self, name, ndma=0, disjoint=False):
        return Buf(name, [self.sem_alloc() for _ in range(ndma)], disjoint)

    def free_buf(self, b):
        self.free_sems.extend(b.dma_sems)
        b.dma_sems = []

    def _track(self, op, reads, writes):
        deps = []
        for b in reads:
            deps.extend(b.writers.values())
        for b in writes:
            if not b.disjoint:
                deps.extend(b.writers.values())
            deps.extend(b.readers.values())
        key = id(op.sem) if op.is_dma else op.eng
        for b in reads:
            b.readers[key] = op
        for b in writes:
            if b.disjoint:
                b.writers[key] = op
            else:
                b.writers = {key: op}
                b.readers = {}
        seen = set()
        for d in deps:
            if d is op or id(d) in seen:
                continue
            seen.add(id(d))
            if op.eng == "pe" and d.eng == "pe" and not d.is_dma and not op.is_dma:
                continue
            op.deps.append(d)
            d.needed = True

    def op(self, eng, fn, reads=(), writes=()):
        o = Op(eng, fn)
        self._track(o, reads, writes)
        self.streams[eng].append(o)
        return o

    def dma(self, fns, dst, reads=(), queue="sp", writes=()):
        if not isinstance(fns, (list, tuple)):
            fns = [fns]
        o = Op(queue, list(fns))
        o.is_dma = True
        s = dst.dma_sems[dst.rr % len(dst.dma_sems)]
        dst.rr += 1
        o.sem = s
        if s.last_op is not None:
            o.deps.append(s.last_op)
        s.count += 16 * len(fns)
        o.val = s.count
        s.last_op = o
        o.needed = True
        self._track(o, reads, [dst] + list(writes))
        self.streams[queue].append(o)
        return o

    def barrier(self):
        lasts = []
        for e in ENGS:
            for o in reversed(self.streams[e]):
                if not o.is_dma and o.fn is not None:
                    lasts.append(o)
                    break
        dmas = [s.last_op for s in self.all_sems if s.last_op is not None]
        for e in ENGS:
            o = Op(e, None)
            for d in lasts + dmas:
                o.deps.append(d)
                d.needed = True
            self.streams[e].append(o)

    def finalize_vals(self):
        for e in ENGS:
            if e == "sp":
                continue
            s = self.eng_sems[e]
            c = 0
            for o in self.streams[e]:
                if o.is_dma or o.fn is None:
                    continue
                if o.needed:
                    c += 1
                    o.val = c
                    o.sem = s

    def emit_stream(self, eng_name, eng):
        seen = {}
        for o in self.streams[eng_name]:
            waits = {}
            for d in o.deps:
                sid = id(d.sem)
                if sid not in waits or waits[sid][1] < d.val:
                    waits[sid] = (d.sem, d.val)
            for sid, (s, v) in waits.items():
                if seen.get(sid, 0) >= v:
                    continue
                seen[sid] = v
                eng.wait_ge(s.h, v)
            if o.fn is None:
                continue
            if o.is_dma:
                for f in o.fn:
                    f(eng).then_inc(o.sem.h, 16)
            else:
                ins = o.fn(eng)
                if o.needed:
                    ins.then_inc(o.sem.h, 1)

    def run_block(self, final_ops=()):
        self.finalize_vals()
        fw = {}
        for o in final_ops:
            if id(o.sem) not in fw or fw[id(o.sem)][1] < o.val:
                fw[id(o.sem)] = (o.sem, o.val)
        with self.nc.Block() as block:
            @block.tensor
            def _(e):
                self.emit_stream("pe", e)

            @block.scalar
            def _(e):
                self.emit_stream("act", e)

            @block.vector
            def _(e):
                self.emit_stream("dve", e)

            @block.gpsimd
            def _(e):
                self.emit_stream("pool", e)

            @block.sync
            def _(e):
                self.emit_stream("sp", e)
                for s, v in fw.values():
                    e.wait_ge(s.h, v)


class Tile:
    def __init__(self, ap, b):
        self.ap = ap
        self.b = b


DT_SIZE = {F32: 4, BF16: 2}


def _bucket(d):
    d = np.maximum(d, 0)
    df = np.maximum(d, 1).astype(np.float32)
    large = 16 + (np.log(df / np.float32(16)) / np.float32(math.log(2048 / 16)) * np.float32(16)).astype(np.int32)
    large = np.minimum(large, 31)
    return np.where(d < 16, d, large)


LA, LW, LB_, LC = 2688, 1536, 1152, 1024
OFF = 384


def _onehot(valid, bidx, L):
    oh = np.zeros((33, L), np.float32)
    idx = np.where(valid, bidx, 32)
    oh[idx, np.arange(L)] = 1.0
    return oh


def make_consts():
    c = {}
    c["c_ident"] = np.eye(128, dtype=np.float32)
    c["c_anti"] = np.eye(128, dtype=np.float32)[::-1].copy()
    i = np.arange(LA) - 511
    c["c_oh_a"] = _onehot(i >= 0, _bucket(i), LA)
    i = np.arange(LW) - 511
    c["c_oh_w"] = _onehot((i >= 0) & (i < 512), _bucket(i), LW)
    i = np.arange(LB_) - 511
    c["c_oh_b"] = np.stack([_onehot((i >= 0) & (i <= 128), _bucket(i * dil), LB_) for dil in (1, 4, 16)])
    i = np.arange(LC) - 511
    c["c_causal"] = np.where(i >= 0, 0.0, NEG).astype(np.float32)[None, :]
    cm = []
    key = {}
    for ct in range(2):
        for qc in range(8):
            cc = ct * 128 + np.arange(128)[:, None]
            t = qc * 512 + np.arange(512)[None, :]
            valid = (cc <= 254) & (16 * cc + 31 <= t)
            if valid.all() or not valid.any():
                key[(ct, qc)] = "all" if valid.all() else "none"
                continue
            key[(ct, qc)] = len(cm)
            cm.append(np.where(valid, 0.0, NEG).astype(np.float32))
    c["c_cmpmask"] = np.stack(cm)
    c["c_eall"] = (np.arange(S)[None, :] // 64 == np.arange(64)[:, None]).astype(np.float32)
    t = np.arange(S)
    cur = (t // 64)[:, None]
    j = np.arange(64)[None, :]
    add = np.zeros((S, 64), np.float32)
    keep = np.ones((S, 64), np.float32)
    fut = j > cur
    add[fut] = -1e9
    keep[fut] = 0
    for cond, val in ((j == cur - 1, 1e9), (j == cur, 2e9), (j == 0, 3e9)):
        cond = np.broadcast_to(cond, (S, 64))
        add[cond] = val
        keep[cond] = 0
    c["c_fkeep"] = keep.reshape(32, 128, 64).transpose(1, 0, 2).reshape(128, 32 * 64).copy()
    c["c_fadd"] = add.reshape(32, 128, 64).transpose(1, 0, 2).reshape(128, 32 * 64).copy()
    cs = np.arange(256) * 16
    ce = cs + 31
    ss = np.arange(64) * 64
    ov = ((cs[:, None] <= ss[None, :] + 63) & (ce[:, None] >= ss[None, :])).astype(np.float32)
    ov[255] = 0
    ovx = np.zeros((256, 128), np.float32)
    ovx[:, :64] = ov
    ovx[:, 64] = 1.0
    c["c_ovx"] = ovx.reshape(2, 128, 128)
    inv = (10000.0 ** (-np.arange(0, 32, 2, dtype=np.float32) / 32)).astype(np.float32)
    ang = np.arange(S, dtype=np.float32)[None, :] * inv[:, None]
    cos, sin = np.cos(ang).astype(np.float32), np.sin(ang).astype(np.float32)
    sc = np.float32(96 ** -0.5)
    rq = np.zeros((4, 96, S), np.float32)
    rq[0, 0:64] = sc
    rq[0, 64:80] = sc * cos
    rq[0, 80:96] = sc * cos
    rq[1, 64:80] = sc * sin
    rq[1, 80:96] = sc * sin
    rq[2, 64:80] = cos
    rq[2, 80:96] = cos
    rq[3, 64:80] = sin
    rq[3, 80:96] = sin
    c["c_rope"] = rq
    return c, key


CONSTS, CMPKEY = make_consts()

W_NAMES = ["rel_bias_table", "norm_attn_pre", "norm_attn_post", "norm_ffn_pre", "norm_ffn_post", "w_in", "nsa_cmp_pos",
           "nsa_phi_k_w1", "nsa_phi_k_w2", "nsa_phi_v_w1", "nsa_phi_v_w2", "fox_forget_bias", "mla_q_norm",
           "mla_kv_norm", "mla_w_uq", "mla_w_ukv", "w_branch_a", "w_branch_b", "w_branch_c", "w_branch_d",
           "w_merge_gate", "w_o", "ffn_w_up", "ffn_conv_w", "ffn_conv_b", "ffn_w_down"]
W_SHAPES = {
    "rel_bias_table": (32, 10), "norm_attn_pre": (2, 1024), "norm_attn_post": (2, 1024), "norm_ffn_pre": (2, 1024),
    "norm_ffn_post": (2, 1024), "w_in": (2, 1024, 2992), "nsa_cmp_pos": (2, 32, 64), "nsa_phi_k_w1": (2, 2048, 128),
    "nsa_phi_k_w2": (2, 128, 64), "nsa_phi_v_w1": (2, 2048, 128), "nsa_phi_v_w2": (2, 128, 64),
    "fox_forget_bias": (2, 4), "mla_q_norm": (2, 256), "mla_kv_norm": (2, 128), "mla_w_uq": (2, 256, 384),
    "mla_w_ukv": (2, 128, 512), "w_branch_a": (2, 256, 1024), "w_branch_b": (2, 128, 1024),
    "w_branch_c": (2, 256, 1024), "w_branch_d": (2, 256, 1024), "w_merge_gate": (2, 4, 1024, 1024),
    "w_o": (2, 1024, 1024), "ffn_w_up": (2, 1024, 5632), "ffn_conv_w": (2, 3, 5632), "ffn_conv_b": (2, 5632),
    "ffn_w_down": (2, 2816, 1024),
}

F_QA, F_CMP, F_KSLC, F_KWIN, F_GA, F_QB, F_KB, F_QC, F_KC, F_QD, F_KD, NF = 0, 2, 3, 4, 5, 11, 14, 17, 19, 21, 25, 29
V_SLC, V_WIN, V_B, V_C, V_D, NV = 0, 64, 128, 512, 768, 1024


def dram_ap(t, offset, dims):
    return bass.AP(t.tensor, t.offset + offset, [list(d) for d in dims])


class Builder:
    def __init__(self, nc, phases=None, dbg=False):
        self.nc = nc
        self.P = Prog(nc)
        self.phases = phases
        self.dbg = dbg
        self.uid = 0
        probe = nc.alloc_sbuf_tensor("sb_probe", [128, 8], F32)
        base = nc.lookup_mloc(probe).addr
        self.sb_base = (base + 32 + 63) // 64 * 64
        self.sb_limit = base + 32 + nc.sbuf_bytes_remaining - 64
        self.sb_top = self.sb_base
        self.final_ops = []
        self.ps = []
        for i in range(8):
            t = nc.alloc_psum_tensor(f"psb{i}", [128, 512], F32)
            self.ps.append(Tile(t.ap(), self.P.buf(f"ps{i}")))
        self.ps_rr = 0

    def tile(self, shape, dt, ndma=0, name="t", disjoint=False):
        free = 1
        for s_ in shape[1:]:
            free *= s_
        nbytes = (free * DT_SIZE[dt] + 63) // 64 * 64
        assert self.sb_top + nbytes <= self.sb_limit, f"SBUF overflow allocating {name} {shape}: top={self.sb_top - self.sb_base} need={nbytes}"
        self.uid += 1
        t = self.nc.alloc_sbuf_tensor_at(f"{name}_{self.uid}", list(shape), dt, offset=self.sb_top)
        self.sb_top += nbytes
        return Tile(t.ap(), self.P.buf(f"{name}_{self.uid}", ndma, disjoint))

    def mark(self):
        return (self.sb_top, list(self.P.free_sems), len(self.P.all_sems))

    def release(self, mark, tiles=()):
        for t in tiles:
            self.P.free_buf(t.b)
        self.sb_top = mark[0]

    def next_ps(self, lo=0, hi=8):
        n = hi - lo
        i = lo + (self.ps_rr % n)
        self.ps_rr += 1
        return self.ps[i]

    def dram(self, name, shape, dt, kind="Internal"):
        if self.dbg and kind == "Internal" and name in ("OS0", "HT0", "X10", "X20", "GEXTA"):
            kind = "ExternalOutput"
        return self.nc.dram_tensor(name, list(shape), dt, kind=kind).ap()

    def mm(self, ps, lhsT, rhs, start, stop, reads, writes):
        return self.P.op("pe", lambda e: e.matmul(ps, lhsT=lhsT, rhs=rhs, start=start, stop=stop), reads, writes)

    def tr(self, ps, in_, ident, reads, writes):
        return self.P.op("pe", lambda e: e.transpose(out=ps, in_=in_, identity=ident), reads, writes)

    def act(self, out, in_, func, reads, writes, bias=None, scale=None, accum=None):
        kw = {}
        if bias is not None:
            kw["bias"] = bias
        if scale is not None:
            kw["scale"] = scale
        if accum is not None:
            kw["accum_out"] = accum
        return self.P.op("act", lambda e: e.activation(out=out, in_=in_, func=func, **kw), reads, writes)

    def tt(self, eng, out, in0, in1, op, reads, writes):
        return self.P.op(eng, lambda e: e.tensor_tensor(out=out, in0=in0, in1=in1, op=op), reads, writes)

    def ts(self, eng, out, in0, s1, op0, reads, writes, s2=None, op1=None):
        if op1 is None:
            return self.P.op(eng, lambda e: e.tensor_scalar(out=out, in0=in0, scalar1=s1, scalar2=None, op0=op0), reads, writes)
        return self.P.op(eng, lambda e: e.tensor_scalar(out=out, in0=in0, scalar1=s1, scalar2=s2, op0=op0, op1=op1), reads, writes)

    def stt(self, eng, out, in0, scalar, in1, op0, op1, reads, writes):
        return self.P.op(eng, lambda e: e.scalar_tensor_tensor(out=out, in0=in0, scalar=scalar, in1=in1, op0=op0, op1=op1), reads, writes)

    def cp(self, eng, out, in_, reads, writes):
        if eng == "act":
            return self.P.op("act", lambda e: e.copy(out=out, in_=in_), reads, writes)
        return self.P.op(eng, lambda e: e.tensor_copy(out=out, in_=in_), reads, writes)

    def memset(self, eng, ap, val, writes):
        return self.P.op(eng, lambda e: e.memset(ap, val), [], writes)

    def recip(self, out, in_, reads, writes):
        return self.P.op("dve", lambda e: e.reciprocal(out=out, in_=in_), reads, writes)

    def dma(self, out, in_, dst, reads=(), queue="sp", writes=()):
        return self.P.dma(lambda e: e.dma_start(out=out, in_=in_), dst, reads, queue, writes)

    def rstd_from_ss(self, rs, ss, n, reads_writes):
        self.ts("dve", rs, ss, 1.0 / n, ALU.mult, reads_writes, reads_writes, s2=EPS, op1=ALU.add)
        self.act(rs, rs, AF.Sqrt, reads_writes, reads_writes)
        self.recip(rs, rs, reads_writes, reads_writes)

    def wload(self, dst_ap, src_ap, dst_buf, shape, post=None):
        st = self.wstage[self.wstage_rr % len(self.wstage)]
        self.wstage_rr += 1
        free = 1
        for s_ in shape[1:]:
            free *= s_
        assert free <= 1024
        view = st.ap[0:shape[0], 0:free]
        if len(shape) == 3:
            view = view.rearrange("p (a b) -> p a b", a=shape[1])
        self.dma(view, src_ap, st.b)
        if post is None:
            self.cp("pool" if dst_ap.base_partition() == 0 else "dve", dst_ap, view, [st.b], [dst_buf])
        else:
            post(view, st.b)

    def build(self):
        nc, P = self.nc, self.P
        self.x_in = nc.dram_tensor("x", [NB, S, D], F32, kind="ExternalInput").ap()
        self.y_out = nc.dram_tensor("y", [NB, S, D], F32, kind="ExternalOutput").ap()
        self.w = {n: nc.dram_tensor(n, list(W_SHAPES[n]), F32, kind="ExternalInput").ap() for n in W_NAMES}
        self.c = {n: nc.dram_tensor(n, list(v.shape), F32, kind="ExternalInput").ap() for n, v in CONSTS.items()}
        self.FS = [self.dram(f"FS{s}", [NF, 128, S], BF16) for s in range(NB)]
        self.FF = [self.dram(f"FF{s}", [4, S], F32) for s in range(NB)]
        self.VS = [self.dram(f"VS{s}", [S, NV], BF16) for s in range(NB)]
        self.HT = [self.dram(f"HT{s}", [8, 128, S], BF16) for s in range(NB)]
        self.OS = [(self.nc.dram_tensor(f"OS{s}", [7, 128, S], BF16, kind="ExternalInput").ap() if getattr(self, "os_input", False) else self.dram(f"OS{s}", [7, 128, S], BF16)) for s in range(NB)]
        self.X1 = [self.dram(f"X1{s}", [S, D], F32) for s in range(NB)]
        self.X2 = [self.dram(f"X2{s}", [S, D], F32) for s in range(NB)]
        self.AUG = [self.dram(f"AUG{s}", [2, 4, 6, S], BF16) for s in range(NB)]
        self.GA_ = self.dram("GEXTA", [4, LA], BF16)
        self.GW_ = self.dram("GEXTW", [4, LW], BF16)
        self.GB_ = self.dram("GEXTB", [6, LB_], BF16)
        self.GC_ = self.dram("GEXTC", [1, LC], BF16)
        nd = 4
        self.bFS = [P.buf(f"FS{s}", nd, True) for s in range(NB)]
        self.bFF = [P.buf(f"FF{s}", 1, True) for s in range(NB)]
        self.bVS = [P.buf(f"VS{s}", nd, True) for s in range(NB)]
        self.bHT = [P.buf(f"HT{s}", nd, True) for s in range(NB)]
        self.bOS = [P.buf(f"OS{s}", nd, True) for s in range(NB)]
        self.bX1 = [P.buf(f"X1{s}", nd, True) for s in range(NB)]
        self.bX2 = [P.buf(f"X2{s}", nd, True) for s in range(NB)]
        self.bAUG = [P.buf(f"AUG{s}", 1, True) for s in range(NB)]
        self.bG = P.buf("GEXT", 1, True)
        self.bY = P.buf("Y", nd, True)

        self.ident = self.tile([128, 128], BF16, name="ident")
        self.anti = self.tile([128, 128], BF16, name="anti")
        self.ones = self.tile([128, 128], BF16, name="ones")
        self.wstage = [self.tile([128, 1024], F32, 1, name="wst") for _ in range(2)]
        self.wstage_rr = 0
        self.wload(self.ident.ap, self.c["c_ident"], self.ident.b, [128, 128])
        self.wload(self.anti.ap, self.c["c_anti"], self.anti.b, [128, 128])
        self.memset("pool", self.ones.ap, 1.0, [self.ones.b])
        self.prologue_tables()
        P.barrier()
        for l in range(DEPTH):
            if self.want("A"):
                self.phase_A(l)
            if self.want("BC"):
                self.phase_BC(l)
            if self.want("BD"):
                self.phase_BD(l)
            if self.want("BB"):
                self.phase_BB(l)
            if self.want("BA"):
                self.phase_BA(l)
            if self.want("C1"):
                self.phase_C1(l)
            if self.want("C2"):
                self.phase_C2(l)
            if self.phases is not None and self.phases.get("layers", DEPTH) <= l + 1:
                break
        P.run_block(self.final_ops)

    def limit(self, chunks, key="nchunks"):
        if self.phases is not None and key in self.phases:
            return chunks[:self.phases[key]]
        return chunks

    def want(self, ph):
        return self.phases is None or ph in self.phases.get("run", ())

    def prologue_tables(self):
        m = self.mark()
        tabf = self.tile([33, 10], F32, 1, name="tabf")
        tab = self.tile([33, 10], BF16, name="tab")
        self.memset("pool", tabf.ap, NEG, [tabf.b])
        self.dma(tabf.ap[0:32, :], self.w["rel_bias_table"], tabf.b)
        self.cp("pool", tab.ap, tabf.ap, [tabf.b], [tab.b])
        jobs = [(self.c["c_oh_a"], LA, 0, 4, self.GA_), (self.c["c_oh_w"], LW, 0, 4, self.GW_)]
        for g in range(3):
            jobs.append((self.c["c_oh_b"][g], LB_, 4 + 2 * g, 2, self.GB_[2 * g:2 * g + 2, :]))
        ohf = self.tile([33, LA], F32, 1, name="ohf")
        oh = self.tile([33, LA], BF16, name="oh")
        gs = self.tile([4, LA], BF16, name="gs")
        for (src, L, h0, nh, dst) in jobs:
            self.dma(ohf.ap[:, 0:L], src, ohf.b)
            self.cp("pool", oh.ap[:, 0:L], ohf.ap[:, 0:L], [ohf.b], [oh.b])
            for c0 in range(0, L, 512):
                n = min(512, L - c0)
                ps = self.next_ps()
                self.mm(ps.ap[0:nh, 0:n], tab.ap[:, h0:h0 + nh], oh.ap[:, c0:c0 + n], True, True, [tab.b, oh.b], [ps.b])
                self.cp("dve", gs.ap[0:nh, c0:c0 + n], ps.ap[0:nh, 0:n], [ps.b], [gs.b])
            self.dma(dst, gs.ap[0:nh, 0:L], self.bG, [gs.b])
        cz = self.tile([1, LC], F32, 1, name="cz")
        czb = self.tile([1, LC], BF16, name="czb")
        self.dma(cz.ap, self.c["c_causal"], cz.b)
        self.cp("dve", czb.ap, cz.ap, [cz.b], [czb.b])
        self.dma(self.GC_, czb.ap, self.bG, [czb.b])
        self.release(m, [tabf, ohf, cz])

    def hankel(self, tile_ap, buf, src, row, W):
        in_ = bass.AP(src.tensor, src.offset + row * src.ap[0][0], [[1, 128], [1, W]])
        self.dma(tile_ap, in_, buf, [self.bG])

    def sbv(self, t_ap, off, dims):
        return bass.AP(t_ap.tensor, t_ap.offset + off, [[t_ap.ap[0][0], dims[0]]] + [list(d) for d in dims[1:]])

    def load_pvec(self, dst_ap, dst_buf, src_rows_ap, nrows, identf=None):
        fns = []
        for r in range(nrows):
            in_ = bass.AP(src_rows_ap.tensor, src_rows_ap.offset + r * 128, [[1, 128], [1, 1]])
            fns.append(lambda e, r=r, in_=in_: e.dma_start(out=dst_ap[:, r:r + 1], in_=in_))
        self.P.dma(fns, dst_buf)

    def phase_A(self, l):
        P = self.P
        m0 = self.mark()
        w_in = self.w["w_in"][l]
        NCOLS = 4104
        O_QA, O_CMP, O_KSLC, O_KWIN, O_GA, O_QB, O_KB, O_QC, O_KC, O_QL, O_KVL, O_KR, O_KRP, O_FL, O_T1, O_T2 = (
            0, 256, 384, 512, 640, 1408, 1792, 2176, 2432, 2688, 2944, 3072, 3200, 3328, 3336, 3848)
        Win = self.tile([128, 8, NCOLS], BF16, name="Win")

        def load_in(dst_off, src_col, n, post=None):
            c0 = 0
            while c0 < n:
                k = min(128, n - c0)
                src = w_in[:, src_col + c0: src_col + c0 + k].rearrange("(k p) n -> p k n", p=128)
                self.wload(Win.ap[:, :, dst_off + c0: dst_off + c0 + k], src, Win.b, [128, 8, k], post=post)
                c0 += k

        load_in(O_QA, 0, 384)
        for o_, c_ in ((O_KSLC, 384), (O_KWIN, 512)):
            load_in(o_, c_, 64)
            load_in(o_ + 64, c_, 64)

        def post_gates(view, sbuf):
            for h in range(4):
                for b in range(3):
                    gc = h * 3 + b
                    off = O_GA + ((h // 2) * 3 + b) * 128 + (h % 2) * 64
                    src = self.sbv(view, gc, [128, [12, 8], [0, 64]])
                    self.cp("pool", Win.ap[:, :, off:off + 64], src, [sbuf], [Win.b])
        load_in(0, 640, 12, post=post_gates)
        load_in(O_QB, 652, 384)
        load_in(O_KB, 1036, 384)
        load_in(O_QC, 1804, 256)
        load_in(O_KC, 2060, 256)
        load_in(O_QL, 2576, 256)
        load_in(O_KVL, 2832, 128)
        self.memset("pool", Win.ap[:, :, O_KR:O_KR + 256], 0.0, [Win.b])

        def post_kr(view, sbuf):
            self.cp("pool", Win.ap[:, :, O_KR + 64:O_KR + 96], view, [sbuf], [Win.b])
            self.ts("pool", Win.ap[:, :, O_KRP + 64:O_KRP + 80], view[:, :, 16:32], -1.0, ALU.mult, [sbuf], [Win.b])
            self.cp("pool", Win.ap[:, :, O_KRP + 80:O_KRP + 96], view[:, :, 0:16], [sbuf], [Win.b])
        load_in(0, 2960, 32, post=post_kr)
        load_in(O_FL, 2572, 4)
        load_in(O_T1, 448, 64)
        load_in(O_T1 + 64, 576, 64)
        load_in(O_T1 + 128, 1420, 384)
        load_in(O_T2, 2316, 256)

        gq = self.tile([128, 2], F32, 1, name="gq")
        gkv = self.tile([128, 1], F32, 1, name="gkv")
        self.load_pvec(gq.ap, gq.b, self.w["mla_q_norm"][l].rearrange("(k p) -> k p", p=128), 2)
        self.load_pvec(gkv.ap, gkv.b, self.w["mla_kv_norm"][l].rearrange("(k p) -> k p", p=128), 1)
        WQ = self.tile([128, 2, 4, 128], BF16, name="WQ")
        WQP = self.tile([128, 2, 4, 128], BF16, name="WQP")
        WK = self.tile([128, 4, 128], BF16, name="WK")
        WV = self.tile([128, 256], BF16, name="WV")
        self.memset("pool", WQP.ap, 0.0, [WQP.b])
        self.memset("pool", WQ.ap, 0.0, [WQ.b])
        self.memset("pool", WK.ap, 0.0, [WK.b])

        def post_uq(view, sbuf):
            for kc in range(2):
                v4 = view[:, kc, :].rearrange("p (h c) -> p h c", h=4)
                self.ts("pool", WQ.ap[:, kc, :, 0:96], v4, gq.ap[:, kc:kc + 1], ALU.mult, [sbuf, gq.b], [WQ.b])
            for kc in range(2):
                self.ts("pool", WQP.ap[:, kc, :, 64:80], WQ.ap[:, kc, :, 80:96], -1.0, ALU.mult, [WQ.b], [WQP.b])
                self.cp("pool", WQP.ap[:, kc, :, 80:96], WQ.ap[:, kc, :, 64:80], [WQ.b], [WQP.b])
        self.wload(None, self.w["mla_w_uq"][l].rearrange("(k p) n -> p k n", p=128), None, [128, 2, 384], post=post_uq)

        def post_ukv(view, sbuf):
            v4 = view.rearrange("p (h c) -> p h c", h=4)
            self.ts("pool", WK.ap[:, :, 0:64], v4[:, :, 0:64], gkv.ap[:, 0:1], ALU.mult, [sbuf, gkv.b], [WK.b])
            self.ts("pool", WV.ap.rearrange("p (h c) -> p h c", h=4), v4[:, :, 64:128], gkv.ap[:, 0:1], ALU.mult, [sbuf, gkv.b], [WV.b])
        self.wload(None, self.w["mla_w_ukv"][l], None, [128, 512], post=post_ukv)

        gain = self.tile([128, D], F32, 1, name="gain")
        g_src = self.w["norm_attn_pre"][l]
        self.dma(gain.ap, bass.AP(g_src.tensor, g_src.offset, [[0, 128], [1, D]]), gain.b)

        astep = 9 if self.phases is None else self.phases.get("astep", 9)
        xs = [self.tile([128, 4, D], F32, 1, name="xc") for _ in range(2)]
        hTs = [self.tile([128, 8, 512], BF16, name="hT") for _ in range(2)]
        hn = [self.tile([128, D], BF16, name="hn") for _ in range(2)]
        junk = self.tile([128, D], BF16, name="junk")
        stF = [self.tile([128, 8, 512], BF16, name="stF") for _ in range(2)]
        stT = self.tile([128, 4, NV], BF16, name="stT")
        stFF = self.tile([4, 512], F32, name="stFF")
        rope = self.tile([96, 4, 512], F32, 1, name="rope")
        qlT = self.tile([128, 2, 512], BF16, name="qlT")
        sqq = self.tile([128, 2, 512], BF16, name="sqq")
        kvT = self.tile([128, 512], BF16, name="kvT")
        kvf = self.tile([128, 512], F32, name="kvf")
        sqkv = self.tile([128, 512], BF16, name="sqkv")
        rq = self.tile([128, 512], F32, name="rq")
        rkv = self.tile([128, 512], F32, name="rkv")
        t1 = self.tile([128, 512], F32, name="t1")
        t2 = self.tile([128, 512], F32, name="t2")
        kr = self.tile([128, 512], BF16, name="kr")
        stat = self.tile([128, 16], F32, name="stat")
        stF_rr = [0]

        def x_src(s, tc):
            src = self.x_in[s] if l == 0 else self.X2[s]
            return src[tc * 512:(tc + 1) * 512, :].rearrange("(j p) d -> p j d", p=128)

        chunks = self.limit([(s, tc) for s in range(NB) for tc in range(NTC)])
        if astep < 1:
            chunks = []
        if chunks:
            pass
        if chunks:
            self.dma(xs[0].ap, x_src(*chunks[0]), xs[0].b, [] if l == 0 else [self.bX2[chunks[0][0]]])
        evac_rr = [0]

        def evac(out, ps, reads, writes, scale=None, func=None):
            if func is not None:
                return self.act(out, ps, func, reads, writes, scale=scale)
            evac_rr[0] += 1
            if evac_rr[0] % 2:
                if scale is None:
                    return self.cp("act", out, ps, reads, writes)
                return self.act(out, ps, AF.Copy, reads, writes, scale=scale)
            if scale is None:
                return self.cp("dve", out, ps, reads, writes)
            return self.ts("dve", out, ps, scale, ALU.mult, reads, writes)

        for ci, (s, tc) in enumerate(chunks):
            xc = xs[ci % 2]
            hT = hTs[ci % 2]
            if ci + 1 < len(chunks):
                s2, tc2 = chunks[ci + 1]
                self.dma(xs[(ci + 1) % 2].ap, x_src(s2, tc2), xs[(ci + 1) % 2].b, [] if l == 0 else [self.bX2[s2]])
            tok = slice(tc * 512, (tc + 1) * 512)
            self.dma(rope.ap, self.c["c_rope"][:, :, tok].rearrange("a r t -> r a t"), rope.b)
            self.memset("dve", stat.ap[:, 0:4], 0.0, [stat.b])
            for j in range(4):
                hj = hn[j % 2]
                self.act(junk.ap, xc.ap[:, j, :], AF.Square, [xc.b], [junk.b, stat.b], accum=stat.ap[:, j:j + 1])
            self.rstd_from_ss(stat.ap[:, 4:8], stat.ap[:, 0:4], D, [stat.b])
            for j in range(4):
                hj = hn[j % 2]
                self.stt("dve", hj.ap, xc.ap[:, j, :], stat.ap[:, 4 + j:5 + j], gain.ap, ALU.mult, ALU.mult, [xc.b, stat.b, gain.b], [hj.b])
                ps = self.next_ps()
                psb = ps.ap.bitcast(BF16).rearrange("p (k t) -> p k t", k=8)
                for kc in range(8):
                    self.tr(psb[:, kc, :], hj.ap[:, kc * 128:(kc + 1) * 128], self.ident.ap, [hj.b, self.ident.b], [ps.b])
                evac(hT.ap[:, :, j * 128:(j + 1) * 128], psb, [ps.b], [hT.b])
            self.dma(self.HT[s][:, :, tok].rearrange("k p t -> p k t"), hT.ap, self.bHT[s], [hT.b])
            if astep < 2:
                continue

            def fproj(off, M, dst_ap, dst_b, scale=None, func=None):
                ps = self.next_ps()
                for kc in range(8):
                    self.mm(ps.ap[0:M, :], Win.ap[:, kc, off:off + M], hT.ap[:, kc, :], kc == 0, kc == 7, [Win.b, hT.b], [ps.b])
                evac(dst_ap, ps.ap[0:M, :], [ps.b], [dst_b], scale=scale, func=func)
                return ps

            def fgroup(specs, f0):
                st = stF[stF_rr[0] % 2]
                stF_rr[0] += 1
                for i, (off, scale, func) in enumerate(specs):
                    fproj(off, 128, st.ap[:, i, :], st.b, scale, func)
                n = len(specs)
                self.dma(self.FS[s][f0:f0 + n, :, tok].rearrange("n p t -> p n t"), st.ap[:, 0:n, :], self.bFS[s], [st.b])

            fgroup([(O_QA, 0.125, None), (O_QA + 128, 0.125, None), (O_CMP, None, None), (O_KSLC, None, None), (O_KWIN, None, None)], F_QA)
            fgroup([(O_GA + 128 * i, None, AF.Sigmoid) for i in range(6)], F_GA)
            fgroup([(O_QB + 128 * i, 0.125, None) for i in range(3)] + [(O_KB + 128 * i, None, None) for i in range(3)], F_QB)
            fgroup([(O_QC + 128 * i, 0.125, None) for i in range(2)] + [(O_KC + 128 * i, None, None) for i in range(2)], F_QC)
            ps = self.next_ps()
            for kc in range(8):
                self.mm(ps.ap[0:4, :], Win.ap[:, kc, O_FL:O_FL + 4], hT.ap[:, kc, :], kc == 0, kc == 7, [Win.b, hT.b], [ps.b])
            self.cp("dve", stFF.ap, ps.ap[0:4, :], [ps.b], [stFF.b])
            self.dma(self.FF[s][:, tok], stFF.ap, self.bFF[s], [stFF.b])
            if astep < 3:
                continue

            for j in range(4):
                for (off, n, c0) in ((O_T1, 512, 0), (O_T2, 256, 512)):
                    ps = self.next_ps()
                    for kc in range(8):
                        self.mm(ps.ap[:, 0:n], hT.ap[:, kc, j * 128:(j + 1) * 128], Win.ap[:, kc, off:off + n], kc == 0, kc == 7, [Win.b, hT.b], [ps.b])
                    evac(stT.ap[:, j, c0:c0 + n], ps.ap[:, 0:n], [ps.b], [stT.b])

            if astep < 4:
                continue
            for i in range(2):
                ps = self.next_ps()
                for kc in range(8):
                    self.mm(ps.ap, Win.ap[:, kc, O_QL + 128 * i:O_QL + 128 * (i + 1)], hT.ap[:, kc, :], kc == 0, kc == 7, [Win.b, hT.b], [ps.b])
                self.cp("dve", qlT.ap[:, i, :], ps.ap, [ps.b], [qlT.b])
                self.act(sqq.ap[:, i, :], qlT.ap[:, i, :], AF.Square, [qlT.b], [sqq.b])
            ps = self.next_ps()
            for kc in range(8):
                self.mm(ps.ap, Win.ap[:, kc, O_KVL:O_KVL + 128], hT.ap[:, kc, :], kc == 0, kc == 7, [Win.b, hT.b], [ps.b])
            self.cp("dve", kvf.ap, ps.ap, [ps.b], [kvf.b])
            self.act(sqkv.ap, kvf.ap, AF.Square, [kvf.b], [sqkv.b])
            if astep < 3.5:
                continue
            ps = self.next_ps()
            for i in range(2):
                self.mm(ps.ap, self.ones.ap, sqq.ap[:, i, :], i == 0, i == 1, [self.ones.b, sqq.b], [ps.b])
            self.ts("dve", rq.ap, ps.ap, 1.0 / 256, ALU.mult, [ps.b], [rq.b], s2=EPS, op1=ALU.add)
            self.act(rq.ap, rq.ap, AF.Sqrt, [rq.b], [rq.b])
            self.recip(rq.ap, rq.ap, [rq.b], [rq.b])
            ps = self.next_ps()
            self.mm(ps.ap, self.ones.ap, sqkv.ap, True, True, [self.ones.b, sqkv.b], [ps.b])
            self.ts("dve", rkv.ap, ps.ap, 1.0 / 128, ALU.mult, [ps.b], [rkv.b], s2=EPS, op1=ALU.add)
            self.act(rkv.ap, rkv.ap, AF.Sqrt, [rkv.b], [rkv.b])
            self.recip(rkv.ap, rkv.ap, [rkv.b], [rkv.b])
            self.tt("dve", kvT.ap, kvf.ap, rkv.ap, ALU.mult, [kvf.b, rkv.b], [kvT.b])
            if astep < 5:
                continue
            var = 0 if self.phases is None else self.phases.get("var", 0)
            if var == 1:
                evac_rr[0] = 1
            psa = fproj(O_KR, 128, t1.ap, t1.b)
            if var == 1:
                evac_rr[0] = 1
            psb_ = fproj(O_KRP, 128, t2.ap, t2.b)
            if var == 2:
                continue
            self.tt("dve", t1.ap[64:96, :], t1.ap[64:96, :], rope.ap[64:96, 2, :], ALU.mult, [t1.b, rope.b], [t1.b])
            self.tt("dve", t2.ap[64:96, :], t2.ap[64:96, :], rope.ap[64:96, 3, :], ALU.mult, [t2.b, rope.b], [t2.b])
            self.tt("dve", kr.ap[64:96, :], t1.ap[64:96, :], t2.ap[64:96, :], ALU.add, [t1.b, t2.b], [kr.b])
            if astep < 5.1:
                continue
            stq = stF[stF_rr[0] % 2]
            stF_rr[0] += 1
            for h in range(4):
                ps1 = self.next_ps()
                for kc in range(2):
                    self.mm(ps1.ap, WQ.ap[:, kc, h, :], qlT.ap[:, kc, :], kc == 0, kc == 1, [WQ.b, qlT.b], [ps1.b])
                ps2 = self.next_ps()
                for kc in range(2):
                    self.mm(ps2.ap, WQP.ap[:, kc, h, :], qlT.ap[:, kc, :], kc == 0, kc == 1, [WQP.b, qlT.b], [ps2.b])
                self.tt("dve", t1.ap[0:96, :], ps1.ap[0:96, :], rope.ap[:, 0, :], ALU.mult, [ps1.b, rope.b], [t1.b])
                self.tt("dve", t2.ap[64:96, :], ps2.ap[64:96, :], rope.ap[64:96, 1, :], ALU.mult, [ps2.b, rope.b], [t2.b])
                self.tt("dve", t1.ap[64:96, :], t1.ap[64:96, :], t2.ap[64:96, :], ALU.add, [t1.b, t2.b], [t1.b])
                self.tt("dve", stq.ap[0:96, h, :], t1.ap[0:96, :], rq.ap[0:96, :], ALU.mult, [t1.b, rq.b], [stq.b])
                if astep < 5.2:
                    continue
                psk = self.next_ps()
                self.mm(psk.ap, WK.ap[:, h, :], kvT.ap, True, True, [WK.b, kvT.b], [psk.b])
                self.cp("act", stq.ap[0:64, 4 + h, :], psk.ap[0:64, :], [psk.b], [stq.b])
                self.cp("act", stq.ap[64:96, 4 + h, :], kr.ap[64:96, :], [kr.b], [stq.b])
            if astep < 5.3:
                continue
            self.dma(self.FS[s][F_QD:F_QD + 8, 0:96, tok].rearrange("n p t -> p n t"), stq.ap[0:96, :, :], self.bFS[s], [stq.b])
            if astep < 5.4:
                continue
            for j in range(4):
                ps = self.next_ps()
                self.mm(ps.ap[:, 0:256], kvT.ap[:, j * 128:(j + 1) * 128], WV.ap, True, True, [WV.b, kvT.b], [ps.b])
                self.cp("dve", stT.ap[:, j, V_D:V_D + 256], ps.ap[:, 0:256], [ps.b], [stT.b])
            self.dma(self.VS[s][tok, :].rearrange("(j p) c -> p j c", p=128), stT.ap, self.bVS[s], [stT.b])
        self.release(m0, xs + [gain, rope, gq, gkv])
        P.barrier()

    def load_big(self, dst, src2d, nk, ncol, col0=0, dcol0=0):
        c0 = 0
        step = max(1, 1024 // nk)
        step = min(step, 512)
        while c0 < ncol:
            k = min(step, ncol - c0)
            src = src2d[:, col0 + c0: col0 + c0 + k].rearrange("(k p) n -> p k n", p=128)
            self.wload(dst.ap[:, :, dcol0 + c0: dcol0 + c0 + k], src, dst.b, [128, nk, k])
            c0 += k

    def post_norm_residual(self, pss, xc_ap, xc_b, gain, out_ap, out_b, stat, junk):
        self.memset("dve", stat.ap[:, 0:2], 0.0, [stat.b])
        for n in range(2):
            sl = slice(n * 512, (n + 1) * 512)
            self.cp("act", out_ap[:, sl], pss[n].ap, [pss[n].b], [out_b])
            self.act(junk.ap[:, 0:512], out_ap[:, sl], AF.Square, [out_b], [junk.b, stat.b], accum=stat.ap[:, n:n + 1])
        self.tt("dve", stat.ap[:, 2:3], stat.ap[:, 0:1], stat.ap[:, 1:2], ALU.add, [stat.b], [stat.b])
        self.rstd_from_ss(stat.ap[:, 3:4], stat.ap[:, 2:3], D, [stat.b])
        for n in range(2):
            sl = slice(n * 512, (n + 1) * 512)
            self.stt("dve", out_ap[:, sl], out_ap[:, sl], stat.ap[:, 3:4], gain.ap[:, sl], ALU.mult, ALU.mult, [out_b, stat.b, gain.b], [out_b])
            self.tt("pool", out_ap[:, sl], out_ap[:, sl], xc_ap[:, sl], ALU.add, [out_b, xc_b], [out_b])

    def phase_C1(self, l):
        P = self.P
        m0 = self.mark()
        Wg = [self.tile([128, 8, D], BF16, name=f"Wg{i}") for i in range(4)]
        Wb = self.tile([128, 7, D], BF16, name="Wb")
        Wo = self.tile([128, 8, D], BF16, name="Wo")
        for i in range(4):
            self.load_big(Wg[i], self.w["w_merge_gate"][l, i], 8, D)
        for (nm, k0, nk) in (("w_branch_a", 0, 2), ("w_branch_b", 2, 1), ("w_branch_c", 3, 2), ("w_branch_d", 5, 2)):
            for kk in range(nk):
                for c0 in range(0, D, 512):
                    src = self.w[nm][l][kk * 128:(kk + 1) * 128, c0:c0 + 512]
                    self.wload(Wb.ap[:, k0 + kk, c0:c0 + 512], src, Wb.b, [128, 512])
        self.load_big(Wo, self.w["w_o"][l], 8, D)
        gain = self.tile([128, D], F32, 1, name="gain")
        g_src = self.w["norm_attn_post"][l]
        self.dma(gain.ap, bass.AP(g_src.tensor, g_src.offset, [[0, 128], [1, D]]), gain.b)
        hTs = [self.tile([128, 8, 512], BF16, 1, name="hT") for _ in range(2)]
        oTs = [self.tile([128, 7, 512], BF16, 1, name="oT") for _ in range(2)]
        xs = [self.tile([128, 4, D], F32, 1, name="xc") for _ in range(2)]
        mT = self.tile([128, 8, 512], BF16, name="mT")
        macc = self.tile([128, 512], F32, name="macc")
        sg = [self.tile([128, 512], F32, name="sg") for _ in range(2)]
        tm = [self.tile([128, 512], F32, name="tm") for _ in range(2)]
        xo = [self.tile([128, D], F32, name="xo") for _ in range(2)]
        junk = self.tile([128, 512], BF16, name="junk")
        stat = self.tile([128, 8], F32, name="stat")
        branch_k = [(0, 2), (2, 1), (3, 2), (5, 2)]
        chunks = self.limit([(s, tc) for s in range(NB) for tc in range(NTC)], "nchunks_c")

        def loads(ci):
            s, tc = chunks[ci]
            tok = slice(tc * 512, (tc + 1) * 512)
            self.dma(hTs[ci % 2].ap, self.HT[s][:, :, tok].rearrange("k p t -> p k t"), hTs[ci % 2].b, [self.bHT[s]])
            self.dma(oTs[ci % 2].ap, self.OS[s][:, :, tok].rearrange("k p t -> p k t"), oTs[ci % 2].b, [self.bOS[s]])
            src = self.x_in[s] if l == 0 else self.X2[s]
            self.dma(xs[ci % 2].ap, src[tok, :].rearrange("(j p) d -> p j d", p=128), xs[ci % 2].b, [] if l == 0 else [self.bX2[s]])
        loads(0)
        rr = 0
        for ci, (s, tc) in enumerate(chunks):
            if ci + 1 < len(chunks):
                loads(ci + 1)
            hT, oT, xc = hTs[ci % 2], oTs[ci % 2], xs[ci % 2]
            tok = slice(tc * 512, (tc + 1) * 512)
            for oc in range(8):
                ocs = slice(oc * 128, (oc + 1) * 128)
                for i in range(4):
                    psg = self.next_ps()
                    for kc in range(8):
                        self.mm(psg.ap, Wg[i].ap[:, kc, ocs], hT.ap[:, kc, :], kc == 0, kc == 7, [Wg[i].b, hT.b], [psg.b])
                    psb = self.next_ps()
                    k0, nk = branch_k[i]
                    for kk in range(nk):
                        self.mm(psb.ap, Wb.ap[:, k0 + kk, ocs], oT.ap[:, k0 + kk, :], kk == 0, kk == nk - 1, [Wb.b, oT.b], [psb.b])
                    g = sg[rr % 2]
                    t = tm[rr % 2]
                    rr += 1
                    self.act(g.ap, psg.ap, AF.Sigmoid, [psg.b], [g.b])
                    if i == 0:
                        self.tt("dve", macc.ap, g.ap, psb.ap, ALU.mult, [g.b, psb.b], [macc.b])
                    elif i < 3:
                        self.tt("dve", t.ap, g.ap, psb.ap, ALU.mult, [g.b, psb.b], [t.b])
                        self.tt("pool", macc.ap, macc.ap, t.ap, ALU.add, [macc.b, t.b], [macc.b])
                    else:
                        self.tt("dve", t.ap, g.ap, psb.ap, ALU.mult, [g.b, psb.b], [t.b])
                        self.tt("pool", mT.ap[:, oc, :], macc.ap, t.ap, ALU.add, [macc.b, t.b], [mT.b])
            for j in range(4):
                pss = [self.next_ps(), self.next_ps()]
                for n in range(2):
                    for kc in range(8):
                        self.mm(pss[n].ap, mT.ap[:, kc, j * 128:(j + 1) * 128], Wo.ap[:, kc, n * 512:(n + 1) * 512], kc == 0, kc == 7, [mT.b, Wo.b], [pss[n].b])
                o = xo[j % 2]
                self.post_norm_residual(pss, xc.ap[:, j, :], xc.b, gain, o.ap, o.b, stat, junk)
                self.dma(self.X1[s][tc * 512 + j * 128: tc * 512 + (j + 1) * 128, :], o.ap, self.bX1[s], [o.b])
        self.release(m0, hTs + oTs + xs + [gain])
        P.barrier()

    def phase_C2(self, l):
        P = self.P
        m0 = self.mark()
        TC = 256
        Wup = self.tile([128, 8, 2 * DFF], BF16, name="Wup")
        Wdn = self.tile([128, 22, D], BF16, name="Wdn")
        self.load_big(Wup, self.w["ffn_w_up"][l], 8, 2 * DFF)
        self.load_big(Wdn, self.w["ffn_w_down"][l], 22, D)
        cw = self.tile([128, 4, 44], F32, 1, name="cw")
        for j in range(3):
            self.load_pvec(cw.ap[:, j, :], cw.b, self.w["ffn_conv_w"][l, j].rearrange("(c p) -> c p", p=128), 44)
        self.load_pvec(cw.ap[:, 3, :], cw.b, self.w["ffn_conv_b"][l].rearrange("(c p) -> c p", p=128), 44)
        gpre = self.tile([128, D], F32, 1, name="gpre")
        gpost = self.tile([128, D], F32, 1, name="gpost")
        for t_, nm in ((gpre, "norm_ffn_pre"), (gpost, "norm_ffn_post")):
            g_src = self.w[nm][l]
            self.dma(t_.ap, bass.AP(g_src.tensor, g_src.offset, [[0, 128], [1, D]]), t_.b)
        NJ = TC // 128
        xs = [self.tile([128, NJ, D], F32, 1, name="xc") for _ in range(2)]
        hn = [self.tile([128, D], BF16, name="hn") for _ in range(2)]
        hT = self.tile([128, 8, TC], BF16, name="hT")
        aT = self.tile([128, 22, TC], BF16, name="aT")
        ug = [self.tile([128, TC + 2], F32, name="ug") for _ in range(2)]
        uv = [self.tile([128, TC + 2], F32, name="uv") for _ in range(2)]
        cg = [self.tile([128, TC], F32, name="cg") for _ in range(2)]
        cv = [self.tile([128, TC], F32, name="cv") for _ in range(2)]
        halo = self.tile([128, 44, 2], F32, name="halo")
        tv = self.tile([128, TC], F32, name="tv")
        xo = [self.tile([128, D], F32, name="xo") for _ in range(1)]
        junk = self.tile([128, D], BF16, name="junk")
        stat = self.tile([128, 16], F32, name="stat")
        last = (l == DEPTH - 1)
        chunks = self.limit([(s, tc) for s in range(NB) for tc in range(S // TC)], "nchunks_c2")

        def loads(ci):
            s, tc = chunks[ci]
            self.dma(xs[ci % 2].ap, self.X1[s][tc * TC:(tc + 1) * TC, :].rearrange("(j p) d -> p j d", p=128), xs[ci % 2].b, [self.bX1[s]])
        loads(0)
        rr = 0
        for ci, (s, tc) in enumerate(chunks):
            if ci + 1 < len(chunks):
                loads(ci + 1)
            xc = xs[ci % 2]
            if tc == 0:
                self.memset("pool", halo.ap, 0.0, [halo.b])
            self.memset("dve", stat.ap[:, 0:NJ], 0.0, [stat.b])
            for j in range(NJ):
                self.act(junk.ap, xc.ap[:, j, :], AF.Square, [xc.b], [junk.b, stat.b], accum=stat.ap[:, j:j + 1])
            self.rstd_from_ss(stat.ap[:, 4:4 + NJ], stat.ap[:, 0:NJ], D, [stat.b])
            for j in range(NJ):
                hj = hn[j % 2]
                self.stt("dve", hj.ap, xc.ap[:, j, :], stat.ap[:, 4 + j:5 + j], gpre.ap, ALU.mult, ALU.mult, [xc.b, stat.b, gpre.b], [hj.b])
                ps = self.next_ps()
                psb = ps.ap.bitcast(BF16).rearrange("p (k t) -> p k t", k=8)
                for kc in range(8):
                    self.tr(psb[:, kc, :], hj.ap[:, kc * 128:(kc + 1) * 128], self.ident.ap, [hj.b, self.ident.b], [ps.b])
                self.cp("act", hT.ap[:, :, j * 128:(j + 1) * 128], psb, [ps.b], [hT.b])
            for i in range(22):
                U = []
                for (ch, ubuf) in ((i, ug[rr % 2]), (22 + i, uv[rr % 2])):
                    ps = self.next_ps()
                    for kc in range(8):
                        self.mm(ps.ap[:, 0:TC], Wup.ap[:, kc, ch * 128:(ch + 1) * 128], hT.ap[:, kc, :], kc == 0, kc == 7, [Wup.b, hT.b], [ps.b])
                    self.cp("pool", ubuf.ap[:, 0:2], halo.ap[:, ch, :], [halo.b], [ubuf.b])
                    self.cp("act", ubuf.ap[:, 2:TC + 2], ps.ap[:, 0:TC], [ps.b], [ubuf.b])
                    self.cp("pool", halo.ap[:, ch, :], ubuf.ap[:, TC:TC + 2], [ubuf.b], [halo.b])
                    U.append((ch, ubuf))
                outs = (cg[rr % 2], cv[rr % 2])
                for (ch, ubuf), o, eng in zip(U, outs, ("dve", "dve")):
                    self.ts(eng, o.ap, ubuf.ap[:, 0:TC], cw.ap[:, 0, ch:ch + 1], ALU.mult, [ubuf.b, cw.b], [o.b], s2=cw.ap[:, 3, ch:ch + 1], op1=ALU.add)
                    for k_ in (1, 2):
                        if eng == "dve":
                            self.stt(eng, o.ap, ubuf.ap[:, k_:TC + k_], cw.ap[:, k_, ch:ch + 1], o.ap, ALU.mult, ALU.add, [ubuf.b, cw.b, o.b], [o.b])
                        else:
                            self.ts(eng, tv.ap, ubuf.ap[:, k_:TC + k_], cw.ap[:, k_, ch:ch + 1], ALU.mult, [ubuf.b, cw.b], [tv.b])
                            self.tt(eng, o.ap, o.ap, tv.ap, ALU.add, [o.b, tv.b], [o.b])
                g_, v_ = outs
                self.act(ug[rr % 2].ap[:, 0:TC], g_.ap, AF.Sigmoid, [g_.b], [ug[rr % 2].b])
                self.tt("dve", g_.ap, g_.ap, ug[rr % 2].ap[:, 0:TC], ALU.mult, [g_.b, ug[rr % 2].b], [g_.b])
                self.tt("pool", aT.ap[:, i, :], g_.ap, v_.ap, ALU.mult, [g_.b, v_.b], [aT.b])
                rr += 1
            for j in range(NJ):
                pss = [self.next_ps(), self.next_ps()]
                for n in range(2):
                    for kc in range(22):
                        self.mm(pss[n].ap, aT.ap[:, kc, j * 128:(j + 1) * 128], Wdn.ap[:, kc, n * 512:(n + 1) * 512], kc == 0, kc == 21, [aT.b, Wdn.b], [pss[n].b])
                o = xo[0]
                self.post_norm_residual(pss, xc.ap[:, j, :], xc.b, gpost, o.ap, o.b, stat_c2(stat), junk)
                r0 = tc * TC + j * 128
                if last:
                    op = self.dma(self.y_out[s][r0:r0 + 128, :], o.ap, self.bY, [o.b])
                    self.final_ops.append(op)
                else:
                    self.dma(self.X2[s][r0:r0 + 128, :], o.ap, self.bX2[s], [o.b])
        self.release(m0, xs + [gpre, gpost, cw])
        P.barrier()


class _StatView:
    def __init__(self, ap, b):
        self.ap = ap
        self.b = b


def stat_c2(stat):
    return _StatView(stat.ap[:, 8:16], stat.b)


def _attn_job(self, q_ap, n, ktiles, fin, rbufs, pring):
    psO = self.next_ps(4, 6)
    nk = len(ktiles)
    pend = None
    pts = []

    def pv(idx, pt, v_l, c0):
        self.mm(psO.ap[:, c0:n], v_l, pt.ap[:, c0:n], idx == 0, idx == nk - 1, rbufs + [pt.b], [psO.b])
    for idx, (k_l, extras, v_l, c0) in enumerate(ktiles):
        psS = self.next_ps(0, 4)
        mms = [(k_l, q_ap[:, c0:n])] + list(extras)
        for mi, (l_, r_) in enumerate(mms):
            self.mm(psS.ap[:, c0:n], l_, r_, mi == 0, mi == len(mms) - 1, rbufs, [psS.b])
        pt = pring[self.p_rr % len(pring)]
        self.p_rr += 1
        pts.append(pt)
        self.act(pt.ap[:, c0:n], psS.ap[:, c0:n], AF.Exp, [psS.b], [pt.b])
        if pend is not None:
            pv(*pend)
        pend = (idx, pt, v_l, c0)
    pv(*pend)
    fin(psO, pts)


def _recip_den(self, psO, rc, n):
    r = rc.ap[0:64, 0:n]
    self.ts("dve", r, psO.ap[64:128, 0:n], TINY, ALU.max, [psO.b], [rc.b])
    self.recip(r, r, [rc.b], [rc.b])
    return r


def _load_v(self, V, s, col0, nh):
    self.memset("pool", V.ap[:, :, :, 64:128], 1.0, [V.b])
    src = self.VS[s][:, col0:col0 + nh * 64].rearrange("(j p) (h c) -> p j h c", p=128, h=nh)
    for h in range(nh):
        self.dma(V.ap[:, :, h, 0:64], src[:, :, h, :], V.b, [self.bVS[s]])


def _nqc(self):
    if self.phases is not None and "nqc" in self.phases:
        return self.phases["nqc"]
    return 8


def _nseq(self):
    if self.phases is not None and "nseq" in self.phases:
        return self.phases["nseq"]
    return NB


def _phase_causal(self, l, kind):
    P = self.P
    m0 = self.mark()
    rows = 96 if kind == "D" else 70
    vcol = V_D if kind == "D" else V_C
    os0 = 5 if kind == "D" else 3
    Hc = self.tile([128, 896], BF16, 1, name="Hc")
    self.hankel(Hc.ap, Hc.b, self.GC_, 0, 896)
    pring = [self.tile([128, 512], BF16, name="pt") for _ in range(4)]
    rcs = [self.tile([128, 512], F32, name="rc") for _ in range(2)]
    osts = [self.tile([128, 2, 512], BF16, name="ost") for _ in range(2)]
    QT = [self.tile([rows, S], BF16, 1, name="QT") for _ in range(4)]
    KT = [self.tile([rows, S], BF16, 1, name="KT") for _ in range(4)]
    V = self.tile([128, 32, 4, 128], BF16, 1, name="V")
    extra_tiles = []
    if kind == "C":
        W = 1024
        fx = self.tile([4, W], F32, 1, name="fx")
        e1 = self.tile([4, W], F32, name="e1")
        onesf = self.tile([4, W], F32, name="onesf")
        cn = self.tile([4, W], F32, name="cn")
        t32 = self.tile([4, W], F32, name="t32")
        augk = self.tile([4, 6, W], BF16, name="augk")
        augq = self.tile([4, 6, W], BF16, name="augq")
        carry = self.tile([4, 1], F32, name="carry")
        fb = self.tile([4, 1], F32, 1, name="fb")
        fsrc = self.w["fox_forget_bias"][l]
        self.dma(fb.ap, bass.AP(fsrc.tensor, fsrc.offset, [[1, 4], [1, 1]]), fb.b)
        self.memset("pool", onesf.ap, 1.0, [onesf.b])
        self.memset("pool", augk.ap[:, 0:3, :], 1.0, [augk.b])
        self.memset("pool", augq.ap[:, 3:6, :], 1.0, [augq.b])
        extra_tiles = [fx, fb]
    self.p_rr = 0
    rr = 0
    for s in range(_nseq(self)):
        if kind == "C":
            for k in range(S // W):
                cols = slice(k * W, (k + 1) * W)
                self.dma(fx.ap, self.FF[s][:, cols], fx.b, [self.bFF[s]])
                self.ts("dve", e1.ap, fx.ap, fb.ap[:, 0:1], ALU.add, [fx.b, fb.b], [e1.b])
                self.act(e1.ap, e1.ap, AF.Exp, [e1.b], [e1.b], scale=-1.0)
                self.act(e1.ap, e1.ap, AF.Ln, [e1.b], [e1.b], bias=1.0)
                init = 0.0 if k == 0 else carry.ap[:, 0:1]
                self.P.op("dve", lambda e, init=init: e.tensor_tensor_scan(out=cn.ap, data0=onesf.ap, data1=e1.ap, initial=init, op0=ALU.mult, op1=ALU.add),
                          [onesf.b, e1.b, carry.b], [cn.b])
                self.cp("dve", carry.ap, cn.ap[:, W - 1:W], [cn.b], [carry.b])
                self.cp("dve", augk.ap[:, 3, :], cn.ap, [cn.b], [augk.b])
                self.cp("dve", t32.ap, augk.ap[:, 3, :], [augk.b], [t32.b])
                self.tt("dve", e1.ap, cn.ap, t32.ap, ALU.subtract, [cn.b, t32.b], [e1.b])
                self.cp("dve", augk.ap[:, 4, :], e1.ap, [e1.b], [augk.b])
                self.cp("dve", t32.ap, augk.ap[:, 4, :], [augk.b], [t32.b])
                self.tt("dve", e1.ap, e1.ap, t32.ap, ALU.subtract, [e1.b, t32.b], [e1.b])
                self.cp("dve", augk.ap[:, 5, :], e1.ap, [e1.b], [augk.b])
                self.ts("dve", augq.ap[:, 0:3, :], augk.ap[:, 3:6, :], -1.0, ALU.mult, [augk.b], [augq.b])
                self.dma(self.AUG[s][0][:, :, cols], augq.ap, self.bAUG[s], [augq.b])
                self.dma(self.AUG[s][1][:, :, cols], augk.ap, self.bAUG[s], [augk.b])
        for h in range(4):
            if kind == "D":
                self.dma(QT[h].ap, self.FS[s][F_QD + h, 0:96, :], QT[h].b, [self.bFS[s]])
                self.dma(KT[h].ap, self.FS[s][F_KD + h, 0:96, :], KT[h].b, [self.bFS[s]])
            else:
                rws = slice((h % 2) * 64, (h % 2) * 64 + 64)
                self.dma(QT[h].ap[0:64, :], self.FS[s][F_QC + h // 2, rws, :], QT[h].b, [self.bFS[s]])
                self.dma(QT[h].ap[64:70, :], self.AUG[s][0, h], QT[h].b, [self.bAUG[s]])
                self.dma(KT[h].ap[0:64, :], self.FS[s][F_KC + h // 2, rws, :], KT[h].b, [self.bFS[s]])
                self.dma(KT[h].ap[64:70, :], self.AUG[s][1, h], KT[h].b, [self.bAUG[s]])
        _load_v(self, V, s, vcol, 4)
        for c in range(_nqc(self)):
            ost = osts[c % 2]
            for h in range(4):
                rc = rcs[rr % 2]
                rr += 1
                kt = []
                for j in range(4 * c + 4):
                    c0 = max(0, 128 * (j - 4 * c))
                    extras = [(self.anti.ap, Hc.ap[:, 384:384 + 512 - c0])] if j >= 4 * c else []
                    kt.append((KT[h].ap[:, j * 128:(j + 1) * 128], extras, V.ap[:, j, h, :], c0))
                out_ap = ost.ap[(h % 2) * 64:(h % 2) * 64 + 64, h // 2, :]

                def fin(psO, pts, out_ap=out_ap, rc=rc, ost=ost):
                    r = _recip_den(self, psO, rc, 512)
                    self.tt("dve", out_ap, psO.ap[0:64, :], r, ALU.mult, [psO.b, rc.b], [ost.b])
                _attn_job(self, QT[h].ap[:, c * 512:(c + 1) * 512], 512, kt, fin, [QT[h].b, KT[h].b, V.b, Hc.b, self.anti.b], pring)
            self.dma(self.OS[s][os0:os0 + 2, :, c * 512:(c + 1) * 512].rearrange("k p t -> p k t"), ost.ap, self.bOS[s], [ost.b])
    self.release(m0, [Hc, V] + QT + KT + extra_tiles)
    P.barrier()


def _phase_BB(self, l):
    P = self.P
    m0 = self.mark()
    Hb = self.tile([128, 6, 1024], BF16, 1, name="Hb")
    for i in range(6):
        self.hankel(Hb.ap[:, i, :], Hb.b, self.GB_, i, 1024)
    pring = [self.tile([128, 512], BF16, name="pt") for _ in range(4)]
    rcs = [self.tile([128, 512], F32, name="rc") for _ in range(2)]
    osts = [self.tile([128, 512], BF16, name="ost") for _ in range(2)]
    QB = self.tile([128, 3, S], BF16, 1, name="QB")
    KB = self.tile([128, 3, S], BF16, 1, name="KB")
    acc = self.tile([128, 2, S], F32, name="acc")
    Vr = [self.tile([128, 32, 2, 128], BF16, 1, name="Vg") for _ in range(2)]
    self.p_rr = 0
    rr = 0
    for s in range(_nseq(self)):
        self.dma(QB.ap, self.FS[s][F_QB:F_QB + 3, :, :].rearrange("k p t -> p k t"), QB.b, [self.bFS[s]])
        self.dma(KB.ap, self.FS[s][F_KB:F_KB + 3, :, :].rearrange("k p t -> p k t"), KB.b, [self.bFS[s]])
        for g, dil in enumerate((1, 4, 16)):
            Vg = Vr[g % 2]
            nj = 32 // dil
            self.memset("pool", Vg.ap[:, :, :, 64:128], 1.0, [Vg.b])
            for r in range(dil):
                for h in range(2):
                    src = dram_ap(self.VS[s], r * NV + V_B + g * 128 + h * 64, [[dil * NV, 128], [128 * dil * NV, nj], [1, 64]])
                    self.dma(Vg.ap[:, r * nj:(r + 1) * nj, h, 0:64], src, Vg.b, [self.bVS[s]])
            L = S // dil
            n = min(512, L)
            for h in range(2):
                hb = 64 * h
                for r in range(dil):
                    for uq0 in range(0, L, n):
                        kt = []
                        for j in range(uq0 // 128 - 1, (uq0 + n) // 128):
                            if j < 0:
                                continue
                            dlt = uq0 - 128 * j
                            c0 = max(0, -dlt)
                            k_l = self.sbv(KB.ap[hb:hb + 64, g, :], r + dil * 128 * j, [64, [dil, 128]])
                            extras = [(self.anti.ap, Hb.ap[:, 2 * g + h, dlt + 384 + c0:dlt + 384 + n])]
                            kt.append((k_l, extras, Vg.ap[:, r * nj + j, h, :], c0))
                        q_ap = self.sbv(QB.ap[hb:hb + 64, g, :], r + dil * uq0, [64, [dil, n]])
                        accv = self.sbv(acc.ap[:, h, :], r + dil * uq0, [128, [dil, n]])

                        def fin(psO, pts, accv=accv, n=n, g=g):
                            if g == 0:
                                self.cp("act", accv, psO.ap[:, 0:n], [psO.b], [acc.b])
                            else:
                                self.tt("dve", accv, accv, psO.ap[:, 0:n], ALU.add, [psO.b, acc.b], [acc.b])
                        _attn_job(self, q_ap, n, kt, fin, [QB.b, KB.b, Vg.b, Hb.b, self.anti.b], pring)
        for c in range(8):
            ost = osts[c % 2]
            for h in range(2):
                rc = rcs[rr % 2]
                rr += 1
                r_ = rc.ap[0:64, :]
                self.ts("dve", r_, acc.ap[64:128, h, c * 512:(c + 1) * 512], TINY, ALU.max, [acc.b], [rc.b])
                self.recip(r_, r_, [rc.b], [rc.b])
                self.tt("dve", ost.ap[h * 64:h * 64 + 64, :], acc.ap[0:64, h, c * 512:(c + 1) * 512], r_, ALU.mult, [acc.b, rc.b], [ost.b])
            self.dma(self.OS[s][2, :, c * 512:(c + 1) * 512], ost.ap, self.bOS[s], [ost.b])
    self.release(m0, [Hb, QB, KB] + Vr)
    P.barrier()


def _phase_BA(self, l):
    P = self.P
    m0 = self.mark()
    Hs = self.tile([128, 4, 2560], BF16, 1, name="Hs")
    Hw = self.tile([128, 4, 1408], BF16, 1, name="Hw")
    for h in range(4):
        self.hankel(Hs.ap[:, h, :], Hs.b, self.GA_, h, 2560)
        self.hankel(Hw.ap[:, h, :], Hw.b, self.GW_, h, 1408)
    NM = CONSTS["c_cmpmask"].shape[0]
    cmask = self.tile([128, NM, 512], BF16, name="cmask")
    for i in range(NM):
        self.wload(cmask.ap[:, i, :], self.c["c_cmpmask"][i], cmask.b, [128, 512])
    eall = self.tile([64, S], BF16, name="eall")
    for k in range(4):
        self.wload(eall.ap[:, k * 1024:(k + 1) * 1024], self.c["c_eall"][:, k * 1024:(k + 1) * 1024], eall.b, [64, 1024])
    ovx = self.tile([128, 2, 128], BF16, name="ovx")
    for ct in range(2):
        self.wload(ovx.ap[:, ct, :], self.c["c_ovx"][ct], ovx.b, [128, 128])
    w1 = self.tile([128, 32, 128], BF16, name="w1")
    for kv, nm in enumerate(("nsa_phi_k_w1", "nsa_phi_v_w1")):
        src = self.w[nm][l].rearrange("(i d) m -> d i m", d=64)
        for i0 in range(0, 32, 8):
            self.wload(w1.ap[kv * 64:kv * 64 + 64, i0:i0 + 8, :], src[:, i0:i0 + 8, :], w1.b, [64, 8, 128])
    w2 = self.tile([128, 2, 128], BF16, name="w2")
    self.wload(w2.ap[:, 0, 0:64], self.w["nsa_phi_k_w2"][l], w2.b, [128, 64])
    self.wload(w2.ap[:, 0, 64:128], self.w["nsa_phi_k_w2"][l], w2.b, [128, 64])
    self.wload(w2.ap[:, 1, 0:64], self.w["nsa_phi_v_w2"][l], w2.b, [128, 64])
    pp = self.tile([32, 128], BF16, name="pp")
    self.wload(pp.ap[:, 0:64], self.w["nsa_cmp_pos"][l], pp.b, [32, 64])
    self.wload(pp.ap[:, 64:128], self.w["nsa_cmp_pos"][l], pp.b, [32, 64])
    posT = self.tile([128, 32], BF16, name="posT")
    ps = self.next_ps(6, 8)
    psb = ps.ap.bitcast(BF16)
    self.tr(psb[:, 0:32], pp.ap, self.ident.ap[0:32, 0:32], [pp.b, self.ident.b], [ps.b])
    self.cp("dve", posT.ap, psb[:, 0:32], [ps.b], [posT.b])
    posc = self.tile([128, 2], F32, name="posc")
    ps = self.next_ps(6, 8)
    for kv in range(2):
        for i in range(32):
            self.mm(ps.ap[:, kv:kv + 1], w1.ap[kv * 64:kv * 64 + 64, i, :], posT.ap[kv * 64:kv * 64 + 64, i:i + 1], i == 0, i == 31, [w1.b, posT.b], [ps.b])
    self.cp("dve", posc.ap, ps.ap[:, 0:2], [ps.b], [posc.b])

    pring = [self.tile([128, 512], BF16, name="pt") for _ in range(4)]
    rcs = [self.tile([128, 512], F32, name="rc") for _ in range(2)]
    tmps = [self.tile([128, 512], F32, name="tmp") for _ in range(2)]
    osts = [self.tile([128, 2, 512], BF16, name="ost") for _ in range(2)]
    Oacc = self.tile([128, 2, 512], F32, name="Oacc")
    CMP = self.tile([128, S], BF16, 1, name="CMP")
    gk = [self.tile([128, 256], BF16, name="gkv") for _ in range(2)]
    kcT = self.tile([128, 256], BF16, name="kcT")
    Vc = self.tile([128, 2, 128], BF16, name="Vc")
    QA = self.tile([128, 2, S], BF16, 1, name="QA")
    KS = self.tile([128, S], BF16, 1, name="KS")
    KW = self.tile([128, S], BF16, 1, name="KW")
    V2 = self.tile([128, 32, 2, 128], BF16, 1, name="V2")
    GAr = [self.tile([128, 6, 512], BF16, 1, name="GAc") for _ in range(2)]
    fkr = [self.tile([128, 4, 64], F32, 1, name="fk") for _ in range(2)]
    far = [self.tile([128, 4, 64], F32, 1, name="fa") for _ in range(2)]
    impacc = self.tile([128, 4, 64], F32, name="impacc")
    impt = self.tile([128, 4, 64], F32, name="impt")
    rI = self.tile([128, 4], F32, name="rI")
    m8 = self.tile([128, 16], F32, name="m8")
    wk = self.tile([128, 64], F32, name="wk")
    selb = self.tile([128, 4, 64], BF16, name="selb")
    selT = self.tile([64, 512], BF16, name="selT")
    self.memset("pool", gk[0].ap[:, 255:256], 0.0, [gk[0].b])
    self.memset("pool", gk[1].ap[:, 255:256], 0.0, [gk[1].b])
    self.memset("pool", Vc.ap[:, :, 64:128], 1.0, [Vc.b])
    self.p_rr = 0
    rr = 0
    for s in range(_nseq(self)):
        self.dma(CMP.ap, self.FS[s][F_CMP], CMP.b, [self.bFS[s]])
        for kv in range(2):
            ps = self.next_ps(6, 8)
            for i in range(32):
                rhs = self.sbv(CMP.ap[kv * 64:kv * 64 + 64, :], i, [64, [16, 255]])
                self.mm(ps.ap[:, 0:255], w1.ap[kv * 64:kv * 64 + 64, i, :], rhs, i == 0, i == 31, [w1.b, CMP.b], [ps.b])
            self.act(gk[kv].ap[:, 0:255], ps.ap[:, 0:255], AF.Gelu_apprx_tanh, [ps.b, posc.b], [gk[kv].b], bias=posc.ap[:, kv:kv + 1])
        ps = self.next_ps(6, 8)
        self.mm(ps.ap[:, 0:256], w2.ap[:, 0, :], gk[0].ap, True, True, [w2.b, gk[0].b], [ps.b])
        self.cp("dve", kcT.ap, ps.ap[:, 0:256], [ps.b], [kcT.b])
        for ct in range(2):
            ps = self.next_ps(6, 8)
            self.mm(ps.ap[:, 0:64], gk[1].ap[:, ct * 128:(ct + 1) * 128], w2.ap[:, 1, 0:64], True, True, [w2.b, gk[1].b], [ps.b])
            self.cp("dve", Vc.ap[:, ct, 0:64], ps.ap[:, 0:64], [ps.b], [Vc.b])
        self.dma(QA.ap, self.FS[s][F_QA:F_QA + 2, :, :].rearrange("k p t -> p k t"), QA.b, [self.bFS[s]])
        self.dma(KS.ap, self.FS[s][F_KSLC], KS.b, [self.bFS[s]])
        self.dma(KW.ap, self.FS[s][F_KWIN], KW.b, [self.bFS[s]])
        _load_v(self, V2, s, V_SLC, 2)
        for c in range(_nqc(self)):
            tok = slice(c * 512, (c + 1) * 512)
            GAc, fk, fa = GAr[c % 2], fkr[c % 2], far[c % 2]
            ost = osts[c % 2]
            self.dma(GAc.ap, self.FS[s][F_GA:F_GA + 6, :, tok].rearrange("k p t -> p k t"), GAc.b, [self.bFS[s]])
            self.dma(fk.ap, self.c["c_fkeep"][:, c * 256:(c + 1) * 256].rearrange("p (q j) -> p q j", q=4), fk.b)
            self.dma(fa.ap, self.c["c_fadd"][:, c * 256:(c + 1) * 256].rearrange("p (q j) -> p q j", q=4), fa.b)
            rb_all = [QA.b, KS.b, KW.b, V2.b, Hs.b, Hw.b, kcT.b, Vc.b, cmask.b, eall.b, selT.b, self.anti.b, self.ident.b]

            def gated(psO, h, br, first, last):
                hb, hp = 64 * (h % 2), h // 2
                rc, tmp = rcs[h % 2], tmps[h % 2]
                r = _recip_den(self, psO, rc, 512)
                t = tmp.ap[hb:hb + 64, :]
                g_ap = GAc.ap[hb:hb + 64, hp * 3 + br, :]
                o_ap = Oacc.ap[hb:hb + 64, hp, :]
                self.tt("dve", t, psO.ap[0:64, :], r, ALU.mult, [psO.b, rc.b], [tmp.b])
                if first:
                    self.tt("dve", o_ap, t, g_ap, ALU.mult, [tmp.b, GAc.b], [Oacc.b])
                else:
                    self.tt("dve", t, t, g_ap, ALU.mult, [tmp.b, GAc.b], [tmp.b])
                    if last:
                        self.tt("dve", ost.ap[hb:hb + 64, hp, :], o_ap, t, ALU.add, [Oacc.b, tmp.b], [ost.b])
                    else:
                        self.tt("dve", o_ap, o_ap, t, ALU.add, [Oacc.b, tmp.b], [Oacc.b])

            for h in range(4):
                hb, hp = 64 * (h % 2), h // 2
                kt = []
                cts = []
                for ct in range(2):
                    key = CMPKEY[(ct, c)]
                    if key == "none":
                        continue
                    extras = [] if key == "all" else [(self.ident.ap, cmask.ap[:, key, :])]
                    kt.append((kcT.ap[hb:hb + 64, ct * 128:(ct + 1) * 128], extras, Vc.ap[:, ct, :], 0))
                    cts.append(ct)

                def fin_c(psO, pts, h=h, cts=cts):
                    psI = self.next_ps(6, 8)
                    pv_ = psI.ap.rearrange("p (q c) -> p q c", q=4)
                    for qt in range(4):
                        for ii, (ct, pt) in enumerate(zip(cts, pts)):
                            self.mm(pv_[:, qt, 0:65], pt.ap[:, qt * 128:(qt + 1) * 128], ovx.ap[:, ct, 0:65], ii == 0, ii == len(cts) - 1, [pt.b, ovx.b], [psI.b])
                    self.ts("dve", rI.ap, pv_[:, :, 64], TINY, ALU.max, [psI.b], [rI.b])
                    self.recip(rI.ap, rI.ap, [rI.b], [rI.b])
                    rbc = self.sbv(rI.ap, 0, [128, [1, 4], [0, 64]])
                    if h == 0:
                        self.tt("dve", impacc.ap, pv_[:, :, 0:64], rbc, ALU.mult, [psI.b, rI.b], [impacc.b])
                    else:
                        self.tt("dve", impt.ap, pv_[:, :, 0:64], rbc, ALU.mult, [psI.b, rI.b], [impt.b])
                        self.tt("pool", impacc.ap, impacc.ap, impt.ap, ALU.add, [impacc.b, impt.b], [impacc.b])
                    gated(psO, h, 0, True, False)
                _attn_job(self, QA.ap[hb:hb + 64, hp, tok], 512, kt, fin_c, rb_all, pring)
            self.tt("dve", impacc.ap, impacc.ap, fk.ap, ALU.mult, [impacc.b, fk.b], [impacc.b])
            self.tt("dve", impacc.ap, impacc.ap, fa.ap, ALU.add, [impacc.b, fa.b], [impacc.b])
            for qt in range(4):
                iv = impacc.ap[:, qt, :]
                self.P.op("dve", lambda e, iv=iv: e.max(out=m8.ap[:, 0:8], in_=iv), [impacc.b], [m8.b])
                self.P.op("dve", lambda e, iv=iv: e.match_replace(out=wk.ap, in_to_replace=m8.ap[:, 0:8], in_values=iv, imm_value=-3e9), [impacc.b, m8.b], [wk.b])
                self.P.op("dve", lambda e: e.max(out=m8.ap[:, 8:16], in_=wk.ap), [wk.b], [m8.b])
                self.ts("dve", wk.ap, iv, m8.ap[:, 15:16], ALU.is_ge, [impacc.b, m8.b], [wk.b], s2=-NEG, op1=ALU.mult)
                self.ts("dve", selb.ap[:, qt, :], wk.ap, NEG, ALU.add, [wk.b], [selb.b])
            ps = self.next_ps(6, 8)
            psb = ps.ap.bitcast(BF16)
            for qt in range(4):
                self.tr(psb[0:64, qt * 128:(qt + 1) * 128], selb.ap[:, qt, :], self.ident.ap, [selb.b, self.ident.b], [ps.b])
            self.cp("dve", selT.ap, psb[0:64, 0:512], [ps.b], [selT.b])
            for h in range(4):
                hb, hp = 64 * (h % 2), h // 2
                kt = []
                for j in range(4 * c + 4):
                    dlt = 512 * c - 128 * j
                    c0 = max(0, -dlt)
                    x0 = min(dlt, 1664) + 384
                    extras = [(self.anti.ap, Hs.ap[:, h, x0 + c0:x0 + 512])]
                    if c >= 2:
                        extras.append((eall.ap[:, j * 128:(j + 1) * 128], selT.ap[:, c0:512]))
                    kt.append((KS.ap[hb:hb + 64, j * 128:(j + 1) * 128], extras, V2.ap[:, j, 0, :], c0))
                _attn_job(self, QA.ap[hb:hb + 64, hp, tok], 512, kt, lambda psO, pts, h=h: gated(psO, h, 1, False, False), rb_all, pring)
                kt = []
                for j in range(max(0, 4 * c - 4), 4 * c + 4):
                    dlt = 512 * c - 128 * j
                    c0 = max(0, -dlt)
                    x0 = dlt + 384
                    kt.append((KW.ap[hb:hb + 64, j * 128:(j + 1) * 128], [(self.anti.ap, Hw.ap[:, h, x0 + c0:x0 + 512])], V2.ap[:, j, 1, :], c0))
                _attn_job(self, QA.ap[hb:hb + 64, hp, tok], 512, kt, lambda psO, pts, h=h: gated(psO, h, 2, False, True), rb_all, pring)
            self.dma(self.OS[s][0:2, :, tok].rearrange("k p t -> p k t"), ost.ap, self.bOS[s], [ost.b])
    self.release(m0, [Hs, Hw, CMP, QA, KS, KW, V2] + GAr + fkr + far)
    P.barrier()


Builder.phase_BD = lambda self, l: _phase_causal(self, l, "D")
Builder.phase_BC = lambda self, l: _phase_causal(self, l, "C")
Builder.phase_BB = _phase_BB
Builder.phase_BA = _phase_BA


def build_nc(phases=None, dbg=False, os_input=False):
    nc = bass.Bass("TRN2", target_bir_lowering=False)
    b = Builder(nc, phases, dbg)
    b.os_input = os_input
    b.build()
    return nc, b


def make_in_maps(inputs):
    x = np.ascontiguousarray(np.asarray(inputs["x"], dtype=np.float32))
    common = {n: np.ascontiguousarray(np.asarray(inputs[n], dtype=np.float32)) for n in W_NAMES}
    common.update(CONSTS)
    maps = []
    for c in range(8):
        m = dict(common)
        m["x"] = x[2 * c:2 * c + 2]
        maps.append(m)
    return maps


def kernel(**inputs):
    nc, _ = build_nc()
    res = run_bass_kernel_spmd(nc, make_in_maps(inputs), core_ids=list(range(8)))
    return np.concatenate([r["y"] for r in res.results], axis=0).astype(np.float32)
```

```python
import math
import numpy as np
import concourse.bass as bass
import concourse.mybir as mybir
from concourse.bass_utils import run_bass_kernel_spmd

F32 = mybir.dt.float32
BF16 = mybir.dt.bfloat16
AF = mybir.ActivationFunctionType
ALU = mybir.AluOpType

S = 4096
D = 1024
NB = 2
DEPTH = 2
DFF = 2816
D_IN = 2992
EPS = 1e-6
NEG = -30000.0
TINY = 1e-30
NTC = 8
ENGS = ("pe", "act", "dve", "pool", "sp")


class Sem:
    def __init__(self, handle):
        self.h = handle
        self.count = 0
        self.last_op = None


class Buf:
    def __init__(self, name, dma_sems=None, disjoint=False):
        self.name = name
        self.writers = {}
        self.readers = {}
        self.dma_sems = dma_sems or []
        self.rr = 0
        self.disjoint = disjoint


class Op:
    __slots__ = ("eng", "fn", "deps", "needed", "val", "sem", "is_dma")

    def __init__(self, eng, fn):
        self.eng = eng
        self.fn = fn
        self.deps = []
        self.needed = False
        self.val = None
        self.sem = None
        self.is_dma = False


class Prog:
    def __init__(self, nc):
        self.nc = nc
        self.streams = {e: [] for e in ENGS}
        self.free_sems = []
        self.all_sems = []
        self.eng_sems = {e: Sem(nc.alloc_semaphore(f"eng_{e}")) for e in ENGS if e != "sp"}

    def sem_alloc(self):
        if self.free_sems:
            return self.free_sems.pop()
        s = Sem(self.nc.alloc_semaphore(f"ks{len(self.all_sems)}"))
        self.all_sems.append(s)
        return s

    def buf(self, name, ndma=0, disjoint=False):
        return Buf(name, [self.sem_alloc() for _ in range(ndma)], disjoint)

    def free_buf(self, b):
        self.free_sems.extend(b.dma_sems)
        b.dma_sems = []

    def _track(self, op, reads, writes):
        deps = []
        for b in reads:
            deps.extend(b.writers.values())
        for b in writes:
            if not b.disjoint:
                deps.extend(b.writers.values())
            deps.extend(b.readers.values())
        key = id(op.sem) if op.is_dma else op.eng
        for b in reads:
            b.readers[key] = op
        for b in writes:
            if b.disjoint:
                b.writers[key] = op
            else:
                b.writers = {key: op}
                b.readers = {}
        seen = set()
        for d in deps:
            if d is op or id(d) in seen:
                continue
            seen.add(id(d))
            if op.eng == "pe" and d.eng == "pe" and not d.is_dma and not op.is_dma:
                continue
            op.deps.append(d)
            d.needed = True

    def op(self, eng, fn, reads=(), writes=()):
        o = Op(eng, fn)
        self._track(o, reads, writes)
        self.streams[eng].append(o)
        return o

    def dma(self, fns, dst, reads=(), queue="sp", writes=()):
        if not isinstance(fns, (list, tuple)):
            fns = [fns]
        o = Op(queue, list(fns))
        o.is_dma = True
        s = dst.dma_sems[dst.rr % len(dst.dma_sems)]
        dst.rr += 1
        o.sem = s
        if s.last_op is not None:
            o.deps.append(s.last_op)
        s.count += 16 * len(fns)
        o.val = s.count
        s.last_op = o
        o.needed = True
        self._track(o, reads, [dst] + list(writes))
        self.streams[queue].append(o)
        return o

    def barrier(self):
        lasts = []
        for e in ENGS:
            for o in reversed(self.streams[e]):
                if not o.is_dma and o.fn is not None:
                    lasts.append(o)
                    break
        dmas = [s.last_op for s in self.all_sems if s.last_op is not None]
        for e in ENGS:
            o = Op(e, None)
            for d in lasts + dmas:
                o.deps.append(d)
                d.needed = True
            self.streams[e].append(o)

    def finalize_vals(self):
        for e in ENGS:
            if e == "sp":
                continue
            s = self.eng_sems[e]
            c = 0
            for o in self.streams[e]:
                if o.is_dma or o.fn is None:
                    continue
                if o.needed:
                    c += 1
                    o.val = c
                    o.sem = s

    def emit_stream(self, eng_name, eng):
        seen = {}
        for o in self.streams[eng_name]:
            waits = {}
            for d in o.deps:
                sid = id(d.sem)
                if sid not in waits or waits[sid][1] < d.val:
                    waits[sid] = (d.sem, d.val)
            for sid, (s, v) in waits.items():
                if seen.get(sid, 0) >= v:
                    continue
                seen[sid] = v
                eng.wait_ge(s.h, v)
            if o.fn is None:
                continue
            if o.is_dma:
                for f in o.fn:
                    f(eng).then_inc(o.sem.h, 16)
            else:
                ins = o.fn(eng)
                if o.needed:
                    ins.then_inc(o.sem.h, 1)

    def run_block(self, final_ops=()):
        self.finalize_vals()
        fw = {}
        for o in final_ops:
            if id(o.sem) not in fw or fw[id(o.sem)][1] < o.val:
                fw[id(o.sem)] = (o.sem, o.val)
        with self.nc.Block() as block:
            @block.tensor
            def _(e):
                self.emit_stream("pe", e)

            @block.scalar
            def _(e):
                self.emit_stream("act", e)

            @block.vector
            def _(e):
                self.emit_stream("dve", e)

            @block.gpsimd
            def _(e):
                self.emit_stream("pool", e)

            @block.sync
            def _(e):
                self.emit_stream("sp", e)
                for s, v in fw.values():
                    e.wait_ge(s.h, v)


class Tile:
    def __init__(self, ap, b):
        self.ap = ap
        self.b = b


DT_SIZE = {F32: 4, BF16: 2}


def _bucket(d):
    d = np.maximum(d, 0)
    df = np.maximum(d, 1).astype(np.float32)
    large = 16 + (np.log(df / np.float32(16)) / np.float32(math.log(2048 / 16)) * np.float32(16)).astype(np.int32)
    large = np.minimum(large, 31)
    return np.where(d < 16, d, large)


LA, LW, LB_, LC = 2688, 1536, 1152, 1024
OFF = 384


def _onehot(valid, bidx, L):
    oh = np.zeros((33, L), np.float32)
    idx = np.where(valid, bidx, 32)
    oh[idx, np.arange(L)] = 1.0
    return oh


def make_consts():
    c = {}
    c["c_ident"] = np.eye(128, dtype=np.float32)
    c["c_anti"] = np.eye(128, dtype=np.float32)[::-1].copy()
    i = np.arange(LA) - 511
    c["c_oh_a"] = _onehot(i >= 0, _bucket(i), LA)
    i = np.arange(LW) - 511
    c["c_oh_w"] = _onehot((i >= 0) & (i < 512), _bucket(i), LW)
    i = np.arange(LB_) - 511
    c["c_oh_b"] = np.stack([_onehot((i >= 0) & (i <= 128), _bucket(i * dil), LB_) for dil in (1, 4, 16)])
    i = np.arange(LC) - 511
    c["c_causal"] = np.where(i >= 0, 0.0, NEG).astype(np.float32)[None, :]
    cm = []
    key = {}
    for ct in range(2):
        for qc in range(8):
            cc = ct * 128 + np.arange(128)[:, None]
            t = qc * 512 + np.arange(512)[None, :]
            valid = (cc <= 254) & (16 * cc + 31 <= t)
            if valid.all() or not valid.any():
                key[(ct, qc)] = "all" if valid.all() else "none"
                continue
            key[(ct, qc)] = len(cm)
            cm.append(np.where(valid, 0.0, NEG).astype(np.float32))
    c["c_cmpmask"] = np.stack(cm)
    c["c_eall"] = (np.arange(S)[None, :] // 64 == np.arange(64)[:, None]).astype(np.float32)
    t = np.arange(S)
    cur = (t // 64)[:, None]
    j = np.arange(64)[None, :]
    add = np.zeros((S, 64), np.float32)
    keep = np.ones((S, 64), np.float32)
    fut = j > cur
    add[fut] = -1e9
    keep[fut] = 0
    for cond, val in ((j == cur - 1, 1e9), (j == cur, 2e9), (j == 0, 3e9)):
        cond = np.broadcast_to(cond, (S, 64))
        add[cond] = val
        keep[cond] = 0
    c["c_fkeep"] = keep.reshape(32, 128, 64).transpose(1, 0, 2).reshape(128, 32 * 64).copy()
    c["c_fadd"] = add.reshape(32, 128, 64).transpose(1, 0, 2).reshape(128, 32 * 64).copy()
    cs = np.arange(256) * 16
    ce = cs + 31
    ss = np.arange(64) * 64
    ov = ((cs[:, None] <= ss[None, :] + 63) & (ce[:, None] >= ss[None, :])).astype(np.float32)
    ov[255] = 0
    ovx = np.zeros((256, 128), np.float32)
    ovx[:, :64] = ov
    ovx[:, 64] = 1.0
    c["c_ovx"] = ovx.reshape(2, 128, 128)
    inv = (10000.0 ** (-np.arange(0, 32, 2, dtype=np.float32) / 32)).astype(np.float32)
    ang = np.arange(S, dtype=np.float32)[None, :] * inv[:, None]
    cos, sin = np.cos(ang).astype(np.float32), np.sin(ang).astype(np.float32)
    sc = np.float32(96 ** -0.5)
    rq = np.zeros((4, 96, S), np.float32)
    rq[0, 0:64] = sc
    rq[0, 64:80] = sc * cos
    rq[0, 80:96] = sc * cos
    rq[1, 64:80] = sc * sin
    rq[1, 80:96] = sc * sin
    rq[2, 64:80] = cos
    rq[2, 80:96] = cos
    rq[3, 64:80] = sin
    rq[3, 80:96] = sin
    c["c_rope"] = rq
    return c, key


CONSTS, CMPKEY = make_consts()

W_NAMES = ["rel_bias_table", "norm_attn_pre", "norm_attn_post", "norm_ffn_pre", "norm_ffn_post", "w_in", "nsa_cmp_pos",
           "nsa_phi_k_w1", "nsa_phi_k_w2", "nsa_phi_v_w1", "nsa_phi_v_w2", "fox_forget_bias", "mla_q_norm",
           "mla_kv_norm", "mla_w_uq", "mla_w_ukv", "w_branch_a", "w_branch_b", "w_branch_c", "w_branch_d",
           "w_merge_gate", "w_o", "ffn_w_up", "ffn_conv_w", "ffn_conv_b", "ffn_w_down"]
W_SHAPES = {
    "rel_bias_table": (32, 10), "norm_attn_pre": (2, 1024), "norm_attn_post": (2, 1024), "norm_ffn_pre": (2, 1024),
    "norm_ffn_post": (2, 1024), "w_in": (2, 1024, 2992), "nsa_cmp_pos": (2, 32, 64), "nsa_phi_k_w1": (2, 2048, 128),
    "nsa_phi_k_w2": (2, 128, 64), "nsa_phi_v_w1": (2, 2048, 128), "nsa_phi_v_w2": (2, 128, 64),
    "fox_forget_bias": (2, 4), "mla_q_norm": (2, 256), "mla_kv_norm": (2, 128), "mla_w_uq": (2, 256, 384),
    "mla_w_ukv": (2, 128, 512), "w_branch_a": (2, 256, 1024), "w_branch_b": (2, 128, 1024),
    "w_branch_c": (2, 256, 1024), "w_branch_d": (2, 256, 1024), "w_merge_gate": (2, 4, 1024, 1024),
    "w_o": (2, 1024, 1024), "ffn_w_up": (2, 1024, 5632), "ffn_conv_w": (2, 3, 5632), "ffn_conv_b": (2, 5632),
    "ffn_w_down": (2, 2816, 1024),
}

F_QA, F_CMP, F_KSLC, F_KWIN, F_GA, F_QB, F_KB, F_QC, F_KC, F_QD, F_KD, NF = 0, 2, 3, 4, 5, 11, 14, 17, 19, 21, 25, 29
V_SLC, V_WIN, V_B, V_C, V_D, NV = 0, 64, 128, 512, 768, 1024


def dram_ap(t, offset, dims):
    return bass.AP(t.tensor, t.offset + offset, [list(d) for d in dims])


class Builder:
    def __init__(self, nc, phases=None, dbg=False):
        self.nc = nc
        self.P = Prog(nc)
        self.phases = phases
        self.dbg = dbg
        self.uid = 0
        probe = nc.alloc_sbuf_tensor("sb_probe", [128, 8], F32)
        base = nc.lookup_mloc(probe).addr
        self.sb_base = (base + 32 + 63) // 64 * 64
        self.sb_limit = base + 32 + nc.sbuf_bytes_remaining - 64
        self.sb_top = self.sb_base
        self.final_ops = []
        self.ps = []
        for i in range(8):
            t = nc.alloc_psum_tensor(f"psb{i}", [128, 512], F32)
            self.ps.append(Tile(t.ap(), self.P.buf(f"ps{i}")))
        self.ps_rr = 0

    def tile(self, shape, dt, ndma=0, name="t", disjoint=False):
        free = 1
        for s_ in shape[1:]:
            free *= s_
        nbytes = (free * DT_SIZE[dt] + 63) // 64 * 64
        assert self.sb_top + nbytes <= self.sb_limit, f"SBUF overflow allocating {name} {shape}: top={self.sb_top - self.sb_base} need={nbytes}"
        self.uid += 1
        t = self.nc.alloc_sbuf_tensor_at(f"{name}_{self.uid}", list(shape), dt, offset=self.sb_top)
        self.sb_top += nbytes
        return Tile(t.ap(), self.P.buf(f"{name}_{self.uid}", ndma, disjoint))

    def mark(self):
        return (self.sb_top, list(self.P.free_sems), len(self.P.all_sems))

    def release(self, mark, tiles=()):
        for t in tiles:
            self.P.free_buf(t.b)
        self.sb_top = mark[0]

    def next_ps(self, lo=0, hi=8):
        n = hi - lo
        i = lo + (self.ps_rr % n)
        self.ps_rr += 1
        return self.ps[i]

    def dram(self, name, shape, dt, kind="Internal"):
        if self.dbg and kind == "Internal" and name in ("OS0", "HT0", "X10", "X20", "GEXTA"):
            kind = "ExternalOutput"
        return self.nc.dram_tensor(name, list(shape), dt, kind=kind).ap()

    def mm(self, ps, lhsT, rhs, start, stop, reads, writes):
        return self.P.op("pe", lambda e: e.matmul(ps, lhsT=lhsT, rhs=rhs, start=start, stop=stop), reads, writes)

    def tr(self, ps, in_, ident, reads, writes):
        return self.P.op("pe", lambda e: e.transpose(out=ps, in_=in_, identity=ident), reads, writes)

    def act(self, out, in_, func, reads, writes, bias=None, scale=None, accum=None):
        kw = {}
        if bias is not None:
            kw["bias"] = bias
        if scale is not None:
            kw["scale"] = scale
        if accum is not None:
            kw["accum_out"] = accum
        return self.P.op("act", lambda e: e.activation(out=out, in_=in_, func=func, **kw), reads, writes)

    def tt(self, eng, out, in0, in1, op, reads, writes):
        return self.P.op(eng, lambda e: e.tensor_tensor(out=out, in0=in0, in1=in1, op=op), reads, writes)

    def ts(self, eng, out, in0, s1, op0, reads, writes, s2=None, op1=None):
        if op1 is None:
            return self.P.op(eng, lambda e: e.tensor_scalar(out=out, in0=in0, scalar1=s1, scalar2=None, op0=op0), reads, writes)
        return self.P.op(eng, lambda e: e.tensor_scalar(out=out, in0=in0, scalar1=s1, scalar2=s2, op0=op0, op1=op1), reads, writes)

    def stt(self, eng, out, in0, scalar, in1, op0, op1, reads, writes):
        return self.P.op(eng, lambda e: e.scalar_tensor_tensor(out=out, in0=in0, scalar=scalar, in1=in1, op0=op0, op1=op1), reads, writes)

    def cp(self, eng, out, in_, reads, writes):
        if eng == "act":
            return self.P.op("act", lambda e: e.copy(out=out, in_=in_), reads, writes)
        return self.P.op(eng, lambda e: e.tensor_copy(out=out, in_=in_), reads, writes)

    def memset(self, eng, ap, val, writes):
        return self.P.op(eng, lambda e: e.memset(ap, val), [], writes)

    def recip(self, out, in_, reads, writes):
        return self.P.op("dve", lambda e: e.reciprocal(out=out, in_=in_), reads, writes)

    def dma(self, out, in_, dst, reads=(), queue="sp", writes=()):
        return self.P.dma(lambda e: e.dma_start(out=out, in_=in_), dst, reads, queue, writes)

    def rstd_from_ss(self, rs, ss, n, reads_writes):
        self.ts("dve", rs, ss, 1.0 / n, ALU.mult, reads_writes, reads_writes, s2=EPS, op1=ALU.add)
        self.act(rs, rs, AF.Sqrt, reads_writes, reads_writes)
        self.recip(rs, rs, reads_writes, reads_writes)

    def wload(self, dst_ap, src_ap, dst_buf, shape, post=None):
        st = self.wstage[self.wstage_rr % len(self.wstage)]
        self.wstage_rr += 1
        free = 1
        for s_ in shape[1:]:
            free *= s_
        assert free <= 1024
        view = st.ap[0:shape[0], 0:free]
        if len(shape) == 3:
            view = view.rearrange("p (a b) -> p a b", a=shape[1])
        self.dma(view, src_ap, st.b)
        if post is None:
            self.cp("pool" if dst_ap.base_partition() == 0 else "dve", dst_ap, view, [st.b], [dst_buf])
        else:
            post(view, st.b)

    def build(self):
        nc, P = self.nc, self.P
        self.x_in = nc.dram_tensor("x", [NB, S, D], F32, kind="ExternalInput").ap()
        self.y_out = nc.dram_tensor("y", [NB, S, D], F32, kind="ExternalOutput").ap()
        self.w = {n: nc.dram_tensor(n, list(W_SHAPES[n]), F32, kind="ExternalInput").ap() for n in W_NAMES}
        self.c = {n: nc.dram_tensor(n, list(v.shape), F32, kind="ExternalInput").ap() for n, v in CONSTS.items()}
        self.FS = [self.dram(f"FS{s}", [NF, 128, S], BF16) for s in range(NB)]
        self.FF = [self.dram(f"FF{s}", [4, S], F32) for s in range(NB)]
        self.VS = [self.dram(f"VS{s}", [S, NV], BF16) for s in range(NB)]
        self.HT = [self.dram(f"HT{s}", [8, 128, S], BF16) for s in range(NB)]
        self.OS = [(self.nc.dram_tensor(f"OS{s}", [7, 128, S], BF16, kind="ExternalInput").ap() if getattr(self, "os_input", False) else self.dram(f"OS{s}", [7, 128, S], BF16)) for s in range(NB)]
        self.X1 = [self.dram(f"X1{s}", [S, D], F32) for s in range(NB)]
        self.X2 = [self.dram(f"X2{s}", [S, D], F32) for s in range(NB)]
        self.AUG = [self.dram(f"AUG{s}", [2, 4, 6, S], BF16) for s in range(NB)]
        self.GA_ = self.dram("GEXTA", [4, LA], BF16)
        self.GW_ = self.dram("GEXTW", [4, LW], BF16)
        self.GB_ = self.dram("GEXTB", [6, LB_], BF16)
        self.GC_ = self.dram("GEXTC", [1, LC], BF16)
        nd = 4
        self.bFS = [P.buf(f"FS{s}", nd, True) for s in range(NB)]
        self.bFF = [P.buf(f"FF{s}", 1, True) for s in range(NB)]
        self.bVS = [P.buf(f"VS{s}", nd, True) for s in range(NB)]
        self.bHT = [P.buf(f"HT{s}", nd, True) for s in range(NB)]
        self.bOS = [P.buf(f"OS{s}", nd, True) for s in range(NB)]
        self.bX1 = [P.buf(f"X1{s}", nd, True) for s in range(NB)]
        self.bX2 = [P.buf(f"X2{s}", nd, True) for s in range(NB)]
        self.bAUG = [P.buf(f"AUG{s}", 1, True) for s in range(NB)]
        self.bG = P.buf("GEXT", 1, True)
        self.bY = P.buf("Y", nd, True)

        self.ident = self.tile([128, 128], BF16, name="ident")
        self.anti = self.tile([128, 128], BF16, name="anti")
        self.ones = self.tile([128, 128], BF16, name="ones")
        self.wstage = [self.tile([128, 1024], F32, 1, name="wst") for _ in range(2)]
        self.wstage_rr = 0
        self.wload(self.ident.ap, self.c["c_ident"], self.ident.b, [128, 128])
        self.wload(self.anti.ap, self.c["c_anti"], self.anti.b, [128, 128])
        self.memset("pool", self.ones.ap, 1.0, [self.ones.b])
        self.prologue_tables()
        P.barrier()
        for l in range(DEPTH):
            if self.want("A"):
                self.phase_A(l)
            if self.want("BC"):
                self.phase_BC(l)
            if self.want("BD"):
                self.phase_BD(l)
            if self.want("BB"):
                self.phase_BB(l)
            if self.want("BA"):
                self.phase_BA(l)
            if self.want("C1"):
                self.phase_C1(l)
            if self.want("C2"):
                self.phase_C2(l)
            if self.phases is not None and self.phases.get("layers", DEPTH) <= l + 1:
                break
        P.run_block(self.final_ops)

    def limit(self, chunks, key="nchunks"):
        if self.phases is not None and key in self.phases:
            return chunks[:self.phases[key]]
        return chunks

    def want(self, ph):
        return self.phases is None or ph in self.phases.get("run", ())

    def prologue_tables(self):
        m = self.mark()
        tabf = self.tile([33, 10], F32, 1, name="tabf")
        tab = self.tile([33, 10], BF16, name="tab")
        self.memset("pool", tabf.ap, NEG, [tabf.b])
        self.dma(tabf.ap[0:32, :], self.w["rel_bias_table"], tabf.b)
        self.cp("pool", tab.ap, tabf.ap, [tabf.b], [tab.b])
        jobs = [(self.c["c_oh_a"], LA, 0, 4, self.GA_), (self.c["c_oh_w"], LW, 0, 4, self.GW_)]
        for g in range(3):
            jobs.append((self.c["c_oh_b"][g], LB_, 4 + 2 * g, 2, self.GB_[2 * g:2 * g + 2, :]))
        ohf = self.tile([33, LA], F32, 1, name="ohf")
        oh = self.tile([33, LA], BF16, name="oh")
        gs = self.tile([4, LA], BF16, name="gs")
        for (src, L, h0, nh, dst) in jobs:
            self.dma(ohf.ap[:, 0:L], src, ohf.b)
            self.cp("pool", oh.ap[:, 0:L], ohf.ap[:, 0:L], [ohf.b], [oh.b])
            for c0 in range(0, L, 512):
                n = min(512, L - c0)
                ps = self.next_ps()
                self.mm(ps.ap[0:nh, 0:n], tab.ap[:, h0:h0 + nh], oh.ap[:, c0:c0 + n], True, True, [tab.b, oh.b], [ps.b])
                self.cp("dve", gs.ap[0:nh, c0:c0 + n], ps.ap[0:nh, 0:n], [ps.b], [gs.b])
            self.dma(dst, gs.ap[0:nh, 0:L], self.bG, [gs.b])
        cz = self.tile([1, LC], F32, 1, name="cz")
        czb = self.tile([1, LC], BF16, name="czb")
        self.dma(cz.ap, self.c["c_causal"], cz.b)
        self.cp("dve", czb.ap, cz.ap, [cz.b], [czb.b])
        self.dma(self.GC_, czb.ap, self.bG, [czb.b])
        self.release(m, [tabf, ohf, cz])

    def hankel(self, tile_ap, buf, src, row, W):
        in_ = bass.AP(src.tensor, src.offset + row * src.ap[0][0], [[1, 128], [1, W]])
        self.dma(tile_ap, in_, buf, [self.bG])

    def sbv(self, t_ap, off, dims):
        return bass.AP(t_ap.tensor, t_ap.offset + off, [[t_ap.ap[0][0], dims[0]]] + [list(d) for d in dims[1:]])

    def load_pvec(self, dst_ap, dst_buf, src_rows_ap, nrows, identf=None):
        fns = []
        for r in range(nrows):
            in_ = bass.AP(src_rows_ap.tensor, src_rows_ap.offset + r * 128, [[1, 128], [1, 1]])
            fns.append(lambda e, r=r, in_=in_: e.dma_start(out=dst_ap[:, r:r + 1], in_=in_))
        self.P.dma(fns, dst_buf)

    def phase_A(self, l):
        P = self.P
        m0 = self.mark()
        w_in = self.w["w_in"][l]
        NCOLS = 4104
        O_QA, O_CMP, O_KSLC, O_KWIN, O_GA, O_QB, O_KB, O_QC, O_KC, O_QL, O_KVL, O_KR, O_KRP, O_FL, O_T1, O_T2 = (
            0, 256, 384, 512, 640, 1408, 1792, 2176, 2432, 2688, 2944, 3072, 3200, 3328, 3336, 3848)
        Win = self.tile([128, 8, NCOLS], BF16, name="Win")

        def load_in(dst_off, src_col, n, post=None):
            c0 = 0
            while c0 < n:
                k = min(128, n - c0)
                src = w_in[:, src_col + c0: src_col + c0 + k].rearrange("(k p) n -> p k n", p=128)
                self.wload(Win.ap[:, :, dst_off + c0: dst_off + c0 + k], src, Win.b, [128, 8, k], post=post)
                c0 += k

        load_in(O_QA, 0, 384)
        for o_, c_ in ((O_KSLC, 384), (O_KWIN, 512)):
            load_in(o_, c_, 64)
            load_in(o_ + 64, c_, 64)

        def post_gates(view, sbuf):
            for h in range(4):
                for b in range(3):
                    gc = h * 3 + b
                    off = O_GA + ((h // 2) * 3 + b) * 128 + (h % 2) * 64
                    src = self.sbv(view, gc, [128, [12, 8], [0, 64]])
                    self.cp("pool", Win.ap[:, :, off:off + 64], src, [sbuf], [Win.b])
        load_in(0, 640, 12, post=post_gates)
        load_in(O_QB, 652, 384)
        load_in(O_KB, 1036, 384)
        load_in(O_QC, 1804, 256)
        load_in(O_KC, 2060, 256)
        load_in(O_QL, 2576, 256)
        load_in(O_KVL, 2832, 128)
        self.memset("pool", Win.ap[:, :, O_KR:O_KR + 256], 0.0, [Win.b])

        def post_kr(view, sbuf):
            self.cp("pool", Win.ap[:, :, O_KR + 64:O_KR + 96], view, [sbuf], [Win.b])
            self.ts("pool", Win.ap[:, :, O_KRP + 64:O_KRP + 80], view[:, :, 16:32], -1.0, ALU.mult, [sbuf], [Win.b])
            self.cp("pool", Win.ap[:, :, O_KRP + 80:O_KRP + 96], view[:, :, 0:16], [sbuf], [Win.b])
        load_in(0, 2960, 32, post=post_kr)
        load_in(O_FL, 2572, 4)
        load_in(O_T1, 448, 64)
        load_in(O_T1 + 64, 576, 64)
        load_in(O_T1 + 128, 1420, 384)
        load_in(O_T2, 2316, 256)

        gq = self.tile([128, 2], F32, 1, name="gq")
        gkv = self.tile([128, 1], F32, 1, name="gkv")
        self.load_pvec(gq.ap, gq.b, self.w["mla_q_norm"][l].rearrange("(k p) -> k p", p=128), 2)
        self.load_pvec(gkv.ap, gkv.b, self.w["mla_kv_norm"][l].rearrange("(k p) -> k p", p=128), 1)
        WQ = self.tile([128, 2, 4, 128], BF16, name="WQ")
        WQP = self.tile([128, 2, 4, 128], BF16, name="WQP")
        WK = self.tile([128, 4, 128], BF16, name="WK")
        WV = self.tile([128, 256], BF16, name="WV")
        self.memset("pool", WQP.ap, 0.0, [WQP.b])
        self.memset("pool", WQ.ap, 0.0, [WQ.b])
        self.memset("pool", WK.ap, 0.0, [WK.b])

        def post_uq(view, sbuf):
            for kc in range(2):
                v4 = view[:, kc, :].rearrange("p (h c) -> p h c", h=4)
                self.ts("pool", WQ.ap[:, kc, :, 0:96], v4, gq.ap[:, kc:kc + 1], ALU.mult, [sbuf, gq.b], [WQ.b])
            for kc in range(2):
                self.ts("pool", WQP.ap[:, kc, :, 64:80], WQ.ap[:, kc, :, 80:96], -1.0, ALU.mult, [WQ.b], [WQP.b])
                self.cp("pool", WQP.ap[:, kc, :, 80:96], WQ.ap[:, kc, :, 64:80], [WQ.b], [WQP.b])
        self.wload(None, self.w["mla_w_uq"][l].rearrange("(k p) n -> p k n", p=128), None, [128, 2, 384], post=post_uq)

        def post_ukv(view, sbuf):
            v4 = view.rearrange("p (h c) -> p h c", h=4)
            self.ts("pool", WK.ap[:, :, 0:64], v4[:, :, 0:64], gkv.ap[:, 0:1], ALU.mult, [sbuf, gkv.b], [WK.b])
            self.ts("pool", WV.ap.rearrange("p (h c) -> p h c", h=4), v4[:, :, 64:128], gkv.ap[:, 0:1], ALU.mult, [sbuf, gkv.b], [WV.b])
        self.wload(None, self.w["mla_w_ukv"][l], None, [128, 512], post=post_ukv)

        gain = self.tile([128, D], F32, 1, name="gain")
        g_src = self.w["norm_attn_pre"][l]
        self.dma(gain.ap, bass.AP(g_src.tensor, g_src.offset, [[0, 128], [1, D]]), gain.b)

        astep = 9 if self.phases is None else self.phases.get("astep", 9)
        xs = [self.tile([128, 4, D], F32, 1, name="xc") for _ in range(2)]
        hTs = [self.tile([128, 8, 512], BF16, name="hT") for _ in range(2)]
        hn = [self.tile([128, D], BF16, name="hn") for _ in range(2)]
        junk = self.tile([128, D], BF16, name="junk")
        stF = [self.tile([128, 8, 512], BF16, name="stF") for _ in range(2)]
        stT = self.tile([128, 4, NV], BF16, name="stT")
        stFF = self.tile([4, 512], F32, name="stFF")
        rope = self.tile([96, 4, 512], F32, 1, name="rope")
        qlT = self.tile([128, 2, 512], BF16, name="qlT")
        sqq = self.tile([128, 2, 512], BF16, name="sqq")
        kvT = self.tile([128, 512], BF16, name="kvT")
        kvf = self.tile([128, 512], F32, name="kvf")
        sqkv = self.tile([128, 512], BF16, name="sqkv")
        rq = self.tile([128, 512], F32, name="rq")
        rkv = self.tile([128, 512], F32, name="rkv")
        t1 = self.tile([128, 512], F32, name="t1")
        t2 = self.tile([128, 512], F32, name="t2")
        kr = self.tile([128, 512], BF16, name="kr")
        stat = self.tile([128, 16], F32, name="stat")
        stF_rr = [0]

        def x_src(s, tc):
            src = self.x_in[s] if l == 0 else self.X2[s]
            return src[tc * 512:(tc + 1) * 512, :].rearrange("(j p) d -> p j d", p=128)

        chunks = self.limit([(s, tc) for s in range(NB) for tc in range(NTC)])
        if astep < 1:
            chunks = []
        if chunks:
            pass
        if chunks:
            self.dma(xs[0].ap, x_src(*chunks[0]), xs[0].b, [] if l == 0 else [self.bX2[chunks[0][0]]])
        evac_rr = [0]

        def evac(out, ps, reads, writes, scale=None, func=None):
            if func is not None:
                return self.act(out, ps, func, reads, writes, scale=scale)
            evac_rr[0] += 1
            if evac_rr[0] % 2:
                if scale is None:
                    return self.cp("act", out, ps, reads, writes)
                return self.act(out, ps, AF.Copy, reads, writes, scale=scale)
            if scale is None:
                return self.cp("dve", out, ps, reads, writes)
            return self.ts("dve", out, ps, scale, ALU.mult, reads, writes)

        for ci, (s, tc) in enumerate(chunks):
            xc = xs[ci % 2]
            hT = hTs[ci % 2]
            if ci + 1 < len(chunks):
                s2, tc2 = chunks[ci + 1]
                self.dma(xs[(ci + 1) % 2].ap, x_src(s2, tc2), xs[(ci + 1) % 2].b, [] if l == 0 else [self.bX2[s2]])
            tok = slice(tc * 512, (tc + 1) * 512)
            self.dma(rope.ap, self.c["c_rope"][:, :, tok].rearrange("a r t -> r a t"), rope.b)
            self.memset("dve", stat.ap[:, 0:4], 0.0, [stat.b])
            for j in range(4):
                hj = hn[j % 2]
                self.act(junk.ap, xc.ap[:, j, :], AF.Square, [xc.b], [junk.b, stat.b], accum=stat.ap[:, j:j + 1])
            self.rstd_from_ss(stat.ap[:, 4:8], stat.ap[:, 0:4], D, [stat.b])
            for j in range(4):
                hj = hn[j % 2]
                self.stt("dve", hj.ap, xc.ap[:, j, :], stat.ap[:, 4 + j:5 + j], gain.ap, ALU.mult, ALU.mult, [xc.b, stat.b, gain.b], [hj.b])
                ps = self.next_ps()
                psb = ps.ap.bitcast(BF16).rearrange("p (k t) -> p k t", k=8)
                for kc in range(8):
                    self.tr(psb[:, kc, :], hj.ap[:, kc * 128:(kc + 1) * 128], self.ident.ap, [hj.b, self.ident.b], [ps.b])
                evac(hT.ap[:, :, j * 128:(j + 1) * 128], psb, [ps.b], [hT.b])
            self.dma(self.HT[s][:, :, tok].rearrange("k p t -> p k t"), hT.ap, self.bHT[s], [hT.b])
            if astep < 2:
                continue

            def fproj(off, M, dst_ap, dst_b, scale=None, func=None):
                ps = self.next_ps()
                for kc in range(8):
                    self.mm(ps.ap[0:M, :], Win.ap[:, kc, off:off + M], hT.ap[:, kc, :], kc == 0, kc == 7, [Win.b, hT.b], [ps.b])
                evac(dst_ap, ps.ap[0:M, :], [ps.b], [dst_b], scale=scale, func=func)
                return ps

            def fgroup(specs, f0):
                st = stF[stF_rr[0] % 2]
                stF_rr[0] += 1
                for i, (off, scale, func) in enumerate(specs):
                    fproj(off, 128, st.ap[:, i, :], st.b, scale, func)
                n = len(specs)
                self.dma(self.FS[s][f0:f0 + n, :, tok].rearrange("n p t -> p n t"), st.ap[:, 0:n, :], self.bFS[s], [st.b])

            fgroup([(O_QA, 0.125, None), (O_QA + 128, 0.125, None), (O_CMP, None, None), (O_KSLC, None, None), (O_KWIN, None, None)], F_QA)
            fgroup([(O_GA + 128 * i, None, AF.Sigmoid) for i in range(6)], F_GA)
            fgroup([(O_QB + 128 * i, 0.125, None) for i in range(3)] + [(O_KB + 128 * i, None, None) for i in range(3)], F_QB)
            fgroup([(O_QC + 128 * i, 0.125, None) for i in range(2)] + [(O_KC + 128 * i, None, None) for i in range(2)], F_QC)
            ps = self.next_ps()
            for kc in range(8):
                self.mm(ps.ap[0:4, :], Win.ap[:, kc, O_FL:O_FL + 4], hT.ap[:, kc, :], kc == 0, kc == 7, [Win.b, hT.b], [ps.b])
            self.cp("dve", stFF.ap, ps.ap[0:4, :], [ps.b], [stFF.b])
            self.dma(self.FF[s][:, tok], stFF.ap, self.bFF[s], [stFF.b])
            if astep < 3:
                continue

            for j in range(4):
                for (off, n, c0) in ((O_T1, 512, 0), (O_T2, 256, 512)):
                    ps = self.next_ps()
                    for kc in range(8):
                        self.mm(ps.ap[:, 0:n], hT.ap[:, kc, j * 128:(j + 1) * 128], Win.ap[:, kc, off:off + n], kc == 0, kc == 7, [Win.b, hT.b], [ps.b])
                    evac(stT.ap[:, j, c0:c0 + n], ps.ap[:, 0:n], [ps.b], [stT.b])

            if astep < 4:
                continue
            for i in range(2):
                ps = self.next_ps()
                for kc in range(8):
                    self.mm(ps.ap, Win.ap[:, kc, O_QL + 128 * i:O_QL + 128 * (i + 1)], hT.ap[:, kc, :], kc == 0, kc == 7, [Win.b, hT.b], [ps.b])
                self.cp("dve", qlT.ap[:, i, :], ps.ap, [ps.b], [qlT.b])
                self.act(sqq.ap[:, i, :], qlT.ap[:, i, :], AF.Square, [qlT.b], [sqq.b])
            ps = self.next_ps()
            for kc in range(8):
                self.mm(ps.ap, Win.ap[:, kc, O_KVL:O_KVL + 128], hT.ap[:, kc, :], kc == 0, kc == 7, [Win.b, hT.b], [ps.b])
            self.cp("dve", kvf.ap, ps.ap, [ps.b], [kvf.b])
            self.act(sqkv.ap, kvf.ap, AF.Square, [kvf.b], [sqkv.b])
            if astep < 3.5:
                continue
            ps = self.next_ps()
            for i in range(2):
                self.mm(ps.ap, self.ones.ap, sqq.ap[:, i, :], i == 0, i == 1, [self.ones.b, sqq.b], [ps.b])
            self.ts("dve", rq.ap, ps.ap, 1.0 / 256, ALU.mult, [ps.b], [rq.b], s2=EPS, op1=ALU.add)
            self.act(rq.ap, rq.ap, AF.Sqrt, [rq.b], [rq.b])
            self.recip(rq.ap, rq.ap, [rq.b], [rq.b])
            ps = self.next_ps()
            self.mm(ps.ap, self.ones.ap, sqkv.ap, True, True, [self.ones.b, sqkv.b], [ps.b])
            self.ts("dve", rkv.ap, ps.ap, 1.0 / 128, ALU.mult, [ps.b], [rkv.b], s2=EPS, op1=ALU.add)
            self.act(rkv.ap, rkv.ap, AF.Sqrt, [rkv.b], [rkv.b])
            self.recip(rkv.ap, rkv.ap, [rkv.b], [rkv.b])
            self.tt("dve", kvT.ap, kvf.ap, rkv.ap, ALU.mult, [kvf.b, rkv.b], [kvT.b])
            if astep < 5:
                continue
            var = 0 if self.phases is None else self.phases.get("var", 0)
            if var == 1:
                evac_rr[0] = 1
            psa = fproj(O_KR, 128, t1.ap, t1.b)
            if var == 1:
                evac_rr[0] = 1
            psb_ = fproj(O_KRP, 128, t2.ap, t2.b)
            if var == 2:
                continue
            self.tt("dve", t1.ap[64:96, :], t1.ap[64:96, :], rope.ap[64:96, 2, :], ALU.mult, [t1.b, rope.b], [t1.b])
            self.tt("dve", t2.ap[64:96, :], t2.ap[64:96, :], rope.ap[64:96, 3, :], ALU.mult, [t2.b, rope.b], [t2.b])
            self.tt("dve", kr.ap[64:96, :], t1.ap[64:96, :], t2.ap[64:96, :], ALU.add, [t1.b, t2.b], [kr.b])
            if astep < 5.1:
                continue
            stq = stF[stF_rr[0] % 2]
            stF_rr[0] += 1
            for h in range(4):
                ps1 = self.next_ps()
                for kc in range(2):
                    self.mm(ps1.ap, WQ.ap[:, kc, h, :], qlT.ap[:, kc, :], kc == 0, kc == 1, [WQ.b, qlT.b], [ps1.b])
                ps2 = self.next_ps()
                for kc in range(2):
                    self.mm(ps2.ap, WQP.ap[:, kc, h, :], qlT.ap[:, kc, :], kc == 0, kc == 1, [WQP.b, qlT.b], [ps2.b])
                self.tt("dve", t1.ap[0:96, :], ps1.ap[0:96, :], rope.ap[:, 0, :], ALU.mult, [ps1.b, rope.b], [t1.b])
                self.tt("dve", t2.ap[64:96, :], ps2.ap[64:96, :], rope.ap[64:96, 1, :], ALU.mult, [ps2.b, rope.b], [t2.b])
                self.tt("dve", t1.ap[64:96, :], t1.ap[64:96, :], t2.ap[64:96, :], ALU.add, [t1.b, t2.b], [t1.b])
                self.tt("dve", stq.ap[0:96, h, :], t1.ap[0:96, :], rq.ap[0:96, :], ALU.mult, [t1.b, rq.b], [stq.b])
                if astep < 5.2:
                    continue
                psk = self.next_ps()
                self.mm(psk.ap, WK.ap[:, h, :], kvT.ap, True, True, [WK.b, kvT.b], [psk.b])
                self.cp("act", stq.ap[0:64, 4 + h, :], psk.ap[0:64, :], [psk.b], [stq.b])
                self.cp("act", stq.ap[64:96, 4 + h, :], kr.ap[64:96, :], [kr.b], [stq.b])
            if astep < 5.3:
                continue
            self.dma(self.FS[s][F_QD:F_QD + 8, 0:96, tok].rearrange("n p t -> p n t"), stq.ap[0:96, :, :], self.bFS[s], [stq.b])
            if astep < 5.4:
                continue
            for j in range(4):
                ps = self.next_ps()
                self.mm(ps.ap[:, 0:256], kvT.ap[:, j * 128:(j + 1) * 128], WV.ap, True, True, [WV.b, kvT.b], [ps.b])
                self.cp("dve", stT.ap[:, j, V_D:V_D + 256], ps.ap[:, 0:256], [ps.b], [stT.b])
            self.dma(self.VS[s][tok, :].rearrange("(j p) c -> p j c", p=128), stT.ap, self.bVS[s], [stT.b])
        self.release(m0, xs + [gain, rope, gq, gkv])
        P.barrier()

    def load_big(self, dst, src2d, nk, ncol, col0=0, dcol0=0):
        c0 = 0
        step = max(1, 1024 // nk)
        step = min(step, 512)
        while c0 < ncol:
            k = min(step, ncol - c0)
            src = src2d[:, col0 + c0: col0 + c0 + k].rearrange("(k p) n -> p k n", p=128)
            self.wload(dst.ap[:, :, dcol0 + c0: dcol0 + c0 + k], src, dst.b, [128, nk, k])
            c0 += k

    def post_norm_residual(self, pss, xc_ap, xc_b, gain, out_ap, out_b, stat, junk):
        self.memset("dve", stat.ap[:, 0:2], 0.0, [stat.b])
        for n in range(2):
            sl = slice(n * 512, (n + 1) * 512)
            self.cp("act", out_ap[:, sl], pss[n].ap, [pss[n].b], [out_b])
            self.act(junk.ap[:, 0:512], out_ap[:, sl], AF.Square, [out_b], [junk.b, stat.b], accum=stat.ap[:, n:n + 1])
        self.tt("dve", stat.ap[:, 2:3], stat.ap[:, 0:1], stat.ap[:, 1:2], ALU.add, [stat.b], [stat.b])
        self.rstd_from_ss(stat.ap[:, 3:4], stat.ap[:, 2:3], D, [stat.b])
        for n in range(2):
            sl = slice(n * 512, (n + 1) * 512)
            self.stt("dve", out_ap[:, sl], out_ap[:, sl], stat.ap[:, 3:4], gain.ap[:, sl], ALU.mult, ALU.mult, [out_b, stat.b, gain.b], [out_b])
            self.tt("dve", out_ap[:, sl], out_ap[:, sl], xc_ap[:, sl], ALU.add, [out_b, xc_b], [out_b])

    def phase_C1(self, l):
        P = self.P
        m0 = self.mark()
        Wg = [self.tile([128, 8, D], BF16, name=f"Wg{i}") for i in range(4)]
        Wb = self.tile([128, 7, D], BF16, name="Wb")
        Wo = self.tile([128, 8, D], BF16, name="Wo")
        for i in range(4):
            self.load_big(Wg[i], self.w["w_merge_gate"][l, i], 8, D)
        for (nm, k0, nk) in (("w_branch_a", 0, 2), ("w_branch_b", 2, 1), ("w_branch_c", 3, 2), ("w_branch_d", 5, 2)):
            for kk in range(nk):
                for c0 in range(0, D, 512):
                    src = self.w[nm][l][kk * 128:(kk + 1) * 128, c0:c0 + 512]
                    self.wload(Wb.ap[:, k0 + kk, c0:c0 + 512], src, Wb.b, [128, 512])
        self.load_big(Wo, self.w["w_o"][l], 8, D)
        gain = self.tile([128, D], F32, 1, name="gain")
        g_src = self.w["norm_attn_post"][l]
        self.dma(gain.ap, bass.AP(g_src.tensor, g_src.offset, [[0, 128], [1, D]]), gain.b)
        hTs = [self.tile([128, 8, 512], BF16, 1, name="hT") for _ in range(2)]
        oTs = [self.tile([128, 7, 512], BF16, 1, name="oT") for _ in range(2)]
        xs = [self.tile([128, 4, D], F32, 1, name="xc") for _ in range(2)]
        mT = self.tile([128, 8, 512], BF16, name="mT")
        macc = self.tile([128, 512], F32, name="macc")
        sg = [self.tile([128, 512], F32, name="sg") for _ in range(2)]
        tm = [self.tile([128, 512], F32, name="tm") for _ in range(2)]
        xo = [self.tile([128, D], F32, name="xo") for _ in range(2)]
        junk = self.tile([128, 512], BF16, name="junk")
        stat = self.tile([128, 8], F32, name="stat")
        branch_k = [(0, 2), (2, 1), (3, 2), (5, 2)]
        chunks = self.limit([(s, tc) for s in range(NB) for tc in range(NTC)], "nchunks_c")

        def loads(ci):
            s, tc = chunks[ci]
            tok = slice(tc * 512, (tc + 1) * 512)
            self.dma(hTs[ci % 2].ap, self.HT[s][:, :, tok].rearrange("k p t -> p k t"), hTs[ci % 2].b, [self.bHT[s]])
            self.dma(oTs[ci % 2].ap, self.OS[s][:, :, tok].rearrange("k p t -> p k t"), oTs[ci % 2].b, [self.bOS[s]])
            src = self.x_in[s] if l == 0 else self.X2[s]
            self.dma(xs[ci % 2].ap, src[tok, :].rearrange("(j p) d -> p j d", p=128), xs[ci % 2].b, [] if l == 0 else [self.bX2[s]])
        loads(0)
        rr = 0
        for ci, (s, tc) in enumerate(chunks):
            if ci + 1 < len(chunks):
                loads(ci + 1)
            hT, oT, xc = hTs[ci % 2], oTs[ci % 2], xs[ci % 2]
            tok = slice(tc * 512, (tc + 1) * 512)
            for oc in range(8):
                ocs = slice(oc * 128, (oc + 1) * 128)
                for i in range(4):
                    psg = self.next_ps()
                    for kc in range(8):
                        self.mm(psg.ap, Wg[i].ap[:, kc, ocs], hT.ap[:, kc, :], kc == 0, kc == 7, [Wg[i].b, hT.b], [psg.b])
                    psb = self.next_ps()
                    k0, nk = branch_k[i]
                    for kk in range(nk):
                        self.mm(psb.ap, Wb.ap[:, k0 + kk, ocs], oT.ap[:, k0 + kk, :], kk == 0, kk == nk - 1, [Wb.b, oT.b], [psb.b])
                    g = sg[rr % 2]
                    t = tm[rr % 2]
                    rr += 1
                    self.act(g.ap, psg.ap, AF.Sigmoid, [psg.b], [g.b])
                    if i == 0:
                        self.tt("dve", macc.ap, g.ap, psb.ap, ALU.mult, [g.b, psb.b], [macc.b])
                    elif i < 3:
                        self.tt("dve", t.ap, g.ap, psb.ap, ALU.mult, [g.b, psb.b], [t.b])
                        self.tt("dve", macc.ap, macc.ap, t.ap, ALU.add, [macc.b, t.b], [macc.b])
                    else:
                        self.tt("dve", t.ap, g.ap, psb.ap, ALU.mult, [g.b, psb.b], [t.b])
                        self.tt("dve", mT.ap[:, oc, :], macc.ap, t.ap, ALU.add, [macc.b, t.b], [mT.b])
            for j in range(4):
                pss = [self.next_ps(), self.next_ps()]
                for n in range(2):
                    for kc in range(8):
                        self.mm(pss[n].ap, mT.ap[:, kc, j * 128:(j + 1) * 128], Wo.ap[:, kc, n * 512:(n + 1) * 512], kc == 0, kc == 7, [mT.b, Wo.b], [pss[n].b])
                o = xo[j % 2]
                self.post_norm_residual(pss, xc.ap[:, j, :], xc.b, gain, o.ap, o.b, stat, junk)
                self.dma(self.X1[s][tc * 512 + j * 128: tc * 512 + (j + 1) * 128, :], o.ap, self.bX1[s], [o.b])
        self.release(m0, hTs + oTs + xs + [gain])
        P.barrier()

    def phase_C2(self, l):
        P = self.P
        m0 = self.mark()
        TC = 256
        Wup = self.tile([128, 8, 2 * DFF], BF16, name="Wup")
        Wdn = self.tile([128, 22, D], BF16, name="Wdn")
        self.load_big(Wup, self.w["ffn_w_up"][l], 8, 2 * DFF)
        self.load_big(Wdn, self.w["ffn_w_down"][l], 22, D)
        cw = self.tile([128, 4, 44], F32, 1, name="cw")
        for j in range(3):
            self.load_pvec(cw.ap[:, j, :], cw.b, self.w["ffn_conv_w"][l, j].rearrange("(c p) -> c p", p=128), 44)
        self.load_pvec(cw.ap[:, 3, :], cw.b, self.w["ffn_conv_b"][l].rearrange("(c p) -> c p", p=128), 44)
        gpre = self.tile([128, D], F32, 1, name="gpre")
        gpost = self.tile([128, D], F32, 1, name="gpost")
        for t_, nm in ((gpre, "norm_ffn_pre"), (gpost, "norm_ffn_post")):
            g_src = self.w[nm][l]
            self.dma(t_.ap, bass.AP(g_src.tensor, g_src.offset, [[0, 128], [1, D]]), t_.b)
        NJ = TC // 128
        xs = [self.tile([128, NJ, D], F32, 1, name="xc") for _ in range(2)]
        hn = [self.tile([128, D], BF16, name="hn") for _ in range(2)]
        hT = self.tile([128, 8, TC], BF16, name="hT")
        aT = self.tile([128, 22, TC], BF16, name="aT")
        ug = [self.tile([128, TC + 2], F32, name="ug") for _ in range(2)]
        uv = [self.tile([128, TC + 2], F32, name="uv") for _ in range(2)]
        cg = [self.tile([128, TC], F32, name="cg") for _ in range(2)]
        cv = [self.tile([128, TC], F32, name="cv") for _ in range(2)]
        halo = self.tile([128, 44, 2], F32, name="halo")
        tv = self.tile([128, TC], F32, name="tv")
        xo = [self.tile([128, D], F32, name="xo") for _ in range(1)]
        junk = self.tile([128, D], BF16, name="junk")
        stat = self.tile([128, 16], F32, name="stat")
        last = (l == DEPTH - 1)
        chunks = self.limit([(s, tc) for s in range(NB) for tc in range(S // TC)], "nchunks_c2")

        def loads(ci):
            s, tc = chunks[ci]
            self.dma(xs[ci % 2].ap, self.X1[s][tc * TC:(tc + 1) * TC, :].rearrange("(j p) d -> p j d", p=128), xs[ci % 2].b, [self.bX1[s]])
        loads(0)
        rr = 0
        for ci, (s, tc) in enumerate(chunks):
            if ci + 1 < len(chunks):
                loads(ci + 1)
            xc = xs[ci % 2]
            if tc == 0:
                self.memset("pool", halo.ap, 0.0, [halo.b])
            self.memset("dve", stat.ap[:, 0:NJ], 0.0, [stat.b])
            for j in range(NJ):
                self.act(junk.ap, xc.ap[:, j, :], AF.Square, [xc.b], [junk.b, stat.b], accum=stat.ap[:, j:j + 1])
            self.rstd_from_ss(stat.ap[:, 4:4 + NJ], stat.ap[:, 0:NJ], D, [stat.b])
            for j in range(NJ):
                hj = hn[j % 2]
                self.stt("dve", hj.ap, xc.ap[:, j, :], stat.ap[:, 4 + j:5 + j], gpre.ap, ALU.mult, ALU.mult, [xc.b, stat.b, gpre.b], [hj.b])
                ps = self.next_ps()
                psb = ps.ap.bitcast(BF16).rearrange("p (k t) -> p k t", k=8)
                for kc in range(8):
                    self.tr(psb[:, kc, :], hj.ap[:, kc * 128:(kc + 1) * 128], self.ident.ap, [hj.b, self.ident.b], [ps.b])
                self.cp("act", hT.ap[:, :, j * 128:(j + 1) * 128], psb, [ps.b], [hT.b])
            for i in range(22):
                U = []
                for (ch, ubuf) in ((i, ug[rr % 2]), (22 + i, uv[rr % 2])):
                    ps = self.next_ps()
                    for kc in range(8):
                        self.mm(ps.ap[:, 0:TC], Wup.ap[:, kc, ch * 128:(ch + 1) * 128], hT.ap[:, kc, :], kc == 0, kc == 7, [Wup.b, hT.b], [ps.b])
                    self.cp("pool", ubuf.ap[:, 0:2], halo.ap[:, ch, :], [halo.b], [ubuf.b])
                    self.cp("act", ubuf.ap[:, 2:TC + 2], ps.ap[:, 0:TC], [ps.b], [ubuf.b])
                    self.cp("pool", halo.ap[:, ch, :], ubuf.ap[:, TC:TC + 2], [ubuf.b], [halo.b])
                    U.append((ch, ubuf))
                outs = (cg[rr % 2], cv[rr % 2])
                for (ch, ubuf), o, eng in zip(U, outs, ("dve", "dve")):
                    self.ts(eng, o.ap, ubuf.ap[:, 0:TC], cw.ap[:, 0, ch:ch + 1], ALU.mult, [ubuf.b, cw.b], [o.b], s2=cw.ap[:, 3, ch:ch + 1], op1=ALU.add)
                    for k_ in (1, 2):
                        if eng == "dve":
                            self.stt(eng, o.ap, ubuf.ap[:, k_:TC + k_], cw.ap[:, k_, ch:ch + 1], o.ap, ALU.mult, ALU.add, [ubuf.b, cw.b, o.b], [o.b])
                        else:
                            self.ts(eng, tv.ap, ubuf.ap[:, k_:TC + k_], cw.ap[:, k_, ch:ch + 1], ALU.mult, [ubuf.b, cw.b], [tv.b])
                            self.tt(eng, o.ap, o.ap, tv.ap, ALU.add, [o.b, tv.b], [o.b])
                g_, v_ = outs
                self.act(ug[rr % 2].ap[:, 0:TC], g_.ap, AF.Sigmoid, [g_.b], [ug[rr % 2].b])
                self.tt("dve", g_.ap, g_.ap, ug[rr % 2].ap[:, 0:TC], ALU.mult, [g_.b, ug[rr % 2].b], [g_.b])
                self.tt("dve", aT.ap[:, i, :], g_.ap, v_.ap, ALU.mult, [g_.b, v_.b], [aT.b])
                rr += 1
            for j in range(NJ):
                pss = [self.next_ps(), self.next_ps()]
                for n in range(2):
                    for kc in range(22):
                        self.mm(pss[n].ap, aT.ap[:, kc, j * 128:(j + 1) * 128], Wdn.ap[:, kc, n * 512:(n + 1) * 512], kc == 0, kc == 21, [aT.b, Wdn.b], [pss[n].b])
                o = xo[0]
                self.post_norm_residual(pss, xc.ap[:, j, :], xc.b, gpost, o.ap, o.b, stat_c2(stat), junk)
                r0 = tc * TC + j * 128
                if last:
                    op = self.dma(self.y_out[s][r0:r0 + 128, :], o.ap, self.bY, [o.b])
                    self.final_ops.append(op)
                else:
                    self.dma(self.X2[s][r0:r0 + 128, :], o.ap, self.bX2[s], [o.b])
        self.release(m0, xs + [gpre, gpost, cw])
        P.barrier()


class _StatView:
    def __init__(self, ap, b):
        self.ap = ap
        self.b = b


def stat_c2(stat):
    return _StatView(stat.ap[:, 8:16], stat.b)


def _attn_job(self, q_ap, n, ktiles, fin, rbufs, pring):
    psO = self.next_ps(4, 6)
    nk = len(ktiles)
    pend = None
    pts = []

    def pv(idx, pt, v_l, c0):
        self.mm(psO.ap[:, c0:n], v_l, pt.ap[:, c0:n], idx == 0, idx == nk - 1, rbufs + [pt.b], [psO.b])
    for idx, (k_l, extras, v_l, c0) in enumerate(ktiles):
        psS = self.next_ps(0, 4)
        mms = [(k_l, q_ap[:, c0:n])] + list(extras)
        for mi, (l_, r_) in enumerate(mms):
            self.mm(psS.ap[:, c0:n], l_, r_, mi == 0, mi == len(mms) - 1, rbufs, [psS.b])
        pt = pring[self.p_rr % len(pring)]
        self.p_rr += 1
        pts.append(pt)
        self.act(pt.ap[:, c0:n], psS.ap[:, c0:n], AF.Exp, [psS.b], [pt.b])
        if pend is not None:
            pv(*pend)
        pend = (idx, pt, v_l, c0)
    pv(*pend)
    fin(psO, pts)


def _recip_den(self, psO, rc, n):
    r = rc.ap[0:64, 0:n]
    self.ts("dve", r, psO.ap[64:128, 0:n], TINY, ALU.max, [psO.b], [rc.b])
    self.recip(r, r, [rc.b], [rc.b])
    return r


def _load_v(self, V, s, col0, nh):
    self.memset("pool", V.ap[:, :, :, 64:128], 1.0, [V.b])
    src = self.VS[s][:, col0:col0 + nh * 64].rearrange("(j p) (h c) -> p j h c", p=128, h=nh)
    for h in range(nh):
        self.dma(V.ap[:, :, h, 0:64], src[:, :, h, :], V.b, [self.bVS[s]])


def _nqc(self):
    if self.phases is not None and "nqc" in self.phases:
        return self.phases["nqc"]
    return 8


def _nseq(self):
    if self.phases is not None and "nseq" in self.phases:
        return self.phases["nseq"]
    return NB


def _phase_causal(self, l, kind):
    P = self.P
    m0 = self.mark()
    rows = 96 if kind == "D" else 70
    vcol = V_D if kind == "D" else V_C
    os0 = 5 if kind == "D" else 3
    Hc = self.tile([128, 896], BF16, 1, name="Hc")
    self.hankel(Hc.ap, Hc.b, self.GC_, 0, 896)
    pring = [self.tile([128, 512], BF16, name="pt") for _ in range(4)]
    rcs = [self.tile([128, 512], F32, name="rc") for _ in range(2)]
    osts = [self.tile([128, 2, 512], BF16, name="ost") for _ in range(2)]
    QT = [self.tile([rows, S], BF16, 1, name="QT") for _ in range(4)]
    KT = [self.tile([rows, S], BF16, 1, name="KT") for _ in range(4)]
    V = self.tile([128, 32, 4, 128], BF16, 1, name="V")
    extra_tiles = []
    if kind == "C":
        W = 1024
        fx = self.tile([4, W], F32, 1, name="fx")
        e1 = self.tile([4, W], F32, name="e1")
        onesf = self.tile([4, W], F32, name="onesf")
        cn = self.tile([4, W], F32, name="cn")
        t32 = self.tile([4, W], F32, name="t32")
        augk = self.tile([4, 6, W], BF16, name="augk")
        augq = self.tile([4, 6, W], BF16, name="augq")
        carry = self.tile([4, 1], F32, name="carry")
        fb = self.tile([4, 1], F32, 1, name="fb")
        fsrc = self.w["fox_forget_bias"][l]
        self.dma(fb.ap, bass.AP(fsrc.tensor, fsrc.offset, [[1, 4], [1, 1]]), fb.b)
        self.memset("pool", onesf.ap, 1.0, [onesf.b])
        self.memset("pool", augk.ap[:, 0:3, :], 1.0, [augk.b])
        self.memset("pool", augq.ap[:, 3:6, :], 1.0, [augq.b])
        extra_tiles = [fx, fb]
    self.p_rr = 0
    rr = 0
    for s in range(_nseq(self)):
        if kind == "C":
            for k in range(S // W):
                cols = slice(k * W, (k + 1) * W)
                self.dma(fx.ap, self.FF[s][:, cols], fx.b, [self.bFF[s]])
                self.ts("dve", e1.ap, fx.ap, fb.ap[:, 0:1], ALU.add, [fx.b, fb.b], [e1.b])
                self.act(e1.ap, e1.ap, AF.Exp, [e1.b], [e1.b], scale=-1.0)
                self.act(e1.ap, e1.ap, AF.Ln, [e1.b], [e1.b], bias=1.0)
                init = 0.0 if k == 0 else carry.ap[:, 0:1]
                self.P.op("dve", lambda e, init=init: e.tensor_tensor_scan(out=cn.ap, data0=onesf.ap, data1=e1.ap, initial=init, op0=ALU.mult, op1=ALU.add),
                          [onesf.b, e1.b, carry.b], [cn.b])
                self.cp("dve", carry.ap, cn.ap[:, W - 1:W], [cn.b], [carry.b])
                self.cp("dve", augk.ap[:, 3, :], cn.ap, [cn.b], [augk.b])
                self.cp("dve", t32.ap, augk.ap[:, 3, :], [augk.b], [t32.b])
                self.tt("dve", e1.ap, cn.ap, t32.ap, ALU.subtract, [cn.b, t32.b], [e1.b])
                self.cp("dve", augk.ap[:, 4, :], e1.ap, [e1.b], [augk.b])
                self.cp("dve", t32.ap, augk.ap[:, 4, :], [augk.b], [t32.b])
                self.tt("dve", e1.ap, e1.ap, t32.ap, ALU.subtract, [e1.b, t32.b], [e1.b])
                self.cp("dve", augk.ap[:, 5, :], e1.ap, [e1.b], [augk.b])
                self.ts("dve", augq.ap[:, 0:3, :], augk.ap[:, 3:6, :], -1.0, ALU.mult, [augk.b], [augq.b])
                self.dma(self.AUG[s][0][:, :, cols], augq.ap, self.bAUG[s], [augq.b])
                self.dma(self.AUG[s][1][:, :, cols], augk.ap, self.bAUG[s], [augk.b])
        for h in range(4):
            if kind == "D":
                self.dma(QT[h].ap, self.FS[s][F_QD + h, 0:96, :], QT[h].b, [self.bFS[s]])
                self.dma(KT[h].ap, self.FS[s][F_KD + h, 0:96, :], KT[h].b, [self.bFS[s]])
            else:
                rws = slice((h % 2) * 64, (h % 2) * 64 + 64)
                self.dma(QT[h].ap[0:64, :], self.FS[s][F_QC + h // 2, rws, :], QT[h].b, [self.bFS[s]])
                self.dma(QT[h].ap[64:70, :], self.AUG[s][0, h], QT[h].b, [self.bAUG[s]])
                self.dma(KT[h].ap[0:64, :], self.FS[s][F_KC + h // 2, rws, :], KT[h].b, [self.bFS[s]])
                self.dma(KT[h].ap[64:70, :], self.AUG[s][1, h], KT[h].b, [self.bAUG[s]])
        _load_v(self, V, s, vcol, 4)
        for c in range(_nqc(self)):
            ost = osts[c % 2]
            for h in range(4):
                rc = rcs[rr % 2]
                rr += 1
                kt = []
                for j in range(4 * c + 4):
                    c0 = max(0, 128 * (j - 4 * c))
                    extras = [(self.anti.ap, Hc.ap[:, 384:384 + 512 - c0])] if j >= 4 * c else []
                    kt.append((KT[h].ap[:, j * 128:(j + 1) * 128], extras, V.ap[:, j, h, :], c0))
                out_ap = ost.ap[(h % 2) * 64:(h % 2) * 64 + 64, h // 2, :]

                def fin(psO, pts, out_ap=out_ap, rc=rc, ost=ost):
                    r = _recip_den(self, psO, rc, 512)
                    self.tt("dve", out_ap, psO.ap[0:64, :], r, ALU.mult, [psO.b, rc.b], [ost.b])
                _attn_job(self, QT[h].ap[:, c * 512:(c + 1) * 512], 512, kt, fin, [QT[h].b, KT[h].b, V.b, Hc.b, self.anti.b], pring)
            self.dma(self.OS[s][os0:os0 + 2, :, c * 512:(c + 1) * 512].rearrange("k p t -> p k t"), ost.ap, self.bOS[s], [ost.b])
    self.release(m0, [Hc, V] + QT + KT + extra_tiles)
    P.barrier()


def _phase_BB(self, l):
    P = self.P
    m0 = self.mark()
    Hb = self.tile([128, 6, 1024], BF16, 1, name="Hb")
    for i in range(6):
        self.hankel(Hb.ap[:, i, :], Hb.b, self.GB_, i, 1024)
    pring = [self.tile([128, 512], BF16, name="pt") for _ in range(4)]
    rcs = [self.tile([128, 512], F32, name="rc") for _ in range(2)]
    osts = [self.tile([128, 512], BF16, name="ost") for _ in range(2)]
    QB = self.tile([128, 3, S], BF16, 1, name="QB")
    KB = self.tile([128, 3, S], BF16, 1, name="KB")
    acc = self.tile([128, 2, S], F32, name="acc")
    Vr = [self.tile([128, 32, 2, 128], BF16, 1, name="Vg") for _ in range(2)]
    self.p_rr = 0
    rr = 0
    for s in range(_nseq(self)):
        self.dma(QB.ap, self.FS[s][F_QB:F_QB + 3, :, :].rearrange("k p t -> p k t"), QB.b, [self.bFS[s]])
        self.dma(KB.ap, self.FS[s][F_KB:F_KB + 3, :, :].rearrange("k p t -> p k t"), KB.b, [self.bFS[s]])
        for g, dil in enumerate((1, 4, 16)):
            Vg = Vr[g % 2]
            nj = 32 // dil
            self.memset("pool", Vg.ap[:, :, :, 64:128], 1.0, [Vg.b])
            for r in range(dil):
                for h in range(2):
                    src = dram_ap(self.VS[s], r * NV + V_B + g * 128 + h * 64, [[dil * NV, 128], [128 * dil * NV, nj], [1, 64]])
                    self.dma(Vg.ap[:, r * nj:(r + 1) * nj, h, 0:64], src, Vg.b, [self.bVS[s]])
            L = S // dil
            n = min(512, L)
            for h in range(2):
                hb = 64 * h
                for r in range(dil):
                    for uq0 in range(0, L, n):
                        kt = []
                        for j in range(uq0 // 128 - 1, (uq0 + n) // 128):
                            if j < 0:
                                continue
                            dlt = uq0 - 128 * j
                            c0 = max(0, -dlt)
                            k_l = self.sbv(KB.ap[hb:hb + 64, g, :], r + dil * 128 * j, [64, [dil, 128]])
                            extras = [(self.anti.ap, Hb.ap[:, 2 * g + h, dlt + 384 + c0:dlt + 384 + n])]
                            kt.append((k_l, extras, Vg.ap[:, r * nj + j, h, :], c0))
                        q_ap = self.sbv(QB.ap[hb:hb + 64, g, :], r + dil * uq0, [64, [dil, n]])
                        accv = self.sbv(acc.ap[:, h, :], r + dil * uq0, [128, [dil, n]])

                        def fin(psO, pts, accv=accv, n=n, g=g):
                            if g == 0:
                                self.cp("act", accv, psO.ap[:, 0:n], [psO.b], [acc.b])
                            else:
                                self.tt("dve", accv, accv, psO.ap[:, 0:n], ALU.add, [psO.b, acc.b], [acc.b])
                        _attn_job(self, q_ap, n, kt, fin, [QB.b, KB.b, Vg.b, Hb.b, self.anti.b], pring)
        for c in range(8):
            ost = osts[c % 2]
            for h in range(2):
                rc = rcs[rr % 2]
                rr += 1
                r_ = rc.ap[0:64, :]
                self.ts("dve", r_, acc.ap[64:128, h, c * 512:(c + 1) * 512], TINY, ALU.max, [acc.b], [rc.b])
                self.recip(r_, r_, [rc.b], [rc.b])
                self.tt("dve", ost.ap[h * 64:h * 64 + 64, :], acc.ap[0:64, h, c * 512:(c + 1) * 512], r_, ALU.mult, [acc.b, rc.b], [ost.b])
            self.dma(self.OS[s][2, :, c * 512:(c + 1) * 512], ost.ap, self.bOS[s], [ost.b])
    self.release(m0, [Hb, QB, KB] + Vr)
    P.barrier()


def _phase_BA(self, l):
    P = self.P
    m0 = self.mark()
    Hs = self.tile([128, 4, 2560], BF16, 1, name="Hs")
    Hw = self.tile([128, 4, 1408], BF16, 1, name="Hw")
    for h in range(4):
        self.hankel(Hs.ap[:, h, :], Hs.b, self.GA_, h, 2560)
        self.hankel(Hw.ap[:, h, :], Hw.b, self.GW_, h, 1408)
    NM = CONSTS["c_cmpmask"].shape[0]
    cmask = self.tile([128, NM, 512], BF16, name="cmask")
    for i in range(NM):
        self.wload(cmask.ap[:, i, :], self.c["c_cmpmask"][i], cmask.b, [128, 512])
    eall = self.tile([64, S], BF16, name="eall")
    for k in range(4):
        self.wload(eall.ap[:, k * 1024:(k + 1) * 1024], self.c["c_eall"][:, k * 1024:(k + 1) * 1024], eall.b, [64, 1024])
    ovx = self.tile([128, 2, 128], BF16, name="ovx")
    for ct in range(2):
        self.wload(ovx.ap[:, ct, :], self.c["c_ovx"][ct], ovx.b, [128, 128])
    w1 = self.tile([128, 32, 128], BF16, name="w1")
    for kv, nm in enumerate(("nsa_phi_k_w1", "nsa_phi_v_w1")):
        src = self.w[nm][l].rearrange("(i d) m -> d i m", d=64)
        for i0 in range(0, 32, 8):
            self.wload(w1.ap[kv * 64:kv * 64 + 64, i0:i0 + 8, :], src[:, i0:i0 + 8, :], w1.b, [64, 8, 128])
    w2 = self.tile([128, 2, 128], BF16, name="w2")
    self.wload(w2.ap[:, 0, 0:64], self.w["nsa_phi_k_w2"][l], w2.b, [128, 64])
    self.wload(w2.ap[:, 0, 64:128], self.w["nsa_phi_k_w2"][l], w2.b, [128, 64])
    self.wload(w2.ap[:, 1, 0:64], self.w["nsa_phi_v_w2"][l], w2.b, [128, 64])
    pp = self.tile([32, 128], BF16, name="pp")
    self.wload(pp.ap[:, 0:64], self.w["nsa_cmp_pos"][l], pp.b, [32, 64])
    self.wload(pp.ap[:, 64:128], self.w["nsa_cmp_pos"][l], pp.b, [32, 64])
    posT = self.tile([128, 32], BF16, name="posT")
    ps = self.next_ps(6, 8)
    psb = ps.ap.bitcast(BF16)
    self.tr(psb[:, 0:32], pp.ap, self.ident.ap[0:32, 0:32], [pp.b, self.ident.b], [ps.b])
    self.cp("dve", posT.ap, psb[:, 0:32], [ps.b], [posT.b])
    posc = self.tile([128, 2], F32, name="posc")
    ps = self.next_ps(6, 8)
    for kv in range(2):
        for i in range(32):
            self.mm(ps.ap[:, kv:kv + 1], w1.ap[kv * 64:kv * 64 + 64, i, :], posT.ap[kv * 64:kv * 64 + 64, i:i + 1], i == 0, i == 31, [w1.b, posT.b], [ps.b])
    self.cp("dve", posc.ap, ps.ap[:, 0:2], [ps.b], [posc.b])

    pring = [self.tile([128, 512], BF16, name="pt") for _ in range(4)]
    rcs = [self.tile([128, 512], F32, name="rc") for _ in range(2)]
    tmps = [self.tile([128, 512], F32, name="tmp") for _ in range(2)]
    osts = [self.tile([128, 2, 512], BF16, name="ost") for _ in range(2)]
    Oacc = self.tile([128, 2, 512], F32, name="Oacc")
    CMP = self.tile([128, S], BF16, 1, name="CMP")
    gk = [self.tile([128, 256], BF16, name="gkv") for _ in range(2)]
    kcT = self.tile([128, 256], BF16, name="kcT")
    Vc = self.tile([128, 2, 128], BF16, name="Vc")
    QA = self.tile([128, 2, S], BF16, 1, name="QA")
    KS = self.tile([128, S], BF16, 1, name="KS")
    KW = self.tile([128, S], BF16, 1, name="KW")
    V2 = self.tile([128, 32, 2, 128], BF16, 1, name="V2")
    GAr = [self.tile([128, 6, 512], BF16, 1, name="GAc") for _ in range(2)]
    fkr = [self.tile([128, 4, 64], F32, 1, name="fk") for _ in range(2)]
    far = [self.tile([128, 4, 64], F32, 1, name="fa") for _ in range(2)]
    impacc = self.tile([128, 4, 64], F32, name="impacc")
    impt = self.tile([128, 4, 64], F32, name="impt")
    rI = self.tile([128, 4], F32, name="rI")
    m8 = self.tile([128, 16], F32, name="m8")
    wk = self.tile([128, 64], F32, name="wk")
    selb = self.tile([128, 4, 64], BF16, name="selb")
    selT = self.tile([64, 512], BF16, name="selT")
    self.memset("pool", gk[0].ap[:, 255:256], 0.0, [gk[0].b])
    self.memset("pool", gk[1].ap[:, 255:256], 0.0, [gk[1].b])
    self.memset("pool", Vc.ap[:, :, 64:128], 1.0, [Vc.b])
    self.p_rr = 0
    rr = 0
    for s in range(_nseq(self)):
        self.dma(CMP.ap, self.FS[s][F_CMP], CMP.b, [self.bFS[s]])
        for kv in range(2):
            ps = self.next_ps(6, 8)
            for i in range(32):
                rhs = self.sbv(CMP.ap[kv * 64:kv * 64 + 64, :], i, [64, [16, 255]])
                self.mm(ps.ap[:, 0:255], w1.ap[kv * 64:kv * 64 + 64, i, :], rhs, i == 0, i == 31, [w1.b, CMP.b], [ps.b])
            self.act(gk[kv].ap[:, 0:255], ps.ap[:, 0:255], AF.Gelu_apprx_tanh, [ps.b, posc.b], [gk[kv].b], bias=posc.ap[:, kv:kv + 1])
        ps = self.next_ps(6, 8)
        self.mm(ps.ap[:, 0:256], w2.ap[:, 0, :], gk[0].ap, True, True, [w2.b, gk[0].b], [ps.b])
        self.cp("dve", kcT.ap, ps.ap[:, 0:256], [ps.b], [kcT.b])
        for ct in range(2):
            ps = self.next_ps(6, 8)
            self.mm(ps.ap[:, 0:64], gk[1].ap[:, ct * 128:(ct + 1) * 128], w2.ap[:, 1, 0:64], True, True, [w2.b, gk[1].b], [ps.b])
            self.cp("dve", Vc.ap[:, ct, 0:64], ps.ap[:, 0:64], [ps.b], [Vc.b])
        self.dma(QA.ap, self.FS[s][F_QA:F_QA + 2, :, :].rearrange("k p t -> p k t"), QA.b, [self.bFS[s]])
        self.dma(KS.ap, self.FS[s][F_KSLC], KS.b, [self.bFS[s]])
        self.dma(KW.ap, self.FS[s][F_KWIN], KW.b, [self.bFS[s]])
        _load_v(self, V2, s, V_SLC, 2)
        for c in range(_nqc(self)):
            tok = slice(c * 512, (c + 1) * 512)
            GAc, fk, fa = GAr[c % 2], fkr[c % 2], far[c % 2]
            ost = osts[c % 2]
            self.dma(GAc.ap, self.FS[s][F_GA:F_GA + 6, :, tok].rearrange("k p t -> p k t"), GAc.b, [self.bFS[s]])
            self.dma(fk.ap, self.c["c_fkeep"][:, c * 256:(c + 1) * 256].rearrange("p (q j) -> p q j", q=4), fk.b)
            self.dma(fa.ap, self.c["c_fadd"][:, c * 256:(c + 1) * 256].rearrange("p (q j) -> p q j", q=4), fa.b)
            rb_all = [QA.b, KS.b, KW.b, V2.b, Hs.b, Hw.b, kcT.b, Vc.b, cmask.b, eall.b, selT.b, self.anti.b, self.ident.b]

            def gated(psO, h, br, first, last):
                hb, hp = 64 * (h % 2), h // 2
                rc, tmp = rcs[h % 2], tmps[h % 2]
                r = _recip_den(self, psO, rc, 512)
                t = tmp.ap[hb:hb + 64, :]
                g_ap = GAc.ap[hb:hb + 64, hp * 3 + br, :]
                o_ap = Oacc.ap[hb:hb + 64, hp, :]
                self.tt("dve", t, psO.ap[0:64, :], r, ALU.mult, [psO.b, rc.b], [tmp.b])
                if first:
                    self.tt("dve", o_ap, t, g_ap, ALU.mult, [tmp.b, GAc.b], [Oacc.b])
                else:
                    self.tt("dve", t, t, g_ap, ALU.mult, [tmp.b, GAc.b], [tmp.b])
                    if last:
                        self.tt("dve", ost.ap[hb:hb + 64, hp, :], o_ap, t, ALU.add, [Oacc.b, tmp.b], [ost.b])
                    else:
                        self.tt("dve", o_ap, o_ap, t, ALU.add, [Oacc.b, tmp.b], [Oacc.b])

            for h in range(4):
                hb, hp = 64 * (h % 2), h // 2
                kt = []
                cts = []
                for ct in range(2):
                    key = CMPKEY[(ct, c)]
                    if key == "none":
                        continue
                    extras = [] if key == "all" else [(self.ident.ap, cmask.ap[:, key, :])]
                    kt.append((kcT.ap[hb:hb + 64, ct * 128:(ct + 1) * 128], extras, Vc.ap[:, ct, :], 0))
                    cts.append(ct)

                def fin_c(psO, pts, h=h, cts=cts):
                    psI = self.next_ps(6, 8)
                    pv_ = psI.ap.rearrange("p (q c) -> p q c", q=4)
                    for qt in range(4):
                        for ii, (ct, pt) in enumerate(zip(cts, pts)):
                            self.mm(pv_[:, qt, 0:65], pt.ap[:, qt * 128:(qt + 1) * 128], ovx.ap[:, ct, 0:65], ii == 0, ii == len(cts) - 1, [pt.b, ovx.b], [psI.b])
                    self.ts("dve", rI.ap, pv_[:, :, 64], TINY, ALU.max, [psI.b], [rI.b])
                    self.recip(rI.ap, rI.ap, [rI.b], [rI.b])
                    rbc = self.sbv(rI.ap, 0, [128, [1, 4], [0, 64]])
                    if h == 0:
                        self.tt("dve", impacc.ap, pv_[:, :, 0:64], rbc, ALU.mult, [psI.b, rI.b], [impacc.b])
                    else:
                        self.tt("dve", impt.ap, pv_[:, :, 0:64], rbc, ALU.mult, [psI.b, rI.b], [impt.b])
                        self.tt("pool", impacc.ap, impacc.ap, impt.ap, ALU.add, [impacc.b, impt.b], [impacc.b])
                    gated(psO, h, 0, True, False)
                _attn_job(self, QA.ap[hb:hb + 64, hp, tok], 512, kt, fin_c, rb_all, pring)
            self.tt("dve", impacc.ap, impacc.ap, fk.ap, ALU.mult, [impacc.b, fk.b], [impacc.b])
            self.tt("dve", impacc.ap, impacc.ap, fa.ap, ALU.add, [impacc.b, fa.b], [impacc.b])
            for qt in range(4):
                iv = impacc.ap[:, qt, :]
                self.P.op("dve", lambda e, iv=iv: e.max(out=m8.ap[:, 0:8], in_=iv), [impacc.b], [m8.b])
                self.P.op("dve", lambda e, iv=iv: e.match_replace(out=wk.ap, in_to_replace=m8.ap[:, 0:8], in_values=iv, imm_value=-3e9), [impacc.b, m8.b], [wk.b])
                self.P.op("dve", lambda e: e.max(out=m8.ap[:, 8:16], in_=wk.ap), [wk.b], [m8.b])
                self.ts("dve", wk.ap, iv, m8.ap[:, 15:16], ALU.is_ge, [impacc.b, m8.b], [wk.b], s2=-NEG, op1=ALU.mult)
                self.ts("dve", selb.ap[:, qt, :], wk.ap, NEG, ALU.add, [wk.b], [selb.b])
            ps = self.next_ps(6, 8)
            psb = ps.ap.bitcast(BF16)
            for qt in range(4):
                self.tr(psb[0:64, qt * 128:(qt + 1) * 128], selb.ap[:, qt, :], self.ident.ap, [selb.b, self.ident.b], [ps.b])
            self.cp("dve", selT.ap, psb[0:64, 0:512], [ps.b], [selT.b])
            for h in range(4):
                hb, hp = 64 * (h % 2), h // 2
                kt = []
                for j in range(4 * c + 4):
                    dlt = 512 * c - 128 * j
                    c0 = max(0, -dlt)
                    x0 = min(dlt, 1664) + 384
                    extras = [(self.anti.ap, Hs.ap[:, h, x0 + c0:x0 + 512])]
                    if c >= 2:
                        extras.append((eall.ap[:, j * 128:(j + 1) * 128], selT.ap[:, c0:512]))
                    kt.append((KS.ap[hb:hb + 64, j * 128:(j + 1) * 128], extras, V2.ap[:, j, 0, :], c0))
                _attn_job(self, QA.ap[hb:hb + 64, hp, tok], 512, kt, lambda psO, pts, h=h: gated(psO, h, 1, False, False), rb_all, pring)
                kt = []
                for j in range(max(0, 4 * c - 4), 4 * c + 4):
                    dlt = 512 * c - 128 * j
                    c0 = max(0, -dlt)
                    x0 = dlt + 384
                    kt.append((KW.ap[hb:hb + 64, j * 128:(j + 1) * 128], [(self.anti.ap, Hw.ap[:, h, x0 + c0:x0 + 512])], V2.ap[:, j, 1, :], c0))
                _attn_job(self, QA.ap[hb:hb + 64, hp, tok], 512, kt, lambda psO, pts, h=h: gated(psO, h, 2, False, True), rb_all, pring)
            self.dma(self.OS[s][0:2, :, tok].rearrange("k p t -> p k t"), ost.ap, self.bOS[s], [ost.b])
    self.release(m0, [Hs, Hw, CMP, QA, KS, KW, V2] + GAr + fkr + far)
    P.barrier()


Builder.phase_BD = lambda self, l: _phase_causal(self, l, "D")
Builder.phase_BC = lambda self, l: _phase_causal(self, l, "C")
Builder.phase_BB = _phase_BB
Builder.phase_BA = _phase_BA


def build_nc(phases=None, dbg=False, os_input=False):
    nc = bass.Bass("TRN2", target_bir_lowering=False)
    b = Builder(nc, phases, dbg)
    b.os_input = os_input
    b.build()
    return nc, b


def make_in_maps(inputs):
    x = np.ascontiguousarray(np.asarray(inputs["x"], dtype=np.float32))
    common = {n: np.ascontiguousarray(np.asarray(inputs[n], dtype=np.float32)) for n in W_NAMES}
    common.update(CONSTS)
    maps = []
    for c in range(8):
        m = dict(common)
        m["x"] = x[2 * c:2 * c + 2]
        maps.append(m)
    return maps


def kernel(**inputs):
    nc, _ = build_nc()
    res = run_bass_kernel_spmd(nc, make_in_maps(inputs), core_ids=list(range(8)))
    return np.concatenate([r["y"] for r in res.results], axis=0).astype(np.float32)
```

```python
import math
import numpy as np
import concourse.bass as bass
import concourse.mybir as mybir
from concourse.bass_utils import run_bass_kernel_spmd

F32 = mybir.dt.float32
BF16 = mybir.dt.bfloat16
AF = mybir.ActivationFunctionType
ALU = mybir.AluOpType

S = 4096
D = 1024
NB = 2
DEPTH = 2
DFF = 2816
D_IN = 2992
EPS = 1e-6
NEG = -30000.0
TINY = 1e-30
NTC = 8
ENGS = ("pe", "act", "dve", "pool", "sp")


class Sem:
    def __init__(self, handle):
        self.h = handle
        self.count = 0
        self.last_op = None


class Buf:
    def __init__(self, name, dma_sems=None, disjoint=False):
        self.name = name
        self.writers = {}
        self.readers = {}
        self.dma_sems = dma_sems or []
        self.rr = 0
        self.disjoint = disjoint


class Op:
    __slots__ = ("eng", "fn", "deps", "needed", "val", "sem", "is_dma")

    def __init__(self, eng, fn):
        self.eng = eng
        self.fn = fn
        self.deps = []
        self.needed = False
        self.val = None
        self.sem = None
        self.is_dma = False


class Prog:
    def __init__(self, nc):
        self.nc = nc
        self.streams = {e: [] for e in ENGS}
        self.free_sems = []
        self.all_sems = []
        self.eng_sems = {e: Sem(nc.alloc_semaphore(f"eng_{e}")) for e in ENGS if e != "sp"}

    def sem_alloc(self):
        if self.free_sems:
            return self.free_sems.pop()
        s = Sem(self.nc.alloc_semaphore(f"ks{len(self.all_sems)}"))
        self.all_sems.append(s)
        return s

    def buf(self, name, ndma=0, disjoint=False):
        return Buf(name, [self.sem_alloc() for _ in range(ndma)], disjoint)

    def free_buf(self, b):
        self.free_sems.extend(b.dma_sems)
        b.dma_sems = []

    def _track(self, op, reads, writes):
        deps = []
        for b in reads:
            deps.extend(b.writers.values())
        for b in writes:
            if not b.disjoint:
                deps.extend(b.writers.values())
            deps.extend(b.readers.values())
        key = id(op.sem) if op.is_dma else op.eng
        for b in reads:
            b.readers[key] = op
        for b in writes:
            if b.disjoint:
                b.writers[key] = op
            else:
                b.writers = {key: op}
                b.readers = {}
        seen = set()
        for d in deps:
            if d is op or id(d) in seen:
                continue
            seen.add(id(d))
            if op.eng == "pe" and d.eng == "pe" and not d.is_dma and not op.is_dma:
                continue
            op.deps.append(d)
            d.needed = True

    def op(self, eng, fn, reads=(), writes=()):
        o = Op(eng, fn)
        self._track(o, reads, writes)
        self.streams[eng].append(o)
        return o

    def dma(self, fns, dst, reads=(), queue="sp", writes=()):
        if not isinstance(fns, (list, tuple)):
            fns = [fns]
        o = Op(queue, list(fns))
        o.is_dma = True
        s = dst.dma_sems[dst.rr % len(dst.dma_sems)]
        dst.rr += 1
        o.sem = s
        if s.last_op is not None:
            o.deps.append(s.last_op)
        s.count += 16 * len(fns)
        o.val = s.count
        s.last_op = o
        o.needed = True
        self._track(o, reads, [dst] + list(writes))
        self.streams[queue].append(o)
        return o

    def barrier(self):
        lasts = []
        for e in ENGS:
            for o in reversed(self.streams[e]):
                if not o.is_dma and o.fn is not None:
                    lasts.append(o)
                    break
        dmas = [s.last_op for s in self.all_sems if s.last_op is not None]
        for e in ENGS:
            o = Op(e, None)
            for d in lasts + dmas:
                o.deps.append(d)
                d.needed = True
            self.streams[e].append(o)

    def finalize_vals(self):
        for e in ENGS:
            if e == "sp":
                continue
            s = self.eng_sems[e]
            c = 0
            for o in self.streams[e]:
                if o.is_dma or o.fn is None:
                    continue
                if o.needed:
                    c += 1
                    o.val = c
                    o.sem = s

    def emit_stream(self, eng_name, eng):
        seen = {}
        for o in self.streams[eng_name]:
            waits = {}
            for d in o.deps:
                sid = id(d.sem)
                if sid not in waits or waits[sid][1] < d.val:
                    waits[sid] = (d.sem, d.val)
            for sid, (s, v) in waits.items():
                if seen.get(sid, 0) >= v:
                    continue
                seen[sid] = v
                eng.wait_ge(s.h, v)
            if o.fn is None:
                continue
            if o.is_dma:
                for f in o.fn:
                    f(eng).then_inc(o.sem.h, 16)
            else:
                ins = o.fn(eng)
                if o.needed:
                    ins.then_inc(o.sem.h, 1)

    def run_block(self, final_ops=()):
        self.finalize_vals()
        fw = {}
        for o in final_ops:
            if id(o.sem) not in fw or fw[id(o.sem)][1] < o.val:
                fw[id(o.sem)] = (o.sem, o.val)
        with self.nc.Block() as block:
            @block.tensor
            def _(e):
                self.emit_stream("pe", e)

            @block.scalar
            def _(e):
                self.emit_stream("act", e)

            @block.vector
            def _(e):
                self.emit_stream("dve", e)

            @block.gpsimd
            def _(e):
                self.emit_stream("pool", e)

            @block.sync
            def _(e):
                self.emit_stream("sp", e)
                for s, v in fw.values():
                    e.wait_ge(s.h, v)


class Tile:
    def __init__(self, ap, b):
        self.ap = ap
        self.b = b


DT_SIZE = {F32: 4, BF16: 2}


def _bucket(d):
    d = np.maximum(d, 0)
    df = np.maximum(d, 1).astype(np.float32)
    large = 16 + (np.log(df / np.float32(16)) / np.float32(math.log(2048 / 16)) * np.float32(16)).astype(np.int32)
    large = np.minimum(large, 31)
    return np.where(d < 16, d, large)


LA, LW, LB_, LC = 2688, 1536, 1152, 1024
OFF = 384


def _onehot(valid, bidx, L):
    oh = np.zeros((33, L), np.float32)
    idx = np.where(valid, bidx, 32)
    oh[idx, np.arange(L)] = 1.0
    return oh


def make_consts():
    c = {}
    c["c_ident"] = np.eye(128, dtype=np.float32)
    c["c_anti"] = np.eye(128, dtype=np.float32)[::-1].copy()
    i = np.arange(LA) - 511
    c["c_oh_a"] = _onehot(i >= 0, _bucket(i), LA)
    i = np.arange(LW) - 511
    c["c_oh_w"] = _onehot((i >= 0) & (i < 512), _bucket(i), LW)
    i = np.arange(LB_) - 511
    c["c_oh_b"] = np.stack([_onehot((i >= 0) & (i <= 128), _bucket(i * dil), LB_) for dil in (1, 4, 16)])
    i = np.arange(LC) - 511
    c["c_causal"] = np.where(i >= 0, 0.0, NEG).astype(np.float32)[None, :]
    cm = []
    key = {}
    for ct in range(2):
        for qc in range(8):
            cc = ct * 128 + np.arange(128)[:, None]
            t = qc * 512 + np.arange(512)[None, :]
            valid = (cc <= 254) & (16 * cc + 31 <= t)
            if valid.all() or not valid.any():
                key[(ct, qc)] = "all" if valid.all() else "none"
                continue
            key[(ct, qc)] = len(cm)
            cm.append(np.where(valid, 0.0, NEG).astype(np.float32))
    c["c_cmpmask"] = np.stack(cm)
    c["c_eall"] = (np.arange(S)[None, :] // 64 == np.arange(64)[:, None]).astype(np.float32)
    t = np.arange(S)
    cur = (t // 64)[:, None]
    j = np.arange(64)[None, :]
    add = np.zeros((S, 64), np.float32)
    keep = np.ones((S, 64), np.float32)
    fut = j > cur
    add[fut] = -1e9
    keep[fut] = 0
    for cond, val in ((j == cur - 1, 1e9), (j == cur, 2e9), (j == 0, 3e9)):
        cond = np.broadcast_to(cond, (S, 64))
        add[cond] = val
        keep[cond] = 0
    c["c_fkeep"] = keep.reshape(32, 128, 64).transpose(1, 0, 2).reshape(128, 32 * 64).copy()
    c["c_fadd"] = add.reshape(32, 128, 64).transpose(1, 0, 2).reshape(128, 32 * 64).copy()
    cs = np.arange(256) * 16
    ce = cs + 31
    ss = np.arange(64) * 64
    ov = ((cs[:, None] <= ss[None, :] + 63) & (ce[:, None] >= ss[None, :])).astype(np.float32)
    ov[255] = 0
    ovx = np.zeros((256, 128), np.float32)
    ovx[:, :64] = ov
    ovx[:, 64] = 1.0
    c["c_ovx"] = ovx.reshape(2, 128, 128)
    inv = (10000.0 ** (-np.arange(0, 32, 2, dtype=np.float32) / 32)).astype(np.float32)
    ang = np.arange(S, dtype=np.float32)[None, :] * inv[:, None]
    cos, sin = np.cos(ang).astype(np.float32), np.sin(ang).astype(np.float32)
    sc = np.float32(96 ** -0.5)
    rq = np.zeros((4, 96, S), np.float32)
    rq[0, 0:64] = sc
    rq[0, 64:80] = sc * cos
    rq[0, 80:96] = sc * cos
    rq[1, 64:80] = sc * sin
    rq[1, 80:96] = sc * sin
    rq[2, 64:80] = cos
    rq[2, 80:96] = cos
    rq[3, 64:80] = sin
    rq[3, 80:96] = sin
    c["c_rope"] = rq
    return c, key


CONSTS, CMPKEY = make_consts()

W_NAMES = ["rel_bias_table", "norm_attn_pre", "norm_attn_post", "norm_ffn_pre", "norm_ffn_post", "w_in", "nsa_cmp_pos",
           "nsa_phi_k_w1", "nsa_phi_k_w2", "nsa_phi_v_w1", "nsa_phi_v_w2", "fox_forget_bias", "mla_q_norm",
           "mla_kv_norm", "mla_w_uq", "mla_w_ukv", "w_branch_a", "w_branch_b", "w_branch_c", "w_branch_d",
           "w_merge_gate", "w_o", "ffn_w_up", "ffn_conv_w", "ffn_conv_b", "ffn_w_down"]
W_SHAPES = {
    "rel_bias_table": (32, 10), "norm_attn_pre": (2, 1024), "norm_attn_post": (2, 1024), "norm_ffn_pre": (2, 1024),
    "norm_ffn_post": (2, 1024), "w_in": (2, 1024, 2992), "nsa_cmp_pos": (2, 32, 64), "nsa_phi_k_w1": (2, 2048, 128),
    "nsa_phi_k_w2": (2, 128, 64), "nsa_phi_v_w1": (2, 2048, 128), "nsa_phi_v_w2": (2, 128, 64),
    "fox_forget_bias": (2, 4), "mla_q_norm": (2, 256), "mla_kv_norm": (2, 128), "mla_w_uq": (2, 256, 384),
    "mla_w_ukv": (2, 128, 512), "w_branch_a": (2, 256, 1024), "w_branch_b": (2, 128, 1024),
    "w_branch_c": (2, 256, 1024), "w_branch_d": (2, 256, 1024), "w_merge_gate": (2, 4, 1024, 1024),
    "w_o": (2, 1024, 1024), "ffn_w_up": (2, 1024, 5632), "ffn_conv_w": (2, 3, 5632), "ffn_conv_b": (2, 5632),
    "ffn_w_down": (2, 2816, 1024),
}

F_QA, F_CMP, F_KSLC, F_KWIN, F_GA, F_QB, F_KB, F_QC, F_KC, F_QD, F_KD, NF = 0, 2, 3, 4, 5, 11, 14, 17, 19, 21, 25, 29
V_SLC, V_WIN, V_B, V_C, V_D, NV = 0, 64, 128, 512, 768, 1024


def dram_ap(t, offset, dims):
    return bass.AP(t.tensor, t.offset + offset, [list(d) for d in dims])


class Builder:
    def __init__(self, nc, phases=None, dbg=False):
        self.nc = nc
        self.P = Prog(nc)
        self.phases = phases
        self.dbg = dbg
        self.uid = 0
        probe = nc.alloc_sbuf_tensor("sb_probe", [128, 8], F32)
        base = nc.lookup_mloc(probe).addr
        self.sb_base = (base + 32 + 63) // 64 * 64
        self.sb_limit = base + 32 + nc.sbuf_bytes_remaining - 64
        self.sb_top = self.sb_base
        self.final_ops = []
        self.ps = []
        for i in range(8):
            t = nc.alloc_psum_tensor(f"psb{i}", [128, 512], F32)
            self.ps.append(Tile(t.ap(), self.P.buf(f"ps{i}")))
        self.ps_rr = 0

    def tile(self, shape, dt, ndma=0, name="t", disjoint=False):
        free = 1
        for s_ in shape[1:]:
            free *= s_
        nbytes = (free * DT_SIZE[dt] + 63) // 64 * 64
        assert self.sb_top + nbytes <= self.sb_limit, f"SBUF overflow allocating {name} {shape}: top={self.sb_top - self.sb_base} need={nbytes}"
        self.uid += 1
        t = self.nc.alloc_sbuf_tensor_at(f"{name}_{self.uid}", list(shape), dt, offset=self.sb_top)
        self.sb_top += nbytes
        return Tile(t.ap(), self.P.buf(f"{name}_{self.uid}", ndma, disjoint))

    def mark(self):
        return (self.sb_top, list(self.P.free_sems), len(self.P.all_sems))

    def release(self, mark, tiles=()):
        for t in tiles:
            self.P.free_buf(t.b)
        self.sb_top = mark[0]

    def next_ps(self, lo=0, hi=8):
        n = hi - lo
        i = lo + (self.ps_rr % n)
        self.ps_rr += 1
        return self.ps[i]

    def dram(self, name, shape, dt, kind="Internal"):
        if self.dbg and kind == "Internal" and name in ("OS0", "HT0", "X10", "X20", "GEXTA"):
            kind = "ExternalOutput"
        return self.nc.dram_tensor(name, list(shape), dt, kind=kind).ap()

    def mm(self, ps, lhsT, rhs, start, stop, reads, writes):
        return self.P.op("pe", lambda e: e.matmul(ps, lhsT=lhsT, rhs=rhs, start=start, stop=stop), reads, writes)

    def tr(self, ps, in_, ident, reads, writes):
        return self.P.op("pe", lambda e: e.transpose(out=ps, in_=in_, identity=ident), reads, writes)

    def act(self, out, in_, func, reads, writes, bias=None, scale=None, accum=None):
        kw = {}
        if bias is not None:
            kw["bias"] = bias
        if scale is not None:
            kw["scale"] = scale
        if accum is not None:
            kw["accum_out"] = accum
        return self.P.op("act", lambda e: e.activation(out=out, in_=in_, func=func, **kw), reads, writes)

    def tt(self, eng, out, in0, in1, op, reads, writes):
        return self.P.op(eng, lambda e: e.tensor_tensor(out=out, in0=in0, in1=in1, op=op), reads, writes)

    def ts(self, eng, out, in0, s1, op0, reads, writes, s2=None, op1=None):
        if op1 is None:
            return self.P.op(eng, lambda e: e.tensor_scalar(out=out, in0=in0, scalar1=s1, scalar2=None, op0=op0), reads, writes)
        return self.P.op(eng, lambda e: e.tensor_scalar(out=out, in0=in0, scalar1=s1, scalar2=s2, op0=op0, op1=op1), reads, writes)

    def stt(self, eng, out, in0, scalar, in1, op0, op1, reads, writes):
        return self.P.op(eng, lambda e: e.scalar_tensor_tensor(out=out, in0=in0, scalar=scalar, in1=in1, op0=op0, op1=op1), reads, writes)

    def cp(self, eng, out, in_, reads, writes):
        if eng == "act":
            return self.P.op("act", lambda e: e.copy(out=out, in_=in_), reads, writes)
        return self.P.op(eng, lambda e: e.tensor_copy(out=out, in_=in_), reads, writes)

    def memset(self, eng, ap, val, writes):
        return self.P.op(eng, lambda e: e.memset(ap, val), [], writes)

    def recip(self, out, in_, reads, writes):
        return self.P.op("dve", lambda e: e.reciprocal(out=out, in_=in_), reads, writes)

    def dma(self, out, in_, dst, reads=(), queue="sp", writes=()):
        return self.P.dma(lambda e: e.dma_start(out=out, in_=in_), dst, reads, queue, writes)

    def rstd_from_ss(self, rs, ss, n, reads_writes):
        self.ts("dve", rs, ss, 1.0 / n, ALU.mult, reads_writes, reads_writes, s2=EPS, op1=ALU.add)
        self.act(rs, rs, AF.Sqrt, reads_writes, reads_writes)
        self.recip(rs, rs, reads_writes, reads_writes)

    def wload(self, dst_ap, src_ap, dst_buf, shape, post=None):
        st = self.wstage[self.wstage_rr % len(self.wstage)]
        self.wstage_rr += 1
        free = 1
        for s_ in shape[1:]:
            free *= s_
        assert free <= 1024
        view = st.ap[0:shape[0], 0:free]
        if len(shape) == 3:
            view = view.rearrange("p (a b) -> p a b", a=shape[1])
        self.dma(view, src_ap, st.b)
        if post is None:
            eng = ("dve", "act")[self.wstage_rr % 2] if dst_ap.base_partition() == 0 else "dve"
            self.cp(eng, dst_ap, view, [st.b], [dst_buf])
        else:
            post(view, st.b)

    def build(self):
        nc, P = self.nc, self.P
        self.x_in = nc.dram_tensor("x", [NB, S, D], F32, kind="ExternalInput").ap()
        self.y_out = nc.dram_tensor("y", [NB, S, D], F32, kind="ExternalOutput").ap()
        self.w = {n: nc.dram_tensor(n, list(W_SHAPES[n]), F32, kind="ExternalInput").ap() for n in W_NAMES}
        self.c = {n: nc.dram_tensor(n, list(v.shape), F32, kind="ExternalInput").ap() for n, v in CONSTS.items()}
        self.FS = [self.dram(f"FS{s}", [NF, 128, S], BF16) for s in range(NB)]
        self.FF = [self.dram(f"FF{s}", [4, S], F32) for s in range(NB)]
        self.VS = [self.dram(f"VS{s}", [S, NV], BF16) for s in range(NB)]
        self.HT = [self.dram(f"HT{s}", [8, 128, S], BF16) for s in range(NB)]
        self.OS = [(self.nc.dram_tensor(f"OS{s}", [7, 128, S], BF16, kind="ExternalInput").ap() if getattr(self, "os_input", False) else self.dram(f"OS{s}", [7, 128, S], BF16)) for s in range(NB)]
        self.X1 = [self.dram(f"X1{s}", [S, D], F32) for s in range(NB)]
        self.X2 = [self.dram(f"X2{s}", [S, D], F32) for s in range(NB)]
        self.AUG = [self.dram(f"AUG{s}", [2, 4, 6, S], BF16) for s in range(NB)]
        self.GA_ = self.dram("GEXTA", [4, LA], BF16)
        self.GW_ = self.dram("GEXTW", [4, LW], BF16)
        self.GB_ = self.dram("GEXTB", [6, LB_], BF16)
        self.GC_ = self.dram("GEXTC", [1, LC], BF16)
        nd = 4
        self.bFS = [P.buf(f"FS{s}", nd, True) for s in range(NB)]
        self.bFF = [P.buf(f"FF{s}", 1, True) for s in range(NB)]
        self.bVS = [P.buf(f"VS{s}", nd, True) for s in range(NB)]
        self.bHT = [P.buf(f"HT{s}", nd, True) for s in range(NB)]
        self.bOS = [P.buf(f"OS{s}", nd, True) for s in range(NB)]
        self.bX1 = [P.buf(f"X1{s}", nd, True) for s in range(NB)]
        self.bX2 = [P.buf(f"X2{s}", nd, True) for s in range(NB)]
        self.bAUG = [P.buf(f"AUG{s}", 1, True) for s in range(NB)]
        self.bG = P.buf("GEXT", 1, True)
        self.bY = P.buf("Y", nd, True)

        self.ident = self.tile([128, 128], BF16, name="ident")
        self.anti = self.tile([128, 128], BF16, name="anti")
        self.ones = self.tile([128, 128], BF16, name="ones")
        self.wstage = [self.tile([128, 1024], F32, 1, name="wst") for _ in range(2)]
        self.wstage_rr = 0
        self.wload(self.ident.ap, self.c["c_ident"], self.ident.b, [128, 128])
        self.wload(self.anti.ap, self.c["c_anti"], self.anti.b, [128, 128])
        self.memset("pool", self.ones.ap, 1.0, [self.ones.b])
        self.prologue_tables()
        P.barrier()
        for l in range(DEPTH):
            if self.want("A"):
                self.phase_A(l)
            if self.want("BC"):
                self.phase_BC(l)
            if self.want("BD"):
                self.phase_BD(l)
            if self.want("BB"):
                self.phase_BB(l)
            if self.want("BA"):
                self.phase_BA(l)
            if self.want("C1"):
                self.phase_C1(l)
            if self.want("C2"):
                self.phase_C2(l)
            if self.phases is not None and self.phases.get("layers", DEPTH) <= l + 1:
                break
        P.run_block(self.final_ops)

    def limit(self, chunks, key="nchunks"):
        if self.phases is not None and key in self.phases:
            return chunks[:self.phases[key]]
        return chunks

    def want(self, ph):
        return self.phases is None or ph in self.phases.get("run", ())

    def prologue_tables(self):
        m = self.mark()
        tabf = self.tile([33, 10], F32, 1, name="tabf")
        tab = self.tile([33, 10], BF16, name="tab")
        self.memset("pool", tabf.ap, NEG, [tabf.b])
        self.dma(tabf.ap[0:32, :], self.w["rel_bias_table"], tabf.b)
        self.cp("pool", tab.ap, tabf.ap, [tabf.b], [tab.b])
        jobs = [(self.c["c_oh_a"], LA, 0, 4, self.GA_), (self.c["c_oh_w"], LW, 0, 4, self.GW_)]
        for g in range(3):
            jobs.append((self.c["c_oh_b"][g], LB_, 4 + 2 * g, 2, self.GB_[2 * g:2 * g + 2, :]))
        ohf = self.tile([33, LA], F32, 1, name="ohf")
        oh = self.tile([33, LA], BF16, name="oh")
        gs = self.tile([4, LA], BF16, name="gs")
        for (src, L, h0, nh, dst) in jobs:
            self.dma(ohf.ap[:, 0:L], src, ohf.b)
            self.cp("pool", oh.ap[:, 0:L], ohf.ap[:, 0:L], [ohf.b], [oh.b])
            for c0 in range(0, L, 512):
                n = min(512, L - c0)
                ps = self.next_ps()
                self.mm(ps.ap[0:nh, 0:n], tab.ap[:, h0:h0 + nh], oh.ap[:, c0:c0 + n], True, True, [tab.b, oh.b], [ps.b])
                self.cp("dve", gs.ap[0:nh, c0:c0 + n], ps.ap[0:nh, 0:n], [ps.b], [gs.b])
            self.dma(dst, gs.ap[0:nh, 0:L], self.bG, [gs.b])
        cz = self.tile([1, LC], F32, 1, name="cz")
        czb = self.tile([1, LC], BF16, name="czb")
        self.dma(cz.ap, self.c["c_causal"], cz.b)
        self.cp("dve", czb.ap, cz.ap, [cz.b], [czb.b])
        self.dma(self.GC_, czb.ap, self.bG, [czb.b])
        self.release(m, [tabf, ohf, cz])

    def hankel(self, tile_ap, buf, src, row, W):
        in_ = bass.AP(src.tensor, src.offset + row * src.ap[0][0], [[1, 128], [1, W]])
        self.dma(tile_ap, in_, buf, [self.bG])

    def sbv(self, t_ap, off, dims):
        return bass.AP(t_ap.tensor, t_ap.offset + off, [[t_ap.ap[0][0], dims[0]]] + [list(d) for d in dims[1:]])

    def load_pvec(self, dst_ap, dst_buf, src_rows_ap, nrows, identf=None):
        fns = []
        for r in range(nrows):
            in_ = bass.AP(src_rows_ap.tensor, src_rows_ap.offset + r * 128, [[1, 128], [1, 1]])
            fns.append(lambda e, r=r, in_=in_: e.dma_start(out=dst_ap[:, r:r + 1], in_=in_))
        self.P.dma(fns, dst_buf)

    def phase_A(self, l):
        P = self.P
        m0 = self.mark()
        w_in = self.w["w_in"][l]
        NCOLS = 4104
        O_QA, O_CMP, O_KSLC, O_KWIN, O_GA, O_QB, O_KB, O_QC, O_KC, O_QL, O_KVL, O_KR, O_KRP, O_FL, O_T1, O_T2 = (
            0, 256, 384, 512, 640, 1408, 1792, 2176, 2432, 2688, 2944, 3072, 3200, 3328, 3336, 3848)
        Win = self.tile([128, 8, NCOLS], BF16, name="Win")

        def load_in(dst_off, src_col, n, post=None):
            c0 = 0
            while c0 < n:
                k = min(128, n - c0)
                src = w_in[:, src_col + c0: src_col + c0 + k].rearrange("(k p) n -> p k n", p=128)
                self.wload(Win.ap[:, :, dst_off + c0: dst_off + c0 + k], src, Win.b, [128, 8, k], post=post)
                c0 += k

        load_in(O_QA, 0, 384)
        for o_, c_ in ((O_KSLC, 384), (O_KWIN, 512)):
            load_in(o_, c_, 64)
            load_in(o_ + 64, c_, 64)

        def post_gates(view, sbuf):
            for h in range(4):
                for b in range(3):
                    gc = h * 3 + b
                    off = O_GA + ((h // 2) * 3 + b) * 128 + (h % 2) * 64
                    src = self.sbv(view, gc, [128, [12, 8], [0, 64]])
                    self.cp("pool", Win.ap[:, :, off:off + 64], src, [sbuf], [Win.b])
        load_in(0, 640, 12, post=post_gates)
        load_in(O_QB, 652, 384)
        load_in(O_KB, 1036, 384)
        load_in(O_QC, 1804, 256)
        load_in(O_KC, 2060, 256)
        load_in(O_QL, 2576, 256)
        load_in(O_KVL, 2832, 128)
        self.memset("pool", Win.ap[:, :, O_KR:O_KR + 256], 0.0, [Win.b])

        def post_kr(view, sbuf):
            self.cp("pool", Win.ap[:, :, O_KR + 64:O_KR + 96], view, [sbuf], [Win.b])
            self.ts("pool", Win.ap[:, :, O_KRP + 64:O_KRP + 80], view[:, :, 16:32], -1.0, ALU.mult, [sbuf], [Win.b])
            self.cp("pool", Win.ap[:, :, O_KRP + 80:O_KRP + 96], view[:, :, 0:16], [sbuf], [Win.b])
        load_in(0, 2960, 32, post=post_kr)
        load_in(O_FL, 2572, 4)
        load_in(O_T1, 448, 64)
        load_in(O_T1 + 64, 576, 64)
        load_in(O_T1 + 128, 1420, 384)
        load_in(O_T2, 2316, 256)

        gq = self.tile([128, 2], F32, 1, name="gq")
        gkv = self.tile([128, 1], F32, 1, name="gkv")
        self.load_pvec(gq.ap, gq.b, self.w["mla_q_norm"][l].rearrange("(k p) -> k p", p=128), 2)
        self.load_pvec(gkv.ap, gkv.b, self.w["mla_kv_norm"][l].rearrange("(k p) -> k p", p=128), 1)
        WQ = self.tile([128, 2, 4, 128], BF16, name="WQ")
        WQP = self.tile([128, 2, 4, 128], BF16, name="WQP")
        WK = self.tile([128, 4, 128], BF16, name="WK")
        WV = self.tile([128, 256], BF16, name="WV")
        self.memset("pool", WQP.ap, 0.0, [WQP.b])
        self.memset("pool", WQ.ap, 0.0, [WQ.b])
        self.memset("pool", WK.ap, 0.0, [WK.b])

        def post_uq(view, sbuf):
            for kc in range(2):
                v4 = view[:, kc, :].rearrange("p (h c) -> p h c", h=4)
                self.ts("pool", WQ.ap[:, kc, :, 0:96], v4, gq.ap[:, kc:kc + 1], ALU.mult, [sbuf, gq.b], [WQ.b])
            for kc in range(2):
                self.ts("pool", WQP.ap[:, kc, :, 64:80], WQ.ap[:, kc, :, 80:96], -1.0, ALU.mult, [WQ.b], [WQP.b])
                self.cp("pool", WQP.ap[:, kc, :, 80:96], WQ.ap[:, kc, :, 64:80], [WQ.b], [WQP.b])
        self.wload(None, self.w["mla_w_uq"][l].rearrange("(k p) n -> p k n", p=128), None, [128, 2, 384], post=post_uq)

        def post_ukv(view, sbuf):
            v4 = view.rearrange("p (h c) -> p h c", h=4)
            self.ts("pool", WK.ap[:, :, 0:64], v4[:, :, 0:64], gkv.ap[:, 0:1], ALU.mult, [sbuf, gkv.b], [WK.b])
            self.ts("pool", WV.ap.rearrange("p (h c) -> p h c", h=4), v4[:, :, 64:128], gkv.ap[:, 0:1], ALU.mult, [sbuf, gkv.b], [WV.b])
        self.wload(None, self.w["mla_w_ukv"][l], None, [128, 512], post=post_ukv)

        gain = self.tile([128, D], F32, 1, name="gain")
        g_src = self.w["norm_attn_pre"][l]
        self.dma(gain.ap, bass.AP(g_src.tensor, g_src.offset, [[0, 128], [1, D]]), gain.b)

        astep = 9 if self.phases is None else self.phases.get("astep", 9)
        xs = [self.tile([128, 4, D], F32, 1, name="xc") for _ in range(2)]
        hTs = [self.tile([128, 8, 512], BF16, name="hT") for _ in range(2)]
        hn = [self.tile([128, D], BF16, name="hn") for _ in range(2)]
        junk = self.tile([128, D], BF16, name="junk")
        stF = [self.tile([128, 8, 512], BF16, name="stF") for _ in range(2)]
        stT = self.tile([128, 4, NV], BF16, name="stT")
        stFF = self.tile([4, 512], F32, name="stFF")
        rope = self.tile([96, 4, 512], F32, 1, name="rope")
        qlT = self.tile([128, 2, 512], BF16, name="qlT")
        sqq = self.tile([128, 2, 512], BF16, name="sqq")
        kvT = self.tile([128, 512], BF16, name="kvT")
        kvf = self.tile([128, 512], F32, name="kvf")
        sqkv = self.tile([128, 512], BF16, name="sqkv")
        rq = self.tile([128, 512], F32, name="rq")
        rkv = self.tile([128, 512], F32, name="rkv")
        t1 = self.tile([128, 512], F32, name="t1")
        t2 = self.tile([128, 512], F32, name="t2")
        kr = self.tile([128, 512], BF16, name="kr")
        stat = self.tile([128, 16], F32, name="stat")
        stF_rr = [0]

        def x_src(s, tc):
            src = self.x_in[s] if l == 0 else self.X2[s]
            return src[tc * 512:(tc + 1) * 512, :].rearrange("(j p) d -> p j d", p=128)

        chunks = self.limit([(s, tc) for s in range(NB) for tc in range(NTC)])
        if astep < 1:
            chunks = []
        if chunks:
            pass
        if chunks:
            self.dma(xs[0].ap, x_src(*chunks[0]), xs[0].b, [] if l == 0 else [self.bX2[chunks[0][0]]])
        evac_rr = [0]

        def evac(out, ps, reads, writes, scale=None, func=None):
            if func is not None:
                return self.act(out, ps, func, reads, writes, scale=scale)
            evac_rr[0] += 1
            if evac_rr[0] % 2:
                if scale is None:
                    return self.cp("act", out, ps, reads, writes)
                return self.act(out, ps, AF.Copy, reads, writes, scale=scale)
            if scale is None:
                return self.cp("dve", out, ps, reads, writes)
            return self.ts("dve", out, ps, scale, ALU.mult, reads, writes)

        for ci, (s, tc) in enumerate(chunks):
            xc = xs[ci % 2]
            hT = hTs[ci % 2]
            if ci + 1 < len(chunks):
                s2, tc2 = chunks[ci + 1]
                self.dma(xs[(ci + 1) % 2].ap, x_src(s2, tc2), xs[(ci + 1) % 2].b, [] if l == 0 else [self.bX2[s2]])
            tok = slice(tc * 512, (tc + 1) * 512)
            self.dma(rope.ap, self.c["c_rope"][:, :, tok].rearrange("a r t -> r a t"), rope.b)
            self.memset("dve", stat.ap[:, 0:4], 0.0, [stat.b])
            for j in range(4):
                hj = hn[j % 2]
                self.act(junk.ap, xc.ap[:, j, :], AF.Square, [xc.b], [junk.b, stat.b], accum=stat.ap[:, j:j + 1])
            self.rstd_from_ss(stat.ap[:, 4:8], stat.ap[:, 0:4], D, [stat.b])
            for j in range(4):
                hj = hn[j % 2]
                self.stt("dve", hj.ap, xc.ap[:, j, :], stat.ap[:, 4 + j:5 + j], gain.ap, ALU.mult, ALU.mult, [xc.b, stat.b, gain.b], [hj.b])
                ps = self.next_ps()
                psb = ps.ap.bitcast(BF16).rearrange("p (k t) -> p k t", k=8)
                for kc in range(8):
                    self.tr(psb[:, kc, :], hj.ap[:, kc * 128:(kc + 1) * 128], self.ident.ap, [hj.b, self.ident.b], [ps.b])
                evac(hT.ap[:, :, j * 128:(j + 1) * 128], psb, [ps.b], [hT.b])
            self.dma(self.HT[s][:, :, tok].rearrange("k p t -> p k t"), hT.ap, self.bHT[s], [hT.b])
            if astep < 2:
                continue

            def fproj(off, M, dst_ap, dst_b, scale=None, func=None):
                ps = self.next_ps()
                for kc in range(8):
                    self.mm(ps.ap[0:M, :], Win.ap[:, kc, off:off + M], hT.ap[:, kc, :], kc == 0, kc == 7, [Win.b, hT.b], [ps.b])
                evac(dst_ap, ps.ap[0:M, :], [ps.b], [dst_b], scale=scale, func=func)
                return ps

            def fgroup(specs, f0):
                st = stF[stF_rr[0] % 2]
                stF_rr[0] += 1
                for i, (off, scale, func) in enumerate(specs):
                    fproj(off, 128, st.ap[:, i, :], st.b, scale, func)
                n = len(specs)
                self.dma(self.FS[s][f0:f0 + n, :, tok].rearrange("n p t -> p n t"), st.ap[:, 0:n, :], self.bFS[s], [st.b])

            fgroup([(O_QA, 0.125, None), (O_QA + 128, 0.125, None), (O_CMP, None, None), (O_KSLC, None, None), (O_KWIN, None, None)], F_QA)
            fgroup([(O_GA + 128 * i, None, AF.Sigmoid) for i in range(6)], F_GA)
            fgroup([(O_QB + 128 * i, 0.125, None) for i in range(3)] + [(O_KB + 128 * i, None, None) for i in range(3)], F_QB)
            fgroup([(O_QC + 128 * i, 0.125, None) for i in range(2)] + [(O_KC + 128 * i, None, None) for i in range(2)], F_QC)
            ps = self.next_ps()
            for kc in range(8):
                self.mm(ps.ap[0:4, :], Win.ap[:, kc, O_FL:O_FL + 4], hT.ap[:, kc, :], kc == 0, kc == 7, [Win.b, hT.b], [ps.b])
            self.cp("dve", stFF.ap, ps.ap[0:4, :], [ps.b], [stFF.b])
            self.dma(self.FF[s][:, tok], stFF.ap, self.bFF[s], [stFF.b])
            if astep < 3:
                continue

            for j in range(4):
                for (off, n, c0) in ((O_T1, 512, 0), (O_T2, 256, 512)):
                    ps = self.next_ps()
                    for kc in range(8):
                        self.mm(ps.ap[:, 0:n], hT.ap[:, kc, j * 128:(j + 1) * 128], Win.ap[:, kc, off:off + n], kc == 0, kc == 7, [Win.b, hT.b], [ps.b])
                    evac(stT.ap[:, j, c0:c0 + n], ps.ap[:, 0:n], [ps.b], [stT.b])

            if astep < 4:
                continue
            for i in range(2):
                ps = self.next_ps()
                for kc in range(8):
                    self.mm(ps.ap, Win.ap[:, kc, O_QL + 128 * i:O_QL + 128 * (i + 1)], hT.ap[:, kc, :], kc == 0, kc == 7, [Win.b, hT.b], [ps.b])
                self.cp("dve", qlT.ap[:, i, :], ps.ap, [ps.b], [qlT.b])
                self.act(sqq.ap[:, i, :], qlT.ap[:, i, :], AF.Square, [qlT.b], [sqq.b])
            ps = self.next_ps()
            for kc in range(8):
                self.mm(ps.ap, Win.ap[:, kc, O_KVL:O_KVL + 128], hT.ap[:, kc, :], kc == 0, kc == 7, [Win.b, hT.b], [ps.b])
            self.cp("dve", kvf.ap, ps.ap, [ps.b], [kvf.b])
            self.act(sqkv.ap, kvf.ap, AF.Square, [kvf.b], [sqkv.b])
            if astep < 3.5:
                continue
            ps = self.next_ps()
            for i in range(2):
                self.mm(ps.ap, self.ones.ap, sqq.ap[:, i, :], i == 0, i == 1, [self.ones.b, sqq.b], [ps.b])
            self.ts("dve", rq.ap, ps.ap, 1.0 / 256, ALU.mult, [ps.b], [rq.b], s2=EPS, op1=ALU.add)
            self.act(rq.ap, rq.ap, AF.Sqrt, [rq.b], [rq.b])
            self.recip(rq.ap, rq.ap, [rq.b], [rq.b])
            ps = self.next_ps()
            self.mm(ps.ap, self.ones.ap, sqkv.ap, True, True, [self.ones.b, sqkv.b], [ps.b])
            self.ts("dve", rkv.ap, ps.ap, 1.0 / 128, ALU.mult, [ps.b], [rkv.b], s2=EPS, op1=ALU.add)
            self.act(rkv.ap, rkv.ap, AF.Sqrt, [rkv.b], [rkv.b])
            self.recip(rkv.ap, rkv.ap, [rkv.b], [rkv.b])
            self.tt("dve", kvT.ap, kvf.ap, rkv.ap, ALU.mult, [kvf.b, rkv.b], [kvT.b])
            if astep < 5:
                continue
            var = 0 if self.phases is None else self.phases.get("var", 0)
            if var == 1:
                evac_rr[0] = 1
            psa = fproj(O_KR, 128, t1.ap, t1.b)
            if var == 1:
                evac_rr[0] = 1
            psb_ = fproj(O_KRP, 128, t2.ap, t2.b)
            if var == 2:
                continue
            self.tt("dve", t1.ap[64:96, :], t1.ap[64:96, :], rope.ap[64:96, 2, :], ALU.mult, [t1.b, rope.b], [t1.b])
            self.tt("dve", t2.ap[64:96, :], t2.ap[64:96, :], rope.ap[64:96, 3, :], ALU.mult, [t2.b, rope.b], [t2.b])
            self.tt("dve", kr.ap[64:96, :], t1.ap[64:96, :], t2.ap[64:96, :], ALU.add, [t1.b, t2.b], [kr.b])
            if astep < 5.1:
                continue
            stq = stF[stF_rr[0] % 2]
            stF_rr[0] += 1
            for h in range(4):
                ps1 = self.next_ps()
                for kc in range(2):
                    self.mm(ps1.ap, WQ.ap[:, kc, h, :], qlT.ap[:, kc, :], kc == 0, kc == 1, [WQ.b, qlT.b], [ps1.b])
                ps2 = self.next_ps()
                for kc in range(2):
                    self.mm(ps2.ap, WQP.ap[:, kc, h, :], qlT.ap[:, kc, :], kc == 0, kc == 1, [WQP.b, qlT.b], [ps2.b])
                self.tt("dve", t1.ap[0:96, :], ps1.ap[0:96, :], rope.ap[:, 0, :], ALU.mult, [ps1.b, rope.b], [t1.b])
                self.tt("dve", t2.ap[64:96, :], ps2.ap[64:96, :], rope.ap[64:96, 1, :], ALU.mult, [ps2.b, rope.b], [t2.b])
                self.tt("dve", t1.ap[64:96, :], t1.ap[64:96, :], t2.ap[64:96, :], ALU.add, [t1.b, t2.b], [t1.b])
                self.tt("dve", stq.ap[0:96, h, :], t1.ap[0:96, :], rq.ap[0:96, :], ALU.mult, [t1.b, rq.b], [stq.b])
                if astep < 5.2:
                    continue
                psk = self.next_ps()
                self.mm(psk.ap, WK.ap[:, h, :], kvT.ap, True, True, [WK.b, kvT.b], [psk.b])
                self.cp("act", stq.ap[0:64, 4 + h, :], psk.ap[0:64, :], [psk.b], [stq.b])
                self.cp("act", stq.ap[64:96, 4 + h, :], kr.ap[64:96, :], [kr.b], [stq.b])
            if astep < 5.3:
                continue
            self.dma(self.FS[s][F_QD:F_QD + 8, 0:96, tok].rearrange("n p t -> p n t"), stq.ap[0:96, :, :], self.bFS[s], [stq.b])
            if astep < 5.4:
                continue
            for j in range(4):
                ps = self.next_ps()
                self.mm(ps.ap[:, 0:256], kvT.ap[:, j * 128:(j + 1) * 128], WV.ap, True, True, [WV.b, kvT.b], [ps.b])
                self.cp("dve", stT.ap[:, j, V_D:V_D + 256], ps.ap[:, 0:256], [ps.b], [stT.b])
            self.dma(self.VS[s][tok, :].rearrange("(j p) c -> p j c", p=128), stT.ap, self.bVS[s], [stT.b])
        self.release(m0, xs + [gain, rope, gq, gkv])
        P.barrier()

    def load_big(self, dst, src2d, nk, ncol, col0=0, dcol0=0):
        c0 = 0
        step = max(1, 1024 // nk)
        step = min(step, 512)
        while c0 < ncol:
            k = min(step, ncol - c0)
            src = src2d[:, col0 + c0: col0 + c0 + k].rearrange("(k p) n -> p k n", p=128)
            self.wload(dst.ap[:, :, dcol0 + c0: dcol0 + c0 + k], src, dst.b, [128, nk, k])
            c0 += k

    def post_norm_residual(self, pss, xc_ap, xc_b, gain, out_ap, out_b, stat, junk):
        self.memset("dve", stat.ap[:, 0:2], 0.0, [stat.b])
        for n in range(2):
            sl = slice(n * 512, (n + 1) * 512)
            self.cp("act", out_ap[:, sl], pss[n].ap, [pss[n].b], [out_b])
            self.act(junk.ap[:, 0:512], out_ap[:, sl], AF.Square, [out_b], [junk.b, stat.b], accum=stat.ap[:, n:n + 1])
        self.tt("dve", stat.ap[:, 2:3], stat.ap[:, 0:1], stat.ap[:, 1:2], ALU.add, [stat.b], [stat.b])
        self.rstd_from_ss(stat.ap[:, 3:4], stat.ap[:, 2:3], D, [stat.b])
        for n in range(2):
            sl = slice(n * 512, (n + 1) * 512)
            self.stt("dve", out_ap[:, sl], out_ap[:, sl], stat.ap[:, 3:4], gain.ap[:, sl], ALU.mult, ALU.mult, [out_b, stat.b, gain.b], [out_b])
            self.tt("dve", out_ap[:, sl], out_ap[:, sl], xc_ap[:, sl], ALU.add, [out_b, xc_b], [out_b])

    def phase_C1(self, l):
        P = self.P
        m0 = self.mark()
        Wg = [self.tile([128, 8, D], BF16, name=f"Wg{i}") for i in range(4)]
        Wb = self.tile([128, 7, D], BF16, name="Wb")
        Wo = self.tile([128, 8, D], BF16, name="Wo")
        for i in range(4):
            self.load_big(Wg[i], self.w["w_merge_gate"][l, i], 8, D)
        for (nm, k0, nk) in (("w_branch_a", 0, 2), ("w_branch_b", 2, 1), ("w_branch_c", 3, 2), ("w_branch_d", 5, 2)):
            for kk in range(nk):
                for c0 in range(0, D, 512):
                    src = self.w[nm][l][kk * 128:(kk + 1) * 128, c0:c0 + 512]
                    self.wload(Wb.ap[:, k0 + kk, c0:c0 + 512], src, Wb.b, [128, 512])
        self.load_big(Wo, self.w["w_o"][l], 8, D)
        gain = self.tile([128, D], F32, 1, name="gain")
        g_src = self.w["norm_attn_post"][l]
        self.dma(gain.ap, bass.AP(g_src.tensor, g_src.offset, [[0, 128], [1, D]]), gain.b)
        hTs = [self.tile([128, 8, 512], BF16, 1, name="hT") for _ in range(2)]
        oTs = [self.tile([128, 7, 512], BF16, 1, name="oT") for _ in range(2)]
        xs = [self.tile([128, 4, D], F32, 1, name="xc") for _ in range(2)]
        mT = self.tile([128, 8, 512], BF16, name="mT")
        macc = self.tile([128, 512], F32, name="macc")
        sg = [self.tile([128, 512], F32, name="sg") for _ in range(2)]
        tm = [self.tile([128, 512], F32, name="tm") for _ in range(2)]
        xo = [self.tile([128, D], F32, name="xo") for _ in range(2)]
        junk = self.tile([128, 512], BF16, name="junk")
        stat = self.tile([128, 8], F32, name="stat")
        branch_k = [(0, 2), (2, 1), (3, 2), (5, 2)]
        chunks = self.limit([(s, tc) for s in range(NB) for tc in range(NTC)], "nchunks_c")

        def loads(ci):
            s, tc = chunks[ci]
            tok = slice(tc * 512, (tc + 1) * 512)
            self.dma(hTs[ci % 2].ap, self.HT[s][:, :, tok].rearrange("k p t -> p k t"), hTs[ci % 2].b, [self.bHT[s]])
            self.dma(oTs[ci % 2].ap, self.OS[s][:, :, tok].rearrange("k p t -> p k t"), oTs[ci % 2].b, [self.bOS[s]])
            src = self.x_in[s] if l == 0 else self.X2[s]
            self.dma(xs[ci % 2].ap, src[tok, :].rearrange("(j p) d -> p j d", p=128), xs[ci % 2].b, [] if l == 0 else [self.bX2[s]])
        loads(0)
        rr = 0
        for ci, (s, tc) in enumerate(chunks):
            if ci + 1 < len(chunks):
                loads(ci + 1)
            hT, oT, xc = hTs[ci % 2], oTs[ci % 2], xs[ci % 2]
            tok = slice(tc * 512, (tc + 1) * 512)
            for oc in range(8):
                ocs = slice(oc * 128, (oc + 1) * 128)
                for i in range(4):
                    psg = self.next_ps()
                    for kc in range(8):
                        self.mm(psg.ap, Wg[i].ap[:, kc, ocs], hT.ap[:, kc, :], kc == 0, kc == 7, [Wg[i].b, hT.b], [psg.b])
                    psb = self.next_ps()
                    k0, nk = branch_k[i]
                    for kk in range(nk):
                        self.mm(psb.ap, Wb.ap[:, k0 + kk, ocs], oT.ap[:, k0 + kk, :], kk == 0, kk == nk - 1, [Wb.b, oT.b], [psb.b])
                    g = sg[rr % 2]
                    t = tm[rr % 2]
                    rr += 1
                    self.act(g.ap, psg.ap, AF.Sigmoid, [psg.b], [g.b])
                    if i == 0:
                        self.tt("dve", macc.ap, g.ap, psb.ap, ALU.mult, [g.b, psb.b], [macc.b])
                    elif i < 3:
                        self.tt("dve", t.ap, g.ap, psb.ap, ALU.mult, [g.b, psb.b], [t.b])
                        self.tt("dve", macc.ap, macc.ap, t.ap, ALU.add, [macc.b, t.b], [macc.b])
                    else:
                        self.tt("dve", t.ap, g.ap, psb.ap, ALU.mult, [g.b, psb.b], [t.b])
                        self.tt("dve", mT.ap[:, oc, :], macc.ap, t.ap, ALU.add, [macc.b, t.b], [mT.b])
            for j in range(4):
                pss = [self.next_ps(), self.next_ps()]
                for n in range(2):
                    for kc in range(8):
                        self.mm(pss[n].ap, mT.ap[:, kc, j * 128:(j + 1) * 128], Wo.ap[:, kc, n * 512:(n + 1) * 512], kc == 0, kc == 7, [mT.b, Wo.b], [pss[n].b])
                o = xo[j % 2]
                self.post_norm_residual(pss, xc.ap[:, j, :], xc.b, gain, o.ap, o.b, stat, junk)
                self.dma(self.X1[s][tc * 512 + j * 128: tc * 512 + (j + 1) * 128, :], o.ap, self.bX1[s], [o.b])
        self.release(m0, hTs + oTs + xs + [gain])
        P.barrier()

    def phase_C2(self, l):
        P = self.P
        m0 = self.mark()
        TC = 256
        Wup = self.tile([128, 8, 2 * DFF], BF16, name="Wup")
        Wdn = self.tile([128, 22, D], BF16, name="Wdn")
        self.load_big(Wup, self.w["ffn_w_up"][l], 8, 2 * DFF)
        self.load_big(Wdn, self.w["ffn_w_down"][l], 22, D)
        cw = self.tile([128, 4, 44], F32, 1, name="cw")
        for j in range(3):
            self.load_pvec(cw.ap[:, j, :], cw.b, self.w["ffn_conv_w"][l, j].rearrange("(c p) -> c p", p=128), 44)
        self.load_pvec(cw.ap[:, 3, :], cw.b, self.w["ffn_conv_b"][l].rearrange("(c p) -> c p", p=128), 44)
        gpre = self.tile([128, D], F32, 1, name="gpre")
        gpost = self.tile([128, D], F32, 1, name="gpost")
        for t_, nm in ((gpre, "norm_ffn_pre"), (gpost, "norm_ffn_post")):
            g_src = self.w[nm][l]
            self.dma(t_.ap, bass.AP(g_src.tensor, g_src.offset, [[0, 128], [1, D]]), t_.b)
        NJ = TC // 128
        xs = [self.tile([128, NJ, D], F32, 1, name="xc") for _ in range(2)]
        hn = [self.tile([128, D], BF16, name="hn") for _ in range(2)]
        hT = self.tile([128, 8, TC], BF16, name="hT")
        aT = self.tile([128, 22, TC], BF16, name="aT")
        ug = [self.tile([128, TC + 2], F32, name="ug") for _ in range(2)]
        uv = [self.tile([128, TC + 2], F32, name="uv") for _ in range(2)]
        cg = [self.tile([128, TC], F32, name="cg") for _ in range(2)]
        cv = [self.tile([128, TC], F32, name="cv") for _ in range(2)]
        halo = self.tile([128, 44, 2], F32, name="halo")
        tv = self.tile([128, TC], F32, name="tv")
        xo = [self.tile([128, D], F32, name="xo") for _ in range(1)]
        junk = self.tile([128, D], BF16, name="junk")
        stat = self.tile([128, 16], F32, name="stat")
        last = (l == DEPTH - 1)
        chunks = self.limit([(s, tc) for s in range(NB) for tc in range(S // TC)], "nchunks_c2")

        def loads(ci):
            s, tc = chunks[ci]
            self.dma(xs[ci % 2].ap, self.X1[s][tc * TC:(tc + 1) * TC, :].rearrange("(j p) d -> p j d", p=128), xs[ci % 2].b, [self.bX1[s]])
        loads(0)
        rr = 0
        for ci, (s, tc) in enumerate(chunks):
            if ci + 1 < len(chunks):
                loads(ci + 1)
            xc = xs[ci % 2]
            if tc == 0:
                self.memset("pool", halo.ap, 0.0, [halo.b])
            self.memset("dve", stat.ap[:, 0:NJ], 0.0, [stat.b])
            for j in range(NJ):
                self.act(junk.ap, xc.ap[:, j, :], AF.Square, [xc.b], [junk.b, stat.b], accum=stat.ap[:, j:j + 1])
            self.rstd_from_ss(stat.ap[:, 4:4 + NJ], stat.ap[:, 0:NJ], D, [stat.b])
            for j in range(NJ):
                hj = hn[j % 2]
                self.stt("dve", hj.ap, xc.ap[:, j, :], stat.ap[:, 4 + j:5 + j], gpre.ap, ALU.mult, ALU.mult, [xc.b, stat.b, gpre.b], [hj.b])
                ps = self.next_ps()
                psb = ps.ap.bitcast(BF16).rearrange("p (k t) -> p k t", k=8)
                for kc in range(8):
                    self.tr(psb[:, kc, :], hj.ap[:, kc * 128:(kc + 1) * 128], self.ident.ap, [hj.b, self.ident.b], [ps.b])
                self.cp("act", hT.ap[:, :, j * 128:(j + 1) * 128], psb, [ps.b], [hT.b])
            for i in range(22):
                U = []
                for (ch, ubuf) in ((i, ug[rr % 2]), (22 + i, uv[rr % 2])):
                    ps = self.next_ps()
                    for kc in range(8):
                        self.mm(ps.ap[:, 0:TC], Wup.ap[:, kc, ch * 128:(ch + 1) * 128], hT.ap[:, kc, :], kc == 0, kc == 7, [Wup.b, hT.b], [ps.b])
                    self.cp("pool", ubuf.ap[:, 0:2], halo.ap[:, ch, :], [halo.b], [ubuf.b])
                    self.cp("act", ubuf.ap[:, 2:TC + 2], ps.ap[:, 0:TC], [ps.b], [ubuf.b])
                    self.cp("pool", halo.ap[:, ch, :], ubuf.ap[:, TC:TC + 2], [ubuf.b], [halo.b])
                    U.append((ch, ubuf))
                outs = (cg[rr % 2], cv[rr % 2])
                for (ch, ubuf), o, eng in zip(U, outs, ("dve", "dve")):
                    self.ts(eng, o.ap, ubuf.ap[:, 0:TC], cw.ap[:, 0, ch:ch + 1], ALU.mult, [ubuf.b, cw.b], [o.b], s2=cw.ap[:, 3, ch:ch + 1], op1=ALU.add)
                    for k_ in (1, 2):
                        if eng == "dve":
                            self.stt(eng, o.ap, ubuf.ap[:, k_:TC + k_], cw.ap[:, k_, ch:ch + 1], o.ap, ALU.mult, ALU.add, [ubuf.b, cw.b, o.b], [o.b])
                        else:
                            self.ts(eng, tv.ap, ubuf.ap[:, k_:TC + k_], cw.ap[:, k_, ch:ch + 1], ALU.mult, [ubuf.b, cw.b], [tv.b])
                            self.tt(eng, o.ap, o.ap, tv.ap, ALU.add, [o.b, tv.b], [o.b])
                g_, v_ = outs
                self.act(ug[rr % 2].ap[:, 0:TC], g_.ap, AF.Sigmoid, [g_.b], [ug[rr % 2].b])
                self.tt("dve", g_.ap, g_.ap, ug[rr % 2].ap[:, 0:TC], ALU.mult, [g_.b, ug[rr % 2].b], [g_.b])
                self.tt("dve", aT.ap[:, i, :], g_.ap, v_.ap, ALU.mult, [g_.b, v_.b], [aT.b])
                rr += 1
            for j in range(NJ):
                pss = [self.next_ps(), self.next_ps()]
                for n in range(2):
                    for kc in range(22):
                        self.mm(pss[n].ap, aT.ap[:, kc, j * 128:(j + 1) * 128], Wdn.ap[:, kc, n * 512:(n + 1) * 512], kc == 0, kc == 21, [aT.b, Wdn.b], [pss[n].b])
                o = xo[0]
                self.post_norm_residual(pss, xc.ap[:, j, :], xc.b, gpost, o.ap, o.b, stat_c2(stat), junk)
                r0 = tc * TC + j * 128
                if last:
                    op = self.dma(self.y_out[s][r0:r0 + 128, :], o.ap, self.bY, [o.b])
                    self.final_ops.append(op)
                else:
                    self.dma(self.X2[s][r0:r0 + 128, :], o.ap, self.bX2[s], [o.b])
        self.release(m0, xs + [gpre, gpost, cw])
        P.barrier()


class _StatView:
    def __init__(self, ap, b):
        self.ap = ap
        self.b = b


def stat_c2(stat):
    return _StatView(stat.ap[:, 8:16], stat.b)


def _attn_job(self, q_ap, n, ktiles, fin, rbufs, pring):
    psO = self.next_ps(4, 6)
    nk = len(ktiles)
    pend = None
    pts = []

    def pv(idx, pt, v_l, c0):
        self.mm(psO.ap[:, c0:n], v_l, pt.ap[:, c0:n], idx == 0, idx == nk - 1, rbufs + [pt.b], [psO.b])
    for idx, (k_l, extras, v_l, c0) in enumerate(ktiles):
        psS = self.next_ps(0, 4)
        mms = [(k_l, q_ap[:, c0:n])] + list(extras)
        for mi, (l_, r_) in enumerate(mms):
            self.mm(psS.ap[:, c0:n], l_, r_, mi == 0, mi == len(mms) - 1, rbufs, [psS.b])
        pt = pring[self.p_rr % len(pring)]
        self.p_rr += 1
        pts.append(pt)
        self.act(pt.ap[:, c0:n], psS.ap[:, c0:n], AF.Exp, [psS.b], [pt.b])
        if pend is not None:
            pv(*pend)
        pend = (idx, pt, v_l, c0)
    pv(*pend)
    fin(psO, pts)


def _recip_den(self, psO, rc, n):
    r = rc.ap[0:64, 0:n]
    self.ts("dve", r, psO.ap[64:128, 0:n], TINY, ALU.max, [psO.b], [rc.b])
    self.recip(r, r, [rc.b], [rc.b])
    return r


def _load_v(self, V, s, col0, nh):
    self.memset("pool", V.ap[:, :, :, 64:128], 1.0, [V.b])
    src = self.VS[s][:, col0:col0 + nh * 64].rearrange("(j p) (h c) -> p j h c", p=128, h=nh)
    for h in range(nh):
        self.dma(V.ap[:, :, h, 0:64], src[:, :, h, :], V.b, [self.bVS[s]])


def _nqc(self):
    if self.phases is not None and "nqc" in self.phases:
        return self.phases["nqc"]
    return 8


def _nseq(self):
    if self.phases is not None and "nseq" in self.phases:
        return self.phases["nseq"]
    return NB


def _phase_causal(self, l, kind):
    P = self.P
    m0 = self.mark()
    rows = 96 if kind == "D" else 70
    vcol = V_D if kind == "D" else V_C
    os0 = 5 if kind == "D" else 3
    Hc = self.tile([128, 896], BF16, 1, name="Hc")
    self.hankel(Hc.ap, Hc.b, self.GC_, 0, 896)
    pring = [self.tile([128, 512], BF16, name="pt") for _ in range(4)]
    rcs = [self.tile([128, 512], F32, name="rc") for _ in range(2)]
    osts = [self.tile([128, 2, 512], BF16, name="ost") for _ in range(2)]
    QT = [self.tile([rows, S], BF16, 1, name="QT") for _ in range(4)]
    KT = [self.tile([rows, S], BF16, 1, name="KT") for _ in range(4)]
    V = self.tile([128, 32, 4, 128], BF16, 1, name="V")
    extra_tiles = []
    if kind == "C":
        W = 1024
        fx = self.tile([4, W], F32, 1, name="fx")
        e1 = self.tile([4, W], F32, name="e1")
        onesf = self.tile([4, W], F32, name="onesf")
        cn = self.tile([4, W], F32, name="cn")
        t32 = self.tile([4, W], F32, name="t32")
        augk = self.tile([4, 6, W], BF16, name="augk")
        augq = self.tile([4, 6, W], BF16, name="augq")
        carry = self.tile([4, 1], F32, name="carry")
        fb = self.tile([4, 1], F32, 1, name="fb")
        fsrc = self.w["fox_forget_bias"][l]
        self.dma(fb.ap, bass.AP(fsrc.tensor, fsrc.offset, [[1, 4], [1, 1]]), fb.b)
        self.memset("pool", onesf.ap, 1.0, [onesf.b])
        self.memset("pool", augk.ap[:, 0:3, :], 1.0, [augk.b])
        self.memset("pool", augq.ap[:, 3:6, :], 1.0, [augq.b])
        extra_tiles = [fx, fb]
    self.p_rr = 0
    rr = 0
    for s in range(_nseq(self)):
        if kind == "C":
            for k in range(S // W):
                cols = slice(k * W, (k + 1) * W)
                self.dma(fx.ap, self.FF[s][:, cols], fx.b, [self.bFF[s]])
                self.ts("dve", e1.ap, fx.ap, fb.ap[:, 0:1], ALU.add, [fx.b, fb.b], [e1.b])
                self.act(e1.ap, e1.ap, AF.Exp, [e1.b], [e1.b], scale=-1.0)
                self.act(e1.ap, e1.ap, AF.Ln, [e1.b], [e1.b], bias=1.0)
                init = 0.0 if k == 0 else carry.ap[:, 0:1]
                self.P.op("dve", lambda e, init=init: e.tensor_tensor_scan(out=cn.ap, data0=onesf.ap, data1=e1.ap, initial=init, op0=ALU.mult, op1=ALU.add),
                          [onesf.b, e1.b, carry.b], [cn.b])
                self.cp("dve", carry.ap, cn.ap[:, W - 1:W], [cn.b], [carry.b])
                self.cp("dve", augk.ap[:, 3, :], cn.ap, [cn.b], [augk.b])
                self.cp("dve", t32.ap, augk.ap[:, 3, :], [augk.b], [t32.b])
                self.tt("dve", e1.ap, cn.ap, t32.ap, ALU.subtract, [cn.b, t32.b], [e1.b])
                self.cp("dve", augk.ap[:, 4, :], e1.ap, [e1.b], [augk.b])
                self.cp("dve", t32.ap, augk.ap[:, 4, :], [augk.b], [t32.b])
                self.tt("dve", e1.ap, e1.ap, t32.ap, ALU.subtract, [e1.b, t32.b], [e1.b])
                self.cp("dve", augk.ap[:, 5, :], e1.ap, [e1.b], [augk.b])
                self.ts("dve", augq.ap[:, 0:3, :], augk.ap[:, 3:6, :], -1.0, ALU.mult, [augk.b], [augq.b])
                self.dma(self.AUG[s][0][:, :, cols], augq.ap, self.bAUG[s], [augq.b])
                self.dma(self.AUG[s][1][:, :, cols], augk.ap, self.bAUG[s], [augk.b])
        for h in range(4):
            if kind == "D":
                self.dma(QT[h].ap, self.FS[s][F_QD + h, 0:96, :], QT[h].b, [self.bFS[s]])
                self.dma(KT[h].ap, self.FS[s][F_KD + h, 0:96, :], KT[h].b, [self.bFS[s]])
            else:
                rws = slice((h % 2) * 64, (h % 2) * 64 + 64)
                self.dma(QT[h].ap[0:64, :], self.FS[s][F_QC + h // 2, rws, :], QT[h].b, [self.bFS[s]])
                self.dma(QT[h].ap[64:70, :], self.AUG[s][0, h], QT[h].b, [self.bAUG[s]])
                self.dma(KT[h].ap[0:64, :], self.FS[s][F_KC + h // 2, rws, :], KT[h].b, [self.bFS[s]])
                self.dma(KT[h].ap[64:70, :], self.AUG[s][1, h], KT[h].b, [self.bAUG[s]])
        _load_v(self, V, s, vcol, 4)
        for c in range(_nqc(self)):
            ost = osts[c % 2]
            for h in range(4):
                rc = rcs[rr % 2]
                rr += 1
                kt = []
                for j in range(4 * c + 4):
                    c0 = max(0, 128 * (j - 4 * c))
                    extras = [(self.anti.ap, Hc.ap[:, 384:384 + 512 - c0])] if j >= 4 * c else []
                    kt.append((KT[h].ap[:, j * 128:(j + 1) * 128], extras, V.ap[:, j, h, :], c0))
                out_ap = ost.ap[(h % 2) * 64:(h % 2) * 64 + 64, h // 2, :]

                def fin(psO, pts, out_ap=out_ap, rc=rc, ost=ost):
                    r = _recip_den(self, psO, rc, 512)
                    self.tt("dve", out_ap, psO.ap[0:64, :], r, ALU.mult, [psO.b, rc.b], [ost.b])
                _attn_job(self, QT[h].ap[:, c * 512:(c + 1) * 512], 512, kt, fin, [QT[h].b, KT[h].b, V.b, Hc.b, self.anti.b], pring)
            self.dma(self.OS[s][os0:os0 + 2, :, c * 512:(c + 1) * 512].rearrange("k p t -> p k t"), ost.ap, self.bOS[s], [ost.b])
    self.release(m0, [Hc, V] + QT + KT + extra_tiles)
    P.barrier()


def _phase_BB(self, l):
    P = self.P
    m0 = self.mark()
    Hb = self.tile([128, 6, 1024], BF16, 1, name="Hb")
    for i in range(6):
        self.hankel(Hb.ap[:, i, :], Hb.b, self.GB_, i, 1024)
    pring = [self.tile([128, 512], BF16, name="pt") for _ in range(4)]
    rcs = [self.tile([128, 512], F32, name="rc") for _ in range(2)]
    osts = [self.tile([128, 512], BF16, name="ost") for _ in range(2)]
    QB = self.tile([128, 3, S], BF16, 1, name="QB")
    KB = self.tile([128, 3, S], BF16, 1, name="KB")
    acc = self.tile([128, 2, S], F32, name="acc")
    Vr = [self.tile([128, 32, 2, 128], BF16, 1, name="Vg") for _ in range(2)]
    self.p_rr = 0
    rr = 0
    for s in range(_nseq(self)):
        self.dma(QB.ap, self.FS[s][F_QB:F_QB + 3, :, :].rearrange("k p t -> p k t"), QB.b, [self.bFS[s]])
        self.dma(KB.ap, self.FS[s][F_KB:F_KB + 3, :, :].rearrange("k p t -> p k t"), KB.b, [self.bFS[s]])
        for g, dil in enumerate((1, 4, 16)):
            Vg = Vr[g % 2]
            nj = 32 // dil
            self.memset("pool", Vg.ap[:, :, :, 64:128], 1.0, [Vg.b])
            for r in range(dil):
                for h in range(2):
                    src = dram_ap(self.VS[s], r * NV + V_B + g * 128 + h * 64, [[dil * NV, 128], [128 * dil * NV, nj], [1, 64]])
                    self.dma(Vg.ap[:, r * nj:(r + 1) * nj, h, 0:64], src, Vg.b, [self.bVS[s]])
            L = S // dil
            n = min(512, L)
            for h in range(2):
                hb = 64 * h
                for r in range(dil):
                    for uq0 in range(0, L, n):
                        kt = []
                        for j in range(uq0 // 128 - 1, (uq0 + n) // 128):
                            if j < 0:
                                continue
                            dlt = uq0 - 128 * j
                            c0 = max(0, -dlt)
                            k_l = self.sbv(KB.ap[hb:hb + 64, g, :], r + dil * 128 * j, [64, [dil, 128]])
                            extras = [(self.anti.ap, Hb.ap[:, 2 * g + h, dlt + 384 + c0:dlt + 384 + n])]
                            kt.append((k_l, extras, Vg.ap[:, r * nj + j, h, :], c0))
                        q_ap = self.sbv(QB.ap[hb:hb + 64, g, :], r + dil * uq0, [64, [dil, n]])
                        accv = self.sbv(acc.ap[:, h, :], r + dil * uq0, [128, [dil, n]])

                        def fin(psO, pts, accv=accv, n=n, g=g):
                            if g == 0:
                                self.cp("act", accv, psO.ap[:, 0:n], [psO.b], [acc.b])
                            else:
                                self.tt("dve", accv, accv, psO.ap[:, 0:n], ALU.add, [psO.b, acc.b], [acc.b])
                        _attn_job(self, q_ap, n, kt, fin, [QB.b, KB.b, Vg.b, Hb.b, self.anti.b], pring)
        for c in range(8):
            ost = osts[c % 2]
            for h in range(2):
                rc = rcs[rr % 2]
                rr += 1
                r_ = rc.ap[0:64, :]
                self.ts("dve", r_, acc.ap[64:128, h, c * 512:(c + 1) * 512], TINY, ALU.max, [acc.b], [rc.b])
                self.recip(r_, r_, [rc.b], [rc.b])
                self.tt("dve", ost.ap[h * 64:h * 64 + 64, :], acc.ap[0:64, h, c * 512:(c + 1) * 512], r_, ALU.mult, [acc.b, rc.b], [ost.b])
            self.dma(self.OS[s][2, :, c * 512:(c + 1) * 512], ost.ap, self.bOS[s], [ost.b])
    self.release(m0, [Hb, QB, KB] + Vr)
    P.barrier()


def _phase_BA(self, l):
    P = self.P
    m0 = self.mark()
    Hs = self.tile([128, 4, 2560], BF16, 1, name="Hs")
    Hw = self.tile([128, 4, 1408], BF16, 1, name="Hw")
    for h in range(4):
        self.hankel(Hs.ap[:, h, :], Hs.b, self.GA_, h, 2560)
        self.hankel(Hw.ap[:, h, :], Hw.b, self.GW_, h, 1408)
    NM = CONSTS["c_cmpmask"].shape[0]
    cmask = self.tile([128, NM, 512], BF16, name="cmask")
    for i in range(NM):
        self.wload(cmask.ap[:, i, :], self.c["c_cmpmask"][i], cmask.b, [128, 512])
    eall = self.tile([64, S], BF16, name="eall")
    for k in range(4):
        self.wload(eall.ap[:, k * 1024:(k + 1) * 1024], self.c["c_eall"][:, k * 1024:(k + 1) * 1024], eall.b, [64, 1024])
    ovx = self.tile([128, 2, 128], BF16, name="ovx")
    for ct in range(2):
        self.wload(ovx.ap[:, ct, :], self.c["c_ovx"][ct], ovx.b, [128, 128])
    w1 = self.tile([128, 32, 128], BF16, name="w1")
    for kv, nm in enumerate(("nsa_phi_k_w1", "nsa_phi_v_w1")):
        src = self.w[nm][l].rearrange("(i d) m -> d i m", d=64)
        for i0 in range(0, 32, 8):
            self.wload(w1.ap[kv * 64:kv * 64 + 64, i0:i0 + 8, :], src[:, i0:i0 + 8, :], w1.b, [64, 8, 128])
    w2 = self.tile([128, 2, 128], BF16, name="w2")
    self.wload(w2.ap[:, 0, 0:64], self.w["nsa_phi_k_w2"][l], w2.b, [128, 64])
    self.wload(w2.ap[:, 0, 64:128], self.w["nsa_phi_k_w2"][l], w2.b, [128, 64])
    self.wload(w2.ap[:, 1, 0:64], self.w["nsa_phi_v_w2"][l], w2.b, [128, 64])
    pp = self.tile([32, 128], BF16, name="pp")
    self.wload(pp.ap[:, 0:64], self.w["nsa_cmp_pos"][l], pp.b, [32, 64])
    self.wload(pp.ap[:, 64:128], self.w["nsa_cmp_pos"][l], pp.b, [32, 64])
    posT = self.tile([128, 32], BF16, name="posT")
    ps = self.next_ps(6, 8)
    psb = ps.ap.bitcast(BF16)
    self.tr(psb[:, 0:32], pp.ap, self.ident.ap[0:32, 0:32], [pp.b, self.ident.b], [ps.b])
    self.cp("dve", posT.ap, psb[:, 0:32], [ps.b], [posT.b])
    posc = self.tile([128, 2], F32, name="posc")
    ps = self.next_ps(6, 8)
    for kv in range(2):
        for i in range(32):
            self.mm(ps.ap[:, kv:kv + 1], w1.ap[kv * 64:kv * 64 + 64, i, :], posT.ap[kv * 64:kv * 64 + 64, i:i + 1], i == 0, i == 31, [w1.b, posT.b], [ps.b])
    self.cp("dve", posc.ap, ps.ap[:, 0:2], [ps.b], [posc.b])

    pring = [self.tile([128, 512], BF16, name="pt") for _ in range(4)]
    rcs = [self.tile([128, 512], F32, name="rc") for _ in range(2)]
    tmps = [self.tile([128, 512], F32, name="tmp") for _ in range(2)]
    osts = [self.tile([128, 2, 512], BF16, name="ost") for _ in range(2)]
    Oacc = self.tile([128, 2, 512], F32, name="Oacc")
    CMP = self.tile([128, S], BF16, 1, name="CMP")
    gk = [self.tile([128, 256], BF16, name="gkv") for _ in range(2)]
    kcT = self.tile([128, 256], BF16, name="kcT")
    Vc = self.tile([128, 2, 128], BF16, name="Vc")
    QA = self.tile([128, 2, S], BF16, 1, name="QA")
    KS = self.tile([128, S], BF16, 1, name="KS")
    KW = self.tile([128, S], BF16, 1, name="KW")
    V2 = self.tile([128, 32, 2, 128], BF16, 1, name="V2")
    GAr = [self.tile([128, 6, 512], BF16, 1, name="GAc") for _ in range(2)]
    fkr = [self.tile([128, 4, 64], F32, 1, name="fk") for _ in range(2)]
    far = [self.tile([128, 4, 64], F32, 1, name="fa") for _ in range(2)]
    impacc = self.tile([128, 4, 64], F32, name="impacc")
    impt = self.tile([128, 4, 64], F32, name="impt")
    rI = self.tile([128, 4], F32, name="rI")
    m8 = self.tile([128, 16], F32, name="m8")
    wk = self.tile([128, 64], F32, name="wk")
    selb = self.tile([128, 4, 64], BF16, name="selb")
    selT = self.tile([64, 512], BF16, name="selT")
    self.memset("pool", gk[0].ap[:, 255:256], 0.0, [gk[0].b])
    self.memset("pool", gk[1].ap[:, 255:256], 0.0, [gk[1].b])
    self.memset("pool", Vc.ap[:, :, 64:128], 1.0, [Vc.b])
    self.p_rr = 0
    rr = 0
    for s in range(_nseq(self)):
        self.dma(CMP.ap, self.FS[s][F_CMP], CMP.b, [self.bFS[s]])
        for kv in range(2):
            ps = self.next_ps(6, 8)
            for i in range(32):
                rhs = self.sbv(CMP.ap[kv * 64:kv * 64 + 64, :], i, [64, [16, 255]])
                self.mm(ps.ap[:, 0:255], w1.ap[kv * 64:kv * 64 + 64, i, :], rhs, i == 0, i == 31, [w1.b, CMP.b], [ps.b])
            self.act(gk[kv].ap[:, 0:255], ps.ap[:, 0:255], AF.Gelu_apprx_tanh, [ps.b, posc.b], [gk[kv].b], bias=posc.ap[:, kv:kv + 1])
        ps = self.next_ps(6, 8)
        self.mm(ps.ap[:, 0:256], w2.ap[:, 0, :], gk[0].ap, True, True, [w2.b, gk[0].b], [ps.b])
        self.cp("dve", kcT.ap, ps.ap[:, 0:256], [ps.b], [kcT.b])
        for ct in range(2):
            ps = self.next_ps(6, 8)
            self.mm(ps.ap[:, 0:64], gk[1].ap[:, ct * 128:(ct + 1) * 128], w2.ap[:, 1, 0:64], True, True, [w2.b, gk[1].b], [ps.b])
            self.cp("dve", Vc.ap[:, ct, 0:64], ps.ap[:, 0:64], [ps.b], [Vc.b])
        self.dma(QA.ap, self.FS[s][F_QA:F_QA + 2, :, :].rearrange("k p t -> p k t"), QA.b, [self.bFS[s]])
        self.dma(KS.ap, self.FS[s][F_KSLC], KS.b, [self.bFS[s]])
        self.dma(KW.ap, self.FS[s][F_KWIN], KW.b, [self.bFS[s]])
        _load_v(self, V2, s, V_SLC, 2)
        for c in range(_nqc(self)):
            tok = slice(c * 512, (c + 1) * 512)
            GAc, fk, fa = GAr[c % 2], fkr[c % 2], far[c % 2]
            ost = osts[c % 2]
            self.dma(GAc.ap, self.FS[s][F_GA:F_GA + 6, :, tok].rearrange("k p t -> p k t"), GAc.b, [self.bFS[s]])
            self.dma(fk.ap, self.c["c_fkeep"][:, c * 256:(c + 1) * 256].rearrange("p (q j) -> p q j", q=4), fk.b)
            self.dma(fa.ap, self.c["c_fadd"][:, c * 256:(c + 1) * 256].rearrange("p (q j) -> p q j", q=4), fa.b)
            rb_all = [QA.b, KS.b, KW.b, V2.b, Hs.b, Hw.b, kcT.b, Vc.b, cmask.b, eall.b, selT.b, self.anti.b, self.ident.b]

            def gated(psO, h, br, first, last):
                hb, hp = 64 * (h % 2), h // 2
                rc, tmp = rcs[h % 2], tmps[h % 2]
                r = _recip_den(self, psO, rc, 512)
                t = tmp.ap[hb:hb + 64, :]
                g_ap = GAc.ap[hb:hb + 64, hp * 3 + br, :]
                o_ap = Oacc.ap[hb:hb + 64, hp, :]
                self.tt("dve", t, psO.ap[0:64, :], r, ALU.mult, [psO.b, rc.b], [tmp.b])
                if first:
                    self.tt("dve", o_ap, t, g_ap, ALU.mult, [tmp.b, GAc.b], [Oacc.b])
                else:
                    self.tt("dve", t, t, g_ap, ALU.mult, [tmp.b, GAc.b], [tmp.b])
                    if last:
                        self.tt("dve", ost.ap[hb:hb + 64, hp, :], o_ap, t, ALU.add, [Oacc.b, tmp.b], [ost.b])
                    else:
                        self.tt("dve", o_ap, o_ap, t, ALU.add, [Oacc.b, tmp.b], [Oacc.b])

            for h in range(4):
                hb, hp = 64 * (h % 2), h // 2
                kt = []
                cts = []
                for ct in range(2):
                    key = CMPKEY[(ct, c)]
                    if key == "none":
                        continue
                    extras = [] if key == "all" else [(self.ident.ap, cmask.ap[:, key, :])]
                    kt.append((kcT.ap[hb:hb + 64, ct * 128:(ct + 1) * 128], extras, Vc.ap[:, ct, :], 0))
                    cts.append(ct)

                def fin_c(psO, pts, h=h, cts=cts):
                    psI = self.next_ps(6, 8)
                    pv_ = psI.ap.rearrange("p (q c) -> p q c", q=4)
                    for qt in range(4):
                        for ii, (ct, pt) in enumerate(zip(cts, pts)):
                            self.mm(pv_[:, qt, 0:65], pt.ap[:, qt * 128:(qt + 1) * 128], ovx.ap[:, ct, 0:65], ii == 0, ii == len(cts) - 1, [pt.b, ovx.b], [psI.b])
                    self.ts("dve", rI.ap, pv_[:, :, 64], TINY, ALU.max, [psI.b], [rI.b])
                    self.recip(rI.ap, rI.ap, [rI.b], [rI.b])
                    rbc = self.sbv(rI.ap, 0, [128, [1, 4], [0, 64]])
                    if h == 0:
                        self.tt("dve", impacc.ap, pv_[:, :, 0:64], rbc, ALU.mult, [psI.b, rI.b], [impacc.b])
                    else:
                        self.tt("dve", impt.ap, pv_[:, :, 0:64], rbc, ALU.mult, [psI.b, rI.b], [impt.b])
                        self.tt("pool", impacc.ap, impacc.ap, impt.ap, ALU.add, [impacc.b, impt.b], [impacc.b])
                    gated(psO, h, 0, True, False)
                _attn_job(self, QA.ap[hb:hb + 64, hp, tok], 512, kt, fin_c, rb_all, pring)
            self.tt("dve", impacc.ap, impacc.ap, fk.ap, ALU.mult, [impacc.b, fk.b], [impacc.b])
            self.tt("dve", impacc.ap, impacc.ap, fa.ap, ALU.add, [impacc.b, fa.b], [impacc.b])
            for qt in range(4):
                iv = impacc.ap[:, qt, :]
                self.P.op("dve", lambda e, iv=iv: e.max(out=m8.ap[:, 0:8], in_=iv), [impacc.b], [m8.b])
                self.P.op("dve", lambda e, iv=iv: e.match_replace(out=wk.ap, in_to_replace=m8.ap[:, 0:8], in_values=iv, imm_value=-3e9), [impacc.b, m8.b], [wk.b])
                self.P.op("dve", lambda e: e.max(out=m8.ap[:, 8:16], in_=wk.ap), [wk.b], [m8.b])
                self.ts("dve", wk.ap, iv, m8.ap[:, 15:16], ALU.is_ge, [impacc.b, m8.b], [wk.b], s2=-NEG, op1=ALU.mult)
                self.ts("dve", selb.ap[:, qt, :], wk.ap, NEG, ALU.add, [wk.b], [selb.b])
            ps = self.next_ps(6, 8)
            psb = ps.ap.bitcast(BF16)
            for qt in range(4):
                self.tr(psb[0:64, qt * 128:(qt + 1) * 128], selb.ap[:, qt, :], self.ident.ap, [selb.b, self.ident.b], [ps.b])
            self.cp("dve", selT.ap, psb[0:64, 0:512], [ps.b], [selT.b])
            for h in range(4):
                hb, hp = 64 * (h % 2), h // 2
                kt = []
                for j in range(4 * c + 4):
                    dlt = 512 * c - 128 * j
                    c0 = max(0, -dlt)
                    x0 = min(dlt, 1664) + 384
                    extras = [(self.anti.ap, Hs.ap[:, h, x0 + c0:x0 + 512])]
                    if c >= 2:
                        extras.append((eall.ap[:, j * 128:(j + 1) * 128], selT.ap[:, c0:512]))
                    kt.append((KS.ap[hb:hb + 64, j * 128:(j + 1) * 128], extras, V2.ap[:, j, 0, :], c0))
                _attn_job(self, QA.ap[hb:hb + 64, hp, tok], 512, kt, lambda psO, pts, h=h: gated(psO, h, 1, False, False), rb_all, pring)
                kt = []
                for j in range(max(0, 4 * c - 4), 4 * c + 4):
                    dlt = 512 * c - 128 * j
                    c0 = max(0, -dlt)
                    x0 = dlt + 384
                    kt.append((KW.ap[hb:hb + 64, j * 128:(j + 1) * 128], [(self.anti.ap, Hw.ap[:, h, x0 + c0:x0 + 512])], V2.ap[:, j, 1, :], c0))
                _attn_job(self, QA.ap[hb:hb + 64, hp, tok], 512, kt, lambda psO, pts, h=h: gated(psO, h, 2, False, True), rb_all, pring)
            self.dma(self.OS[s][0:2, :, tok].rearrange("k p t -> p k t"), ost.ap, self.bOS[s], [ost.b])
    self.release(m0, [Hs, Hw, CMP, QA, KS, KW, V2] + GAr + fkr + far)
    P.barrier()


Builder.phase_BD = lambda self, l: _phase_causal(self, l, "D")
Builder.phase_BC = lambda self, l: _phase_causal(self, l, "C")
Builder.phase_BB = _phase_BB
Builder.phase_BA = _phase_BA


def build_nc(phases=None, dbg=False, os_input=False):
    nc = bass.Bass("TRN2", target_bir_lowering=False)
    b = Builder(nc, phases, dbg)
    b.os_input = os_input
    b.build()
    return nc, b


def make_in_maps(inputs):
    x = np.ascontiguousarray(np.asarray(inputs["x"], dtype=np.float32))
    common = {n: np.ascontiguousarray(np.asarray(inputs[n], dtype=np.float32)) for n in W_NAMES}
    common.update(CONSTS)
    maps = []
    for c in range(8):
        m = dict(common)
        m["x"] = x[2 * c:2 * c + 2]
        maps.append(m)
    return maps


def kernel(**inputs):
    nc, _ = build_nc()
    res = run_bass_kernel_spmd(nc, make_in_maps(inputs), core_ids=list(range(8)))
    return np.concatenate([r["y"] for r in res.results], axis=0).astype(np.float32)
```
